# Optimizing a Trainium2 kernel written in Bass

```python
import math
import jax
import jax.numpy as jnp
from jax import lax
import numpy as np

D_MODEL = 1024
BATCH = 16
SEQ = 4096
DEPTH = 1

GRID_W = 64
CTX_LEN = 256
LRU_WIDTH = 1024
LRU_BLOCKS = 8
LRU_BLOCK = LRU_WIDTH // LRU_BLOCKS
LRU_C = 8.0
CONV_W = 4
CONV_PAD = (1, 2)
GDN_HEADS = 8
GDN_DK = 128
GDN_DV = 128
GDN_QK = GDN_HEADS * GDN_DK
GDN_VW = GDN_HEADS * GDN_DV
GDN_CHUNK = 64
QKV_COLS = 2 * GDN_QK + GDN_VW
GDN_SCAN_COLS = QKV_COLS + 4 * GDN_HEADS
OFF_XA = 0
OFF_GA = OFF_XA + LRU_WIDTH
OFF_GDN = OFF_GA + LRU_WIDTH
OFF_Z = OFF_GDN + GDN_SCAN_COLS
OFF_MG = OFF_Z + GDN_VW
IN_COLS = OFF_MG + 2 * D_MODEL
N_GROUPS = 4
EXP_PER_GROUP = 8
N_EXPERTS = N_GROUPS * EXP_PER_GROUP
TOP_K = 2
D_EXPERT = 1024
MOE_BLOCK = 128
LN_EPS = 1e-6
L2_EPS = 1e-6
DN_ALPHA = (2.0 * DEPTH) ** 0.25
DN_BETA = (8.0 * DEPTH) ** -0.25

kernel_name = 'hybrid_rglru_gdn_hmoe_diffusion_block'


def _ln_f32(x):
    xf = x.astype(jnp.float32)
    xc = xf - jnp.mean(xf, axis=-1, keepdims=True)
    return xc * lax.rsqrt(jnp.mean(xc * xc, axis=-1, keepdims=True) + LN_EPS)


def modulate(x, shift, scale):
    return (_ln_f32(x) * (1.0 + scale) + shift).astype(x.dtype)


def post_norm(x, branch, g, b):
    return (_ln_f32(DN_ALPHA * x + branch) * g + b).astype(x.dtype)


def rms_norm(t, w):
    t = t.astype(jnp.float32)
    return t * lax.rsqrt(jnp.mean(t * t, axis=-1, keepdims=True) + LN_EPS) * w


def l2norm(t):
    return t * lax.rsqrt(jnp.sum(t * t, axis=-1, keepdims=True) + L2_EPS)


def dwconv(u, w):
    return lax.conv_general_dilated(
        u, w[:, None, :].astype(u.dtype), window_strides=(1,), padding=(CONV_PAD,),
        dimension_numbers=('NWC', 'WIO', 'NWC'), feature_group_count=u.shape[-1])


def grid_to_colmajor(u, rows):
    b, n, ch = u.shape
    return u.reshape(b, rows, GRID_W, ch).transpose(0, 2, 1, 3).reshape(b, n, ch)


def colmajor_to_grid(u, rows):
    b, n, ch = u.shape
    return u.reshape(b, GRID_W, rows, ch).transpose(0, 2, 1, 3).reshape(b, n, ch)


def rglru_direction(u, wa, ba, wx, bx, lam, h0, reverse):
    b, n, wd = u.shape
    ub = u.reshape(b, n, LRU_BLOCKS, LRU_BLOCK)
    r = jax.nn.sigmoid(jnp.einsum('blni,nij->blnj', ub, wa).reshape(b, n, wd) + ba)
    i = jax.nn.sigmoid(jnp.einsum('blni,nij->blnj', ub, wx).reshape(b, n, wd) + bx)
    log_a = -LRU_C * r * jax.nn.softplus(-lam)
    a = jnp.exp(log_a)
    inp = jnp.sqrt(-jnp.expm1(2.0 * log_a)) * (i * u)

    def step(h, a_b):
        a_t, b_t = a_b
        h = a_t * h + b_t
        return h, h

    h_last, hs = lax.scan(step, h0, (a.swapaxes(0, 1), inp.swapaxes(0, 1)), reverse=reverse)
    return hs.swapaxes(0, 1), h_last


def lru_branch(xa, lp, s0_f, s0_b):
    u = (dwconv(xa, lp['conv_a_w']) + lp['conv_a_b']).astype(jnp.float32)
    h_f, s_f = rglru_direction(u, lp['lru_wa'][0], lp['lru_ba'][0], lp['lru_wx'][0], lp['lru_bx'][0],
                               lp['lru_lambda'][0], s0_f, False)
    h_b, s_b = rglru_direction(u, lp['lru_wa'][1], lp['lru_ba'][1], lp['lru_wx'][1], lp['lru_bx'][1],
                               lp['lru_lambda'][1], s0_b, True)
    return h_f + h_b, s_f, s_b


def unit_lower_inverse(nil):
    eye = jnp.eye(nil.shape[-1], dtype=nil.dtype)
    inv = eye + nil
    power = nil
    for _ in range(int(math.log2(GDN_CHUNK)) - 1):
        power = power @ power
        inv = inv + inv @ power
    return inv


def gated_delta_chunked(q, k, v, g, beta, s0):
    b, n, h, dk = q.shape
    dv = v.shape[-1]
    nc = n // GDN_CHUNK
    c4 = lambda t: t.reshape(b, nc, GDN_CHUNK, h, t.shape[-1]).transpose(0, 3, 1, 2, 4)
    qc, kc, vc = c4(q), c4(k), c4(v)
    gc = g.reshape(b, nc, GDN_CHUNK, h).transpose(0, 3, 1, 2)
    bc = beta.reshape(b, nc, GDN_CHUNK, h).transpose(0, 3, 1, 2)
    G = jnp.cumsum(gc, axis=-1)
    idx = jnp.arange(GDN_CHUNK)
    incl = idx[:, None] >= idx[None, :]
    strict = idx[:, None] > idx[None, :]
    diff = G[..., :, None] - G[..., None, :]
    decay = jnp.where(incl, jnp.exp(jnp.where(incl, diff, 0.0)), 0.0)
    kb = kc * bc[..., None]
    vb = vc * bc[..., None]
    lmat = jnp.where(strict, jnp.einsum('bhncd,bhned->bhnce', kb, kc) * decay, 0.0)
    tmat = unit_lower_inverse(-lmat)
    w_c = tmat @ (kb * jnp.exp(G)[..., None])
    u_c = tmat @ vb
    a_c = jnp.einsum('bhncd,bhned->bhnce', qc, kc) * decay
    qg = qc * jnp.exp(G)[..., None]
    kg = kc * jnp.exp(G[..., -1:] - G)[..., None]
    gl = jnp.exp(G[..., -1])

    def step(s, inp):
        qg_t, kg_t, u_t, w_t, a_t, gl_t = inp
        v_new = u_t - w_t @ s
        o = qg_t @ s + a_t @ v_new
        s = s * gl_t[..., None, None] + jnp.einsum('bhck,bhcv->bhkv', kg_t, v_new)
        return s, o

    xs = tuple(jnp.moveaxis(t, 2, 0) for t in (qg, kg, u_c, w_c, a_c, gl))
    s_last, o = lax.scan(step, s0, xs)
    o = jnp.transpose(o, (1, 0, 3, 2, 4)).reshape(b, n, h, dv)
    return o, s_last


def gdn_branch(u, lp, s0_f, s0_b):
    b, n, _ = u.shape
    qkv = jax.nn.silu(dwconv(u[..., :QKV_COLS], lp['conv_qkv_w'])).astype(jnp.float32)
    q = l2norm(qkv[..., :GDN_QK].reshape(b, n, GDN_HEADS, GDN_DK)) * (GDN_DK ** -0.5)
    k = l2norm(qkv[..., GDN_QK:2 * GDN_QK].reshape(b, n, GDN_HEADS, GDN_DK))
    v = qkv[..., 2 * GDN_QK:].reshape(b, n, GDN_HEADS, GDN_DV)
    lg = u[..., QKV_COLS:].astype(jnp.float32).reshape(b, n, 4, GDN_HEADS)
    a_log, dt_bias = lp['gdn_a_log'], lp['gdn_dt_bias']
    g_f = -jnp.exp(a_log[0]) * jax.nn.softplus(lg[:, :, 0] + dt_bias[0])
    g_b = -jnp.exp(a_log[1]) * jax.nn.softplus(lg[:, :, 1] + dt_bias[1])
    beta_f = jax.nn.sigmoid(lg[:, :, 2])
    beta_b = jax.nn.sigmoid(lg[:, :, 3])
    o_f, s_f = gated_delta_chunked(q, k, v, g_f, beta_f, s0_f)
    rev = lambda t: jnp.flip(t, axis=1)
    o_b, s_b = gated_delta_chunked(rev(q), rev(k), rev(v), rev(g_b), rev(beta_b), s0_b)
    return (o_f + rev(o_b)).reshape(b, n, GDN_VW), s_f, s_b


def mixer_output(p, h_lru, o_gdn, lp):
    b, n, _ = p.shape
    ga = p[..., OFF_GA:OFF_GDN]
    z = p[..., OFF_Z:OFF_MG]
    mg_a = p[..., OFF_MG:OFF_MG + D_MODEL]
    mg_b = p[..., OFF_MG + D_MODEL:]
    ya = (jax.nn.gelu(ga.astype(jnp.float32)) * h_lru).astype(p.dtype)
    zg = jax.nn.silu(z.astype(jnp.float32)).reshape(b, n, GDN_HEADS, GDN_DV)
    yb = (rms_norm(o_gdn.reshape(b, n, GDN_HEADS, GDN_DV), lp['gdn_norm_w']) * zg)
    yb = yb.reshape(b, n, GDN_VW).astype(p.dtype)
    merged = jax.nn.sigmoid(mg_a) * (ya @ lp['w_pa']) + jax.nn.sigmoid(mg_b) * (yb @ lp['w_pb'])
    return merged @ lp['w_out']


def grouped_experts(hf, expert_id, weights, lp):
    t, d = hf.shape
    n_assign = t * TOP_K
    e_flat = expert_id.reshape(n_assign)
    tok_flat = jnp.arange(n_assign) // TOP_K
    w_flat = weights.reshape(n_assign)
    order = jnp.argsort(e_flat)
    e_s, tok_s, w_s = e_flat[order], tok_flat[order], w_flat[order]
    counts = jnp.zeros((N_EXPERTS,), jnp.int32).at[e_flat].add(1)
    padded = (counts + MOE_BLOCK - 1) // MOE_BLOCK * MOE_BLOCK
    ends = jnp.cumsum(counts)
    pends = jnp.cumsum(padded)
    rank = jnp.arange(n_assign) - (ends - counts)[e_s]
    dest = (pends - padded)[e_s] + rank
    n_blocks = (n_assign + N_EXPERTS * (MOE_BLOCK - 1) + MOE_BLOCK - 1) // MOE_BLOCK
    x_pad = jnp.zeros((n_blocks * MOE_BLOCK, d), hf.dtype).at[dest].set(hf[tok_s])
    block_expert = jnp.minimum(
        jnp.searchsorted(pends, jnp.arange(n_blocks) * MOE_BLOCK, side='right'), N_EXPERTS - 1)
    w_gate, w_up, w_down = lp['w_e_gate'], lp['w_e_up'], lp['w_e_down']

    def expert_block(args):
        xb, e = args
        return (jax.nn.silu(xb @ w_gate[e]) * (xb @ w_up[e])) @ w_down[e]

    y_pad = lax.map(expert_block, (x_pad.reshape(n_blocks, MOE_BLOCK, d), block_expert))
    y_pad = y_pad.reshape(n_blocks * MOE_BLOCK, d)
    return jnp.zeros_like(hf).at[tok_s].add(w_s[:, None].astype(hf.dtype) * y_pad[dest])


def hier_moe(hf, lp):
    t = hf.shape[0]
    group_prob = jax.nn.softmax((hf @ lp['w_router_g'] + lp['b_router_g']).astype(jnp.float32), axis=-1)
    p_group, g_idx = lax.top_k(group_prob, 1)
    e_logits = (hf @ lp['w_router_e'] + lp['b_router_e']).astype(jnp.float32)
    e_logits = e_logits.reshape(t, N_GROUPS, EXP_PER_GROUP)
    sel = jnp.broadcast_to(g_idx[:, :, None], (t, 1, EXP_PER_GROUP))
    in_group = jnp.take_along_axis(e_logits, sel, axis=1)[:, 0]
    top_p, top_i = lax.top_k(jax.nn.softmax(in_group, axis=-1), TOP_K)
    weights = p_group * top_p / jnp.sum(top_p, axis=-1, keepdims=True)
    expert_id = g_idx * EXP_PER_GROUP + top_i
    return grouped_experts(hf, expert_id, weights, lp)


def moe_sublayer(xs, shift, scale, gate, lp, ln_g, ln_b):
    h = modulate(xs, shift, scale)
    b, n, d = h.shape
    y = hier_moe(h.reshape(b * n, d), lp).reshape(b, n, d)
    return post_norm(xs, gate * y, ln_g, ln_b)


def setup_inputs(seed: int = 0) -> dict:
    key = jax.random.key(seed)
    ks = jax.random.split(key, 34)
    f32 = jnp.float32
    D = D_MODEL

    def nrm(k, shape, scale):
        return jax.random.normal(k, shape, f32) * scale

    a0 = jax.random.uniform(ks[14], (DEPTH, 2, LRU_WIDTH), f32, 0.9, 0.999)
    s = a0 ** (1.0 / LRU_C)
    dt = jnp.exp(jax.random.uniform(ks[17], (DEPTH, 2, GDN_HEADS), f32, math.log(1e-3), math.log(1e-1)))
    return {
        'x': nrm(ks[0], (BATCH, SEQ, D), 1.0),
        'c': nrm(ks[1], (BATCH, D), 1.0),
        'ctx': nrm(ks[2], (BATCH, CTX_LEN, D), 1.0),
        'c_ctx': nrm(ks[3], (D,), 1.0),
        'w_mod': nrm(ks[4], (DEPTH, D, 6 * D), 0.5 * D ** -0.5),
        'b_mod': nrm(ks[5], (DEPTH, 6 * D), 0.01),
        'w_in': nrm(ks[6], (DEPTH, D, IN_COLS), D ** -0.5),
        'b_in': nrm(ks[7], (DEPTH, IN_COLS), 0.01),
        'conv_a_w': nrm(ks[8], (DEPTH, CONV_W, LRU_WIDTH), CONV_W ** -0.5),
        'conv_a_b': nrm(ks[9], (DEPTH, LRU_WIDTH), 0.01),
        'lru_wa': nrm(ks[10], (DEPTH, 2, LRU_BLOCKS, LRU_BLOCK, LRU_BLOCK), LRU_BLOCK ** -0.5),
        'lru_ba': nrm(ks[11], (DEPTH, 2, LRU_WIDTH), 0.01),
        'lru_wx': nrm(ks[12], (DEPTH, 2, LRU_BLOCKS, LRU_BLOCK, LRU_BLOCK), LRU_BLOCK ** -0.5),
        'lru_bx': nrm(ks[13], (DEPTH, 2, LRU_WIDTH), 0.01),
        'lru_lambda': jnp.log(s) - jnp.log1p(-s),
        'conv_qkv_w': nrm(ks[15], (DEPTH, CONV_W, QKV_COLS), CONV_W ** -0.5),
        'gdn_a_log': jnp.log(jax.random.uniform(ks[16], (DEPTH, 2, GDN_HEADS), f32, 1.0, 16.0)),
        'gdn_dt_bias': dt + jnp.log(-jnp.expm1(-dt)),
        'gdn_norm_w': 1.0 + nrm(ks[18], (DEPTH, GDN_DV), 0.01),
        'w_pa': nrm(ks[19], (DEPTH, LRU_WIDTH, D), DN_BETA * LRU_WIDTH ** -0.5),
        'w_pb': nrm(ks[20], (DEPTH, GDN_VW, D), DN_BETA * GDN_VW ** -0.5),
        'w_out': nrm(ks[21], (DEPTH, D, D), DN_BETA * D ** -0.5),
        'ln1_g': 1.0 + nrm(ks[22], (DEPTH, D), 0.01),
        'ln1_b': nrm(ks[23], (DEPTH, D), 0.01),
        'w_router_g': nrm(ks[24], (DEPTH, D, N_GROUPS), D ** -0.5),
        'b_router_g': nrm(ks[25], (DEPTH, N_GROUPS), 0.01),
        'w_router_e': nrm(ks[26], (DEPTH, D, N_EXPERTS), D ** -0.5),
        'b_router_e': nrm(ks[27], (DEPTH, N_EXPERTS), 0.01),
        'w_e_gate': nrm(ks[28], (DEPTH, N_EXPERTS, D, D_EXPERT), DN_BETA * D ** -0.5),
        'w_e_up': nrm(ks[29], (DEPTH, N_EXPERTS, D, D_EXPERT), DN_BETA * D ** -0.5),
        'w_e_down': nrm(ks[30], (DEPTH, N_EXPERTS, D_EXPERT, D), DN_BETA * D_EXPERT ** -0.5),
        'ln2_g': 1.0 + nrm(ks[31], (DEPTH, D), 0.01),
        'ln2_b': nrm(ks[32], (DEPTH, D), 0.01),
    }


def reference(x, c, ctx, c_ctx, w_mod, b_mod, w_in, b_in, conv_a_w, conv_a_b, lru_wa, lru_ba, lru_wx,
              lru_bx, lru_lambda, conv_qkv_w, gdn_a_log, gdn_dt_bias, gdn_norm_w, w_pa, w_pb, w_out,
              ln1_g, ln1_b, w_router_g, b_router_g, w_router_e, b_router_e, w_e_gate, w_e_up, w_e_down,
              ln2_g, ln2_b):
    bsz, n_lat, _ = x.shape
    rows = n_lat // GRID_W
    x_lat, x_ctx = x, ctx
    for layer in range(DEPTH):
        lp = {
            'conv_a_w': conv_a_w[layer], 'conv_a_b': conv_a_b[layer],
            'lru_wa': lru_wa[layer], 'lru_ba': lru_ba[layer], 'lru_wx': lru_wx[layer],
            'lru_bx': lru_bx[layer], 'lru_lambda': lru_lambda[layer],
            'conv_qkv_w': conv_qkv_w[layer], 'gdn_a_log': gdn_a_log[layer],
            'gdn_dt_bias': gdn_dt_bias[layer], 'gdn_norm_w': gdn_norm_w[layer],
            'w_pa': w_pa[layer], 'w_pb': w_pb[layer], 'w_out': w_out[layer],
            'w_router_g': w_router_g[layer], 'b_router_g': b_router_g[layer],
            'w_router_e': w_router_e[layer], 'b_router_e': b_router_e[layer],
            'w_e_gate': w_e_gate[layer], 'w_e_up': w_e_up[layer], 'w_e_down': w_e_down[layer],
        }
        mod_lat = jnp.split(jax.nn.silu(c) @ w_mod[layer] + b_mod[layer], 6, axis=-1)
        sh1, sc1, g1, sh2, sc2, g2 = [m[:, None, :] for m in mod_lat]
        csh1, csc1, cg1, csh2, csc2, cg2 = jnp.split(
            jax.nn.silu(c_ctx) @ w_mod[layer] + b_mod[layer], 6, axis=-1)
        zero_lru = jnp.zeros((bsz, LRU_WIDTH), jnp.float32)
        zero_gdn = jnp.zeros((bsz, GDN_HEADS, GDN_DK, GDN_DV), jnp.float32)

        p_ctx = modulate(x_ctx, csh1, csc1) @ w_in[layer] + b_in[layer]
        lru_ctx, sa_f, sa_b = lru_branch(p_ctx[..., OFF_XA:OFF_GA], lp, zero_lru, zero_lru)
        gdn_ctx, sb_f, sb_b = gdn_branch(p_ctx[..., OFF_GDN:OFF_Z], lp, zero_gdn, zero_gdn)

        p_lat = modulate(x_lat, sh1, sc1) @ w_in[layer] + b_in[layer]
        lru_lat, _, _ = lru_branch(p_lat[..., OFF_XA:OFF_GA], lp, sa_f, sa_b)
        gdn_lat, _, _ = gdn_branch(grid_to_colmajor(p_lat[..., OFF_GDN:OFF_Z], rows), lp, sb_f, sb_b)
        gdn_lat = colmajor_to_grid(gdn_lat, rows)
        mix_lat = mixer_output(p_lat, lru_lat, gdn_lat, lp)

        if layer < DEPTH - 1:
            mix_ctx = mixer_output(p_ctx, lru_ctx, gdn_ctx, lp)
            x_ctx = post_norm(x_ctx, cg1 * mix_ctx, ln1_g[layer], ln1_b[layer])
            x_ctx = moe_sublayer(x_ctx, csh2, csc2, cg2, lp, ln2_g[layer], ln2_b[layer])

        x_lat = post_norm(x_lat, g1 * mix_lat, ln1_g[layer], ln1_b[layer])
        x_lat = moe_sublayer(x_lat, sh2, sc2, g2, lp, ln2_g[layer], ln2_b[layer])
    return x_lat
```

```python
import math
from contextlib import ExitStack
import numpy as np
import concourse.bass as bass
import concourse.mybir as mybir
from concourse.bass_utils import run_bass_kernel_spmd

F32 = mybir.dt.float32
BF16 = mybir.dt.bfloat16
I32 = mybir.dt.int32
AF = mybir.ActivationFunctionType
ALU = mybir.AluOpType
AX = mybir.AxisListType

ENGS = ("sync", "scalar", "gpsimd", "vector", "tensor")
NDMA_SEM = 16
SAME_ENGINE_SYNC = True

D = 1024
GW = 64
ALPHA = 2.0 ** 0.25
LN_EPS = 1e-6
NEXP = 32


class Prog:
    def __init__(self, nc, stack):
        self.nc = nc
        self.q = {e: [] for e in ENGS}
        self.cnt = {e: 0 for e in ENGS}
        self.esem = {e: stack.enter_context(nc.semaphore("es_" + e)) for e in ENGS}
        self.dsem, self.dcnt, self.dnext = {}, {}, {}
        for e in ("sync", "gpsimd"):
            self.dsem[e] = [stack.enter_context(nc.semaphore("ds_%s%d" % (e, i))) for i in range(NDMA_SEM)]
            self.dcnt[e] = [0] * NDMA_SEM
            self.dnext[e] = 0
        self.semobj = {}
        for e in ENGS:
            self.semobj[("e", e)] = self.esem[e]
        for e in self.dsem:
            for i, s in enumerate(self.dsem[e]):
                self.semobj[("d", e, i)] = s
        self.seen = {e: {} for e in ENGS}
        self.last_w = {}
        self.readers = {}
        self.out_tokens = []

    def _deps(self, eng, reads, writes):
        toks = []
        for k in reads:
            t = self.last_w.get(k)
            if t is not None:
                toks.append(t)
        for k in writes:
            t = self.last_w.get(k)
            if t is not None:
                toks.append(t)
            toks.extend(self.readers.get(k, ()))
        need = {}
        for (sk, v) in toks:
            if sk == ("e", eng) and (eng == "tensor" or not SAME_ENGINE_SYNC):
                continue
            if self.seen[eng].get(sk, 0) >= v:
                continue
            if need.get(sk, 0) < v:
                need[sk] = v
        for sk, v in need.items():
            self.seen[eng][sk] = v
        return list(need.items())

    def _commit(self, tok, reads, writes):
        for k in reads:
            if k in writes:
                continue
            self.readers.setdefault(k, []).append(tok)
        for k in writes:
            self.last_w[k] = tok
            self.readers[k] = []

    def op(self, eng, fn, reads=(), writes=()):
        psr = [k for k in reads if k[0] == "psb"]
        if psr:
            writes = list(writes) + psr
        waits = self._deps(eng, reads, writes)
        self.cnt[eng] += 1
        tok = (("e", eng), self.cnt[eng])
        self.q[eng].append((waits, fn, self.esem[eng], 1))
        self._commit(tok, reads, writes)
        return tok

    def dma(self, eng, fn, reads=(), writes=(), is_output=False):
        i = self.dnext[eng]
        self.dnext[eng] = (i + 1) % NDMA_SEM
        sk = ("d", eng, i)
        waits = self._deps(eng, reads, writes)
        prev = self.dcnt[eng][i]
        if prev > 0 and self.seen[eng].get(sk, 0) < prev:
            self.seen[eng][sk] = prev
            waits.append((sk, prev))
        self.dcnt[eng][i] += 16
        tok = (sk, self.dcnt[eng][i])
        self.q[eng].append((waits, fn, self.dsem[eng][i], 16))
        self._commit(tok, reads, writes)
        if is_output:
            self.out_tokens.append(tok)
        return tok

    def barrier(self):
        toks = [(("e", e), self.cnt[e]) for e in ENGS if self.cnt[e] > 0]
        for e in self.dsem:
            for i in range(NDMA_SEM):
                if self.dcnt[e][i] > 0:
                    toks.append((("d", e, i), self.dcnt[e][i]))
        for eng in ENGS:
            waits = []
            for (sk, v) in toks:
                if sk == ("e", eng) and eng == "tensor":
                    continue
                if self.seen[eng].get(sk, 0) >= v:
                    continue
                self.seen[eng][sk] = v
                waits.append((sk, v))
            if waits:
                self.q[eng].append((waits, None, None, 0))
        self.last_w = {}
        self.readers = {}

    def finish(self):
        self.q["sync"].append((list(self.out_tokens), None, None, 0))

    def emit(self, block):
        def run(engname):
            def body(eng):
                for (waits, fn, sem, inc) in self.q[engname]:
                    for (sk, v) in waits:
                        eng.wait_ge(self.semobj[sk], v)
                    if fn is not None:
                        fn(eng).then_inc(sem, inc)
            return body
        block.sync(run("sync"))
        block.scalar(run("scalar"))
        block.gpsimd(run("gpsimd"))
        block.vector(run("vector"))
        block.tensor(run("tensor"))


class V:
    __slots__ = ("ap", "keys")

    def __init__(self, ap, keys):
        self.ap = ap
        self.keys = tuple(keys)

    def __getitem__(self, idx):
        return V(self.ap[idx], self.keys)

    def re(self, pat, **kw):
        return V(self.ap.rearrange(pat, **kw), self.keys)

    def bc(self, shape):
        return V(self.ap.to_broadcast(list(shape)), self.keys)

    def k(self, *sub):
        return V(self.ap, [kk + tuple(sub) for kk in self.keys])

    def cast(self, dt):
        return V(self.ap.bitcast(dt), self.keys)


def _keys(*vs):
    out = []
    for v in vs:
        if isinstance(v, V):
            out.extend(v.keys)
    return out


def _a(v):
    return v.ap if isinstance(v, V) else v


DT_SIZE = {F32: 4, BF16: 2, I32: 4}


class Arena:
    def __init__(self, nc, stack, nbytes):
        self.words = nbytes // 4
        self.t = stack.enter_context(nc.sbuf_tensor("arena", [128, self.words], F32))
        self.off = 0
        self.uid = 0

    def alloc(self, name, free_shape, dt=F32):
        if isinstance(free_shape, int):
            free_shape = (free_shape,)
        nel = 1
        for s in free_shape:
            nel *= s
        words = (nel * DT_SIZE[dt] + 31) // 32 * 8
        assert self.off + words <= self.words, "SBUF arena overflow at %s (%d + %d > %d)" % (name, self.off, words, self.words)
        ap = self.t[:, self.off:self.off + words]
        if dt != F32:
            ap = ap.bitcast(dt)
        ap = ap[:, 0:nel]
        if len(free_shape) == 2:
            ap = ap.rearrange("p (a b) -> p a b", b=free_shape[1])
        elif len(free_shape) == 3:
            ap = ap.rearrange("p (a b c) -> p a b c", b=free_shape[1], c=free_shape[2])
        elif len(free_shape) == 4:
            ap = ap.rearrange("p (a b c d) -> p a b c d", b=free_shape[1], c=free_shape[2], d=free_shape[3])
        self.off += words
        self.uid += 1
        return V(ap, [(name, self.uid)])

    def mark(self):
        return self.off

    def reset(self, m):
        self.off = m


class Ring:
    def __init__(self, items):
        self.items = items
        self.i = 0

    def next(self):
        v = self.items[self.i % len(self.items)]
        self.i += 1
        return v


class KB:
    def __init__(self, NB, L, CTX, CAP, debug=()):
        self.NB, self.L, self.CTX, self.CAP = NB, L, CTX, CAP
        self.R = L // GW
        self.debug = set(debug)
        self.nc = bass.Bass("TRN2", target_bir_lowering=False)
        self.alt = 0

    def din(self, name, shape, dt=F32):
        return V(self.nc.dram_tensor(name, list(shape), dt, kind="ExternalInput").ap(), [])

    def dscr(self, name, shape, dt):
        kind = "ExternalOutput" if name in self.debug else "Internal"
        return V(self.nc.dram_tensor(name, list(shape), dt, kind=kind).ap(), [(name,)])

    def mm(self, out, lhsT, rhs, start=True, stop=True):
        o, l, r = out.ap, lhsT.ap, rhs.ap
        self.P.op("tensor", lambda e: e.matmul(o, lhsT=l, rhs=r, start=start, stop=stop),
                  reads=_keys(lhsT, rhs), writes=_keys(out))

    def tr(self, out, in_, ident):
        o, i, d = out.ap, in_.ap, ident.ap
        self.P.op("tensor", lambda e: e.transpose(o, i, d), reads=_keys(in_, ident), writes=_keys(out))

    def act(self, out, in_, func, bias=0.0, scale=1.0, eng="scalar"):
        o, i, b, s = out.ap, in_.ap, _a(bias), _a(scale)
        self.P.op("scalar", lambda e: e.activation(out=o, in_=i, func=func, bias=b, scale=s),
                  reads=_keys(in_, bias, scale), writes=_keys(out))

    def ts(self, out, in0, s1, s2, op0, op1=None, eng="vector"):
        o, i, a, b = out.ap, in0.ap, _a(s1), _a(s2)
        if op1 is None:
            fn = lambda e: e.tensor_scalar(out=o, in0=i, scalar1=a, scalar2=None, op0=op0)
        else:
            fn = lambda e: e.tensor_scalar(out=o, in0=i, scalar1=a, scalar2=b, op0=op0, op1=op1)
        self.P.op(eng, fn, reads=_keys(in0, s1, s2), writes=_keys(out))

    def tt(self, out, in0, in1, op, eng="vector"):
        o, a, b = out.ap, in0.ap, in1.ap
        self.P.op(eng, lambda e: e.tensor_tensor(out=o, in0=a, in1=b, op=op), reads=_keys(in0, in1), writes=_keys(out))

    def stt(self, out, in0, scalar, in1, op0, op1):
        o, a, s, b = out.ap, in0.ap, _a(scalar), in1.ap
        self.P.op("vector", lambda e: e.scalar_tensor_tensor(out=o, in0=a, scalar=s, in1=b, op0=op0, op1=op1),
                  reads=_keys(in0, scalar, in1), writes=_keys(out))

    def cp(self, out, in_, eng="vector"):
        o, i = out.ap, in_.ap
        if eng == "scalar":
            self.P.op("scalar", lambda e: e.activation(out=o, in_=i, func=AF.Identity), reads=_keys(in_), writes=_keys(out))
        else:
            self.P.op(eng, lambda e: e.tensor_copy(out=o, in_=i), reads=_keys(in_), writes=_keys(out))

    def evac(self, out, in_):
        self.alt ^= 1
        self.cp(out, in_, eng="scalar" if self.alt else "vector")

    def memset(self, out, val, eng="gpsimd"):
        o = out.ap
        self.P.op(eng, lambda e: e.memset(o, val), writes=_keys(out))

    def scan(self, out, d0, d1, init):
        o, a, b, i = out.ap, d0.ap, d1.ap, _a(init)
        self.P.op("vector", lambda e: e.tensor_tensor_scan(out=o, data0=a, data1=b, initial=i, op0=ALU.mult, op1=ALU.add),
                  reads=_keys(d0, d1, init), writes=_keys(out))

    def dma(self, out, in_, eng="sync", is_output=False):
        o, i = out.ap, in_.ap
        if eng == "gpsimd":
            fn = lambda e: e.dma_start(out=o, in_=i, max_dma_last_dim=4096)
        else:
            fn = lambda e: e.dma_start(out=o, in_=i)
        self.P.dma(eng, fn, reads=_keys(in_), writes=_keys(out), is_output=is_output)

    def scatter(self, out_dram, idx, in_sb):
        o, x, i = out_dram.ap, idx.ap, in_sb.ap
        self.P.dma("gpsimd", lambda e: e.indirect_dma_start(out=o, out_offset=bass.IndirectOffsetOnAxis(ap=x, axis=0),
                                                            in_=i, in_offset=None),
                   reads=_keys(idx, in_sb), writes=_keys(out_dram))

    def gather(self, out_sb, in_dram, idx):
        o, x, i = out_sb.ap, idx.ap, in_dram.ap
        self.P.dma("gpsimd", lambda e: e.indirect_dma_start(out=o, out_offset=None, in_=i,
                                                            in_offset=bass.IndirectOffsetOnAxis(ap=x, axis=0)),
                   reads=_keys(idx, in_dram), writes=_keys(out_sb))

    def bank(self):
        return self.banks.next()

    def ln_stats(self, x):
        st = self.st_ring.next()
        mv = st[:, 12:14]
        self.P.op("vector", (lambda o, i: lambda e: e.bn_stats(out=o, in_=i))(st.ap[:, 0:6], x.ap[:, 0:512]),
                  reads=_keys(x), writes=_keys(st))
        self.P.op("vector", (lambda o, i: lambda e: e.bn_stats(out=o, in_=i))(st.ap[:, 6:12], x.ap[:, 512:1024]),
                  reads=_keys(x, st), writes=_keys(st))
        self.P.op("vector", (lambda o, i: lambda e: e.bn_aggr(out=o, in_=i))(mv.ap, st.ap[:, 0:12]),
                  reads=_keys(st), writes=_keys(st))
        self.act(st[:, 14:15], st[:, 13:14], AF.Sqrt, bias=LN_EPS)
        self.recip(st[:, 15:16], st[:, 14:15])
        return st[:, 12:13], st[:, 15:16]

    def recip(self, out, in_):
        o, i = out.ap, in_.ap
        self.P.op("vector", lambda e: e.reciprocal(out=o, in_=i), reads=_keys(in_), writes=_keys(out))

    def build(self):
        NB, L, CTX, CAP, R = self.NB, self.L, self.CTX, self.CAP, self.R
        nc = self.nc
        NBP = NB + 1 + ((NB + 1) % 2)
        NT = NB * L // 128
        x_d = self.din("x", [NB, L, D])
        ctx_d = self.din("ctx", [NB, CTX, D])
        cT_d = self.din("cT", [128, 8, NBP])
        wmod_d = self.din("w_mod", [D, 6 * D])
        bmodT_d = self.din("bmodT", [128, 16])
        bmodbc_d = self.din("bmod_bc", [128, 4 * D])
        wint_d = self.din("w_in_t", [64, 128, 8, 128])
        winlg_d = self.din("w_in_lg", [128, 8, 32])
        binT_d = self.din("b_inT", [128, 65])
        conva_d = self.din("conv_a", [128, 8, 5])
        lruw_d = self.din("lru_w", [128, 4, 8, 128])
        lrub_d = self.din("lru_b", [128, 4, 8])
        lrulam_d = self.din("lru_lam", [128, 2, 8])
        convq_d = self.din("conv_qkv", [128, 24, 4])
        gdnrows_d = self.din("gdn_rows", [128, 32])
        gdnnw_d = self.din("gdn_nw", [128, 1])
        wpa_d = self.din("w_pa", [D, D])
        wpb_d = self.din("w_pb", [D, D])
        wout_d = self.din("w_out", [D, D])
        lnbc_d = self.din("ln_bc", [128, 4, D])
        wrt_d = self.din("w_rt", [128, 8, 36])
        brt_d = self.din("b_rt_bc", [128, 36])
        weg_d = self.din("w_eg", [NEXP, D, D])
        weu_d = self.din("w_eu", [NEXP, D, D])
        wed_d = self.din("w_ed", [NEXP, D, D])
        out_d = V(nc.dram_tensor("out", [NB * L, D], F32, kind="ExternalOutput").ap(), [("out",)])
        yaT_d = self.dscr("yaT", [NB, 8, 128, L], BF16)
        ybT_d = self.dscr("ybT", [NB, 8, 128, L], BF16)
        sg_d = self.dscr("sg", [NB, 16, 128, L], F32)
        x1_d = self.dscr("x1s", [NB * L, D], F32)
        xg_d = self.dscr("xg", [NEXP * CAP, D], BF16)
        yg_d = self.dscr("yg", [NEXP * CAP, D], F32)
        bcs_d = self.dscr("bcs", [NB, 4, 128, D], F32)
        pq_d = self.dscr("pq", [32, 128, L], F32)
        dbg_d = {}

        st = ExitStack()
        with st:
            self.P = P = Prog(nc, st)
            cap_bytes = 206 * 1024
            self.A = A = Arena(nc, st, cap_bytes)
            banks = []
            for i in range(8):
                t = st.enter_context(nc.psum_tensor("psb%d" % i, [128, 512], F32))
                banks.append(V(t[:, :], [("psb", i)]))
            self.banks = Ring(banks)

            ident_f = A.alloc("ident_f", 128)
            ones_f = A.alloc("ones_f", 128)
            mIL = A.alloc("mIL", 128)
            mSL = A.alloc("mSL", 128)
            mIU = A.alloc("mIU", 128)
            mSU = A.alloc("mSU", 128)
            ident_b = A.alloc("ident_b", 128, BF16)
            ones_b = A.alloc("ones_b", 128, BF16)
            mSU_b = A.alloc("mSU_b", 128, BF16)
            self.st_ring = Ring([A.alloc("lnst%d" % i, 16) for i in range(4)])

            def mask(dst, pat_step, cm, op):
                self.memset(dst, 1.0)
                o = dst.ap
                self.P.op("gpsimd", lambda e: e.affine_select(out=o, in_=o, pattern=[[pat_step, 128]], base=0,
                                                               channel_multiplier=cm, compare_op=op, fill=0.0),
                          reads=_keys(dst), writes=_keys(dst))
            self.memset(ones_f, 1.0)
            mask(ident_f, -1, 1, ALU.is_equal)
            mask(mIL, -1, 1, ALU.is_ge)
            mask(mSL, -1, 1, ALU.is_gt)
            mask(mIU, 1, -1, ALU.is_ge)
            mask(mSU, 1, -1, ALU.is_gt)
            self.cp(ident_b, ident_f, eng="gpsimd")
            self.cp(ones_b, ones_f, eng="gpsimd")
            self.cp(mSU_b, mSU, eng="gpsimd")

            modT = A.alloc("modT", (16, NBP))
            binT = A.alloc("binT", 65)
            conva = A.alloc("conva", (8, 5))
            lrub = A.alloc("lrub", (4, 8))
            cch = A.alloc("cch", (2, 8))
            convq = A.alloc("convq", (24, 4))
            gdnrows = A.alloc("gdnrows", 32)
            nega = A.alloc("nega", 16)
            gdnnw = A.alloc("gdnnw", 1)
            lruw = A.alloc("lruw", (4, 8, 128), BF16)
            lru_state = A.alloc("lru_state", (8, 2))
            gdn_state = A.alloc("gdn_state", (8, 2, 128))
            DEST = A.alloc("DEST", (NT, 2), I32)
            WT = A.alloc("WT", (NT, 2))
            CNT = A.alloc("CNT", 32)
            ECAP = A.alloc("ECAP", 32)
            brt = A.alloc("brt", 36)
            for (dst, src) in ((binT, binT_d), (conva, conva_d), (lrub, lrub_d), (cch, lrulam_d), (convq, convq_d),
                               (gdnrows, gdnrows_d), (gdnnw, gdnnw_d), (brt, brt_d)):
                self.dma(dst, src)
            self.dma(lruw, lruw_d, eng="gpsimd")
            self.act(cch, cch, AF.Exp, scale=-1.0)
            self.act(cch, cch, AF.Ln, bias=1.0)
            self.ts(cch, cch, -8.0, None, ALU.mult)
            self.act(nega, gdnrows[:, 16:32], AF.Exp)
            self.ts(nega, nega, -1.0, None, ALU.mult)
            self.memset(CNT, 0.0)
            ec = ECAP.ap
            self.P.op("gpsimd", lambda e: e.iota(ec, pattern=[[CAP, 32]], base=0, channel_multiplier=0,
                                                 allow_small_or_imprecise_dtypes=True), writes=_keys(ECAP))
            zt = A.alloc("zt", D, BF16)
            self.memset(zt, 0.0)
            for e_ in range(NEXP):
                self.dma(V(xg_d.ap[e_ * CAP:(e_ + 1) * CAP, :].rearrange("(n p) d -> p n d", p=128), xg_d.keys),
                         V(zt.ap.unsqueeze(1).to_broadcast([128, CAP // 128, D]), zt.keys))
            base_mark = A.mark()

            cT = A.alloc("cT", (8, NBP))
            self.dma(cT, cT_d)
            sT = A.alloc("sT", (8, NBP))
            self.act(sT, cT, AF.Silu)
            bmodT = A.alloc("bmodT", 16)
            self.dma(bmodT, bmodT_d)
            wmA = A.alloc("wmA", (8, 2048))
            wm_v = wmod_d.re("(k p) n -> p k n", p=128)
            for k in range(8):
                self.dma(wmA[:, k, :].k(k), wm_v[:, k, 0:2048])
            for cc in range(16):
                ps = self.bank()
                for k in range(8):
                    self.mm(ps[:, 0:NBP], wmA[:, k, cc * 128:(cc + 1) * 128].k(k), sT[:, k, :], start=(k == 0), stop=(k == 7))
                self.ts(modT[:, cc, :], ps[:, 0:NBP], bmodT[:, cc:cc + 1], 1.0 if cc >= 8 else 0.0, ALU.add, ALU.add)
            A.reset(A.mark() - 0)
            sbc = [A.alloc("sbc%d" % b, (8, 128)) for b in range(NB)]
            for b in range(NB):
                self.cp(sbc[b], sT[:, :, b:b + 1].bc([128, 8, 128]))
            wmB = Ring([A.alloc("wmB%d" % i, (8, 512)) for i in range(2)])
            bmB = Ring([A.alloc("bmB%d" % i, 512) for i in range(2)])
            stg = Ring([A.alloc("stg%d" % i, 512) for i in range(3)])
            for ct in range(8):
                w = wmB.next()
                bm = bmB.next()
                for k in range(8):
                    self.dma(w[:, k, :].k(k), wm_v[:, k, 2048 + ct * 512:2048 + (ct + 1) * 512])
                self.dma(bm, bmodbc_d[:, ct * 512:(ct + 1) * 512])
                for b in range(NB):
                    ps = self.bank()
                    for k in range(8):
                        self.mm(ps, sbc[b][:, k, :], w[:, k, :].k(k), start=(k == 0), stop=(k == 7))
                    sg_ = stg.next()
                    if ct // 2 == 2:
                        self.stt(sg_, ps, 1.0, bm, ALU.add, ALU.add)
                    else:
                        self.tt(sg_, ps, bm, ALU.add)
                    self.dma(bcs_d[b, ct // 2, :, (ct % 2) * 512:(ct % 2 + 1) * 512].k(b, ct), sg_)
            self.P.barrier()
            A.reset(base_mark)

            import os as _os
            KSTOP = int(_os.environ.get("KSTOP", "99"))
            for b in range(NB if KSTOP > 0 else 0):
                for which in ("ctx", "lat"):
                    if KSTOP == 1 and which == "lat":
                        continue
                    tabs, tab_mark = self.mixer(b, which, locals())
                    self.P.barrier()
                    A.reset(tab_mark)
                    if KSTOP > 2:
                        self.gdn(b, which == "lat", tabs, locals())
                    self.P.barrier()
                    A.reset(base_mark)
                if KSTOP > 3:
                    self.merge(b, locals())
                self.P.barrier()
                A.reset(base_mark)
            if KSTOP > 4:
                self.experts(locals())
            self.P.barrier()
            A.reset(base_mark)
            if KSTOP > 5:
                self.combine(locals())
            P.finish()
            with nc.Block() as block:
                P.emit(block)
        return nc

    def load_wchunk(self, wring, wint_d, c):
        w = wring.next()
        self.dma(w, wint_d[c], eng="gpsimd")
        return w

    def proj(self, w, M, hT, Lx, evac_fn):
        TL = min(512, Lx)
        for t in range(Lx // TL):
            ps = self.bank()
            for k in range(8):
                self.mm(ps[0:M, 0:TL], w[:, k, 0:M], hT[:, k, t * TL:(t + 1) * TL], start=(k == 0), stop=(k == 7))
            evac_fn(t, TL, ps[0:M, 0:TL])

    def mixer(self, b, which, E):
        A, P = self.A, self.P
        NB, L, CTX, R = self.NB, self.L, self.CTX, self.R
        lat = which == "lat"
        Lx = L if lat else CTX
        nch = Lx // 128
        NH = nch * 8
        col = b if lat else NB
        src = E["x_d"][b] if lat else E["ctx_d"][b]
        modT, binT, ident_f, ident_b, ones_f, ones_b = E["modT"], E["binT"], E["ident_f"], E["ident_b"], E["ones_f"], E["ones_b"]
        mIL, mIU = E["mIL"], E["mIU"]
        gdnrows, nega = E["gdnrows"], E["nega"]
        wint_d = E["wint_d"]
        tabs = {}
        for nm in ("BT", "NBT", "GC", "EKG", "GL", "BEG"):
            tabs[nm] = A.alloc(nm, (2, nch, 8))
        tab_mark = A.mark()
        hT = A.alloc("hT", (8, Lx), BF16)
        m0 = A.mark()
        xin = Ring([A.alloc("xin%d" % i, D) for i in range(2)])
        xnr = Ring([A.alloc("xn%d" % i, D) for i in range(2)])
        for t in range(Lx // 128):
            xt = xin.next()
            self.dma(xt, src[t * 128:(t + 1) * 128, :])
            mean, rstd = self.ln_stats(xt)
            xn = xnr.next()
            self.ts(xn, xt, mean, rstd, ALU.subtract, ALU.mult)
            for half in range(2):
                ps = self.bank()
                for kk in range(4):
                    k = half * 4 + kk
                    self.tr(ps[:, kk * 128:(kk + 1) * 128], xn[:, k * 128:(k + 1) * 128], ident_f)
                for kk in range(4):
                    k = half * 4 + kk
                    o = hT[:, k, t * 128:(t + 1) * 128].k(t)
                    if kk % 2 == 0:
                        self.act(o, ps[:, kk * 128:(kk + 1) * 128], AF.Identity, bias=modT[:, k, col:col + 1], scale=modT[:, 8 + k, col:col + 1])
                    else:
                        self.ts(o, ps[:, kk * 128:(kk + 1) * 128], modT[:, 8 + k, col:col + 1], modT[:, k, col:col + 1], ALU.mult, ALU.add)
        hT = V(hT.ap, [kk + (t,) for kk in hT.keys for t in range(Lx // 128)])
        self.P.barrier()
        A.reset(m0)
        wring = Ring([A.alloc("wch%d" % i, (8, 128), BF16) for i in range(3)])
        m1 = A.mark()

        conva, lrub, cch, lruw, lru_state = E["conva"], E["lrub"], E["cch"], E["lruw"], E["lru_state"]
        xa_pad = A.alloc("xa_pad", Lx + 3)
        u = A.alloc("u", Lx)
        u_bf = A.alloc("u_bf", Lx, BF16)
        Ab = A.alloc("Abuf", Lx)
        Ib = A.alloc("Ibuf", Lx)
        HF = A.alloc("HF", Lx)
        Tb = V(xa_pad.ap[:, 0:Lx], xa_pad.keys)
        HB = Tb
        YA = V(Ib.ap.bitcast(BF16)[:, 0:Lx], Ib.keys)
        TL = min(512, Lx)
        for n in range(8):
            w = self.load_wchunk(wring, wint_d, n)
            self.memset(xa_pad[:, 0:1], 0.0)
            self.memset(xa_pad[:, Lx + 1:Lx + 3], 0.0)
            self.proj(w, 128, hT, Lx, lambda t, tl, ps: self.act(xa_pad[:, 1 + t * tl:1 + (t + 1) * tl], ps, AF.Identity,
                                                                    bias=binT[:, n:n + 1]))
            self.ts(u, xa_pad[:, 0:Lx], conva[:, n, 0:1], conva[:, n, 4:5], ALU.mult, ALU.add)
            for j in range(1, 4):
                self.stt(u, xa_pad[:, j:j + Lx], conva[:, n, j:j + 1], u, ALU.mult, ALU.add)
            self.cp(u_bf, u, eng="scalar")
            for d in range(2):
                for t in range(Lx // TL):
                    sl = slice(t * TL, (t + 1) * TL)
                    ps = self.bank()
                    self.mm(ps[:, 0:TL], lruw[:, 2 * d, n, :], u_bf[:, sl])
                    self.act(Ab[:, sl], ps[:, 0:TL], AF.Sigmoid, bias=lrub[:, 2 * d, n:n + 1])
                    ps2 = self.bank()
                    self.mm(ps2[:, 0:TL], lruw[:, 2 * d + 1, n, :], u_bf[:, sl])
                    self.act(Ib[:, sl], ps2[:, 0:TL], AF.Sigmoid, bias=lrub[:, 2 * d + 1, n:n + 1])
                self.act(Ab, Ab, AF.Exp, scale=cch[:, d, n:n + 1])
                self.tt(Tb, Ab, Ab, ALU.mult, eng="gpsimd")
                self.ts(Tb, Tb, 0.99999994, -1.0, ALU.min, ALU.mult)
                self.act(Tb, Tb, AF.Sqrt, bias=1.0)
                self.tt(Ib, Ib, u, ALU.mult, eng="gpsimd")
                self.tt(Ib, Ib, Tb, ALU.mult)
                init = lru_state[:, n, d:d + 1] if lat else 0.0
                if d == 0:
                    self.scan(HF, Ab, Ib, init)
                else:
                    self.scan(HB[:, ::-1], Ab[:, ::-1], Ib[:, ::-1], init)
            if not lat:
                self.cp(lru_state[:, n, 0:1], HF[:, Lx - 1:Lx])
                self.cp(lru_state[:, n, 1:2], HB[:, 0:1])
            else:
                self.tt(HF, HF, HB, ALU.add, eng="gpsimd")
                w = self.load_wchunk(wring, wint_d, 8 + n)
                self.proj(w, 128, hT, Lx, lambda t, tl, ps: self.act(Ab[:, t * tl:(t + 1) * tl], ps, AF.Identity,
                                                                        bias=binT[:, 8 + n:9 + n]))
                self.tt(Tb, Ab, Ab, ALU.mult, eng="gpsimd")
                self.ts(Tb, Tb, 0.044715, 1.0, ALU.mult, ALU.add)
                self.tt(Tb, Tb, Ab, ALU.mult)
                self.act(Tb, Tb, AF.Sigmoid, scale=2.0 * math.sqrt(2.0 / math.pi))
                self.tt(Tb, Tb, Ab, ALU.mult, eng="gpsimd")
                self.tt(YA, Tb, HF, ALU.mult)
                self.dma(E["yaT_d"][b, n].k(b, n), YA)
        self.P.barrier()
        A.reset(m1)

        SG = Ring([A.alloc("SG%d" % i, Lx) for i in range(2)])
        if lat:
            for dc in range(16):
                w = self.load_wchunk(wring, wint_d, 48 + dc)
                sgb = SG.next()
                self.proj(w, 128, hT, Lx, lambda t, tl, ps: self.act(sgb[:, t * tl:(t + 1) * tl], ps, AF.Sigmoid,
                                                                        bias=binT[:, 48 + dc:49 + dc]))
                self.dma(E["sg_d"][b, dc].k(b, dc), sgb)
        pq = E["pq_d"]
        for c in range(16, 48 if lat else 40):
            w = self.load_wchunk(wring, wint_d, c)
            sgb = SG.next()
            func = AF.Silu if c >= 40 else AF.Identity
            self.proj(w, 128, hT, Lx, lambda t, tl, ps: self.act(self.cm_out(sgb, 0, Lx, lat, t, tl), self.cm_in(ps, lat),
                                                                    func, bias=binT[:, c:c + 1]))
            self.dma(V(pq.ap[c - 16][:, 0:Lx], [kk + (c,) for kk in pq.keys]), sgb)
        self.P.barrier()
        A.reset(m1)

        wlg = A.alloc("wlg", (8, 32), BF16)
        self.dma(wlg, E["winlg_d"], eng="gpsimd")
        lgT = A.alloc("lgT", Lx)
        for t in range(Lx // TL):
            ps = self.bank()
            for k in range(8):
                self.mm(ps[0:32, 0:TL], wlg[:, k, :], hT[:, k, t * TL:(t + 1) * TL], start=(k == 0), stop=(k == 7))
            self.act(self.cm_out(lgT, 0, Lx, lat, t, TL)[0:32], self.cm_in(ps[0:32, 0:TL], lat), AF.Identity, bias=binT[0:32, 64:65])
        lg_tm = A.alloc("lg_tm", (nch, 32))
        for n in range(nch):
            ps = self.bank()
            self.tr(ps[:, 0:32], lgT[0:32, n * 128:(n + 1) * 128], ident_f[0:32, 0:32])
            self.evac(lg_tm[:, n, :], ps[:, 0:32])
        XG = A.alloc("XG", (2, nch, 8))
        AXb = A.alloc("AXb", (2, nch, 8))
        Gt = A.alloc("Gt", (2, nch, 8))
        GTOT = A.alloc("GTOT", (2, nch, 8))
        BT, NBT, GC, EKG, GL, BEG = (tabs[nm] for nm in ("BT", "NBT", "GC", "EKG", "GL", "BEG"))
        lg4 = lg_tm.re("p n (j h) -> p j n h", h=8)
        dtb = gdnrows[:, 0:16].re("p (d h) -> p d h", h=8)
        self.tt(XG, lg4[:, 0:2], V(dtb.ap.unsqueeze(2).to_broadcast([128, 2, nch, 8]), dtb.keys), ALU.add)
        self.act(AXb, XG, AF.Abs)
        self.act(AXb, AXb, AF.Exp, scale=-1.0)
        self.act(AXb, AXb, AF.Ln, bias=1.0)
        self.stt(XG, XG, 0.0, AXb, ALU.max, ALU.add)
        ng = nega.re("p (d h) -> p d h", h=8)
        self.tt(Gt, XG, V(ng.ap.unsqueeze(2).to_broadcast([128, 2, nch, 8]), ng.keys), ALU.mult)
        self.act(BT, lg4[:, 2:4], AF.Sigmoid)
        self.ts(NBT, BT, -1.0, None, ALU.mult)
        for d in range(2):
            ps = self.bank()
            tri = mIU if d == 0 else mIL
            self.mm(ps[:, 0:NH], tri, Gt[:, d].re("p n h -> p (n h)"))
            self.evac(GC[:, d].re("p n h -> p (n h)"), ps[:, 0:NH])
            ps = self.bank()
            self.mm(ps[:, 0:NH], ones_f, Gt[:, d].re("p n h -> p (n h)"))
            self.evac(GTOT[:, d].re("p n h -> p (n h)"), ps[:, 0:NH])
        self.tt(EKG, GTOT, GC, ALU.subtract)
        self.act(EKG, EKG, AF.Exp)
        self.act(GL, GTOT, AF.Exp)
        self.act(BEG, GC, AF.Exp)
        self.tt(BEG, BEG, BT, ALU.mult)
        return tabs, tab_mark

    def cm_out(self, buf, off, Lx, lat, t, tl):
        if not lat:
            return buf[:, off + t * tl:off + (t + 1) * tl]
        rows = tl // GW
        v = buf[:, off:off + Lx].re("p (c r) -> p r c", c=GW)
        return v[:, t * rows:(t + 1) * rows, :]

    def cm_in(self, ps, lat):
        if not lat:
            return ps
        return ps.re("p (r c) -> p r c", c=GW)

    def gdn(self, b, lat, tabs, E):
        A, P = self.A, self.P
        NB, R = self.NB, self.R
        Lx = self.L if lat else self.CTX
        nch = Lx // 128
        TL = min(512, Lx)
        binT, ident_f, ident_b, ones_f, ones_b = E["binT"], E["ident_f"], E["ident_b"], E["ones_f"], E["ones_b"]
        mIL, mSL, mIU, mSU = E["mIL"], E["mSL"], E["mIU"], E["mSU"]
        gdnnw, convq, gdn_state = E["gdnnw"], E["convq"], E["gdn_state"]
        BT, NBT, GC, EKG, GL, BEG = (tabs[nm] for nm in ("BT", "NBT", "GC", "EKG", "GL", "BEG"))
        pq = E["pq_d"]
        if not lat:
            self.memset(gdn_state, 0.0)
        PAD = A.alloc("PAD", Lx + 16)
        CV = A.alloc("CV", Lx)
        qT = A.alloc("qT", Lx, BF16)
        kT = A.alloc("kT", Lx, BF16)
        vT = A.alloc("vT", Lx, BF16)
        OT = A.alloc("OT", Lx)
        OTa = V(OT.ap, [kk + (n,) for kk in OT.keys for n in range(nch)])
        k_tm = A.alloc("k_tm", (nch, 128), BF16)
        v_tm = A.alloc("v_tm", (nch, 128), BF16)
        YB = V(PAD.ap[:, 0:Lx // 2].bitcast(BF16), PAD.keys)
        SQr = Ring([A.alloc("SQ%d" % i, TL, BF16) for i in range(2)])
        RTr = Ring([A.alloc("RT%d" % i, TL) for i in range(2)])
        S_bf = [A.alloc("S_bf%d" % d, 128, BF16) for d in range(2)]

        GC_ = 2 if nch % 2 == 0 else 1
        NI = 2 * GC_
        TMP = [{nm: A.alloc("%s_t%d" % (nm, i), 128) for nm in ("DG", "M1", "M2", "ER", "E2i", "KB", "VB")} for i in range(NI)]
        RNG = [{nm: Ring([A.alloc("%s_r%d_%d" % (nm, i, j), 128) for j in range(3)]) for nm in ("N", "M", "X")} for i in range(NI)]
        OUTT = [[{nm: A.alloc("%s_o%d_%d" % (nm, par, i), 128) for nm in ("ACT", "WCT", "UC", "QG", "KG", "VN")} for i in range(NI)]
                for par in range(2)]

        import os as _os
        KG = int(_os.environ.get("KG", "99"))
        for h in range(8 if KG > 0 else 0):
            for (qi, dst) in ((0, qT), (1, kT), (2, vT)):
                c = 16 + qi * 8 + h
                self.memset(PAD[:, 7:8], 0.0)
                self.memset(PAD[:, Lx + 8:Lx + 10], 0.0)
                self.dma(PAD[:, 8:8 + Lx], V(pq.ap[c - 16][:, 0:Lx], [kk + (c,) for kk in pq.keys]))
                cw = convq[:, qi * 8 + h, :]
                self.ts(CV, PAD[:, 7:7 + Lx], cw[:, 0:1], None, ALU.mult)
                for j in range(1, 4):
                    self.stt(CV, PAD[:, 7 + j:7 + j + Lx], cw[:, j:j + 1], CV, ALU.mult, ALU.add)
                self.act(CV, CV, AF.Silu)
                if qi < 2:
                    for t in range(Lx // TL):
                        sl = slice(t * TL, (t + 1) * TL)
                        sq, rt = SQr.next(), RTr.next()
                        self.tt(sq, CV[:, sl], CV[:, sl], ALU.mult, eng="gpsimd")
                        ps = self.bank()
                        self.mm(ps[:, 0:TL], ones_b, sq)
                        self.act(rt, ps[:, 0:TL], AF.Sqrt, bias=1e-6)
                        self.recip(rt, rt)
                        self.stt(dst[:, sl], CV[:, sl], (128.0 ** -0.5) if qi == 0 else 1.0, rt, ALU.mult, ALU.mult)
                else:
                    self.cp(dst, CV, eng="scalar")
            if KG < 2:
                continue
            for n in range(nch):
                ps = self.bank()
                self.mm(ps[:, 0:128], kT[:, n * 128:(n + 1) * 128], ident_b)
                self.mm(ps[:, 128:256], vT[:, n * 128:(n + 1) * 128], ident_b)
                self.cp(k_tm[:, n, :].k(n), ps[:, 0:128], eng="scalar")
                self.cp(v_tm[:, n, :].k(n), ps[:, 128:256], eng="vector")
            self.memset(OTa, 0.0)
            ngroups = nch // GC_

            def group_insts(p):
                L_ = []
                for j in range(GC_):
                    for d in range(2):
                        s_ = p * GC_ + j
                        n = s_ if d == 0 else nch - 1 - s_
                        i = j * 2 + d
                        L_.append({"d": d, "n": n, "cs": slice(n * 128, (n + 1) * 128), "T": TMP[i], "R": RNG[i],
                                   "O": OUTT[p % 2][i]})
                return L_

            def pre(insts):
                for I in insts:
                    d, n, T = I["d"], I["n"], I["T"]
                    ktn, vtn = k_tm[:, n, :].k(n), v_tm[:, n, :].k(n)
                    I["Gp"] = GC[:, d, n, h:h + 1]
                    self.ts(T["KB"], ktn, BEG[:, d, n, h:h + 1], None, ALU.mult, eng="gpsimd")
                    self.ts(I["O"]["KG"], ktn, EKG[:, d, n, h:h + 1], None, ALU.mult, eng="gpsimd")
                    self.ts(T["VB"], vtn, BT[:, d, n, h:h + 1], None, ALU.mult, eng="gpsimd")
                    self.ts(T["DG"], ident_f, I["Gp"], None, ALU.mult, eng="gpsimd")
                for I in insts:
                    I["psg"] = self.bank()
                    self.mm(I["psg"][:, 0:128], ones_f, I["T"]["DG"])
                for I in insts:
                    T, psg = I["T"], I["psg"]
                    self.ts(T["M1"], psg[:, 0:128], I["Gp"], 0.0, ALU.subtract, ALU.max)
                    self.ts(T["M2"], psg[:, 0:128], I["Gp"], 0.0, ALU.subtract, ALU.min)
                    self.act(T["ER"], psg[:, 0:128], AF.Exp)
                for I in insts:
                    T = I["T"]
                    self.act(T["M1"], T["M1"], AF.Exp, scale=-1.0)
                    self.act(T["M2"], T["M2"], AF.Exp)
                for I in insts:
                    T, d = I["T"], I["d"]
                    self.tt(T["M1"], T["M1"], mSL if d == 0 else mSU, ALU.mult, eng="gpsimd")
                    self.tt(T["E2i"], T["M2"], mIU if d == 0 else mIL, ALU.mult, eng="gpsimd")
                for I in insts:
                    cs = I["cs"]
                    I["psk"] = self.bank()
                    self.mm(I["psk"][:, 0:128], kT[:, cs], kT[:, cs])
                    self.mm(I["psk"][:, 128:256], kT[:, cs], qT[:, cs])
                for I in insts:
                    T, d, n = I["T"], I["d"], I["n"]
                    I["Nm"], I["Mm"], I["Xm"] = I["R"]["N"].next(), I["R"]["M"].next(), I["R"]["X"].next()
                    self.stt(I["Nm"], I["psk"][:, 0:128], NBT[:, d, n, h:h + 1], T["M1"], ALU.mult, ALU.mult)
                    self.tt(I["O"]["ACT"], I["psk"][:, 128:256], T["E2i"], ALU.mult)
                for I in insts:
                    I["pst"] = self.bank()
                    self.mm(I["pst"][:, 0:128], I["Nm"], ident_f)
                for I in insts:
                    self.cp(I["Mm"], I["pst"][:, 0:128], eng="scalar")
                    self.tt(I["Xm"], I["Mm"], ident_f, ALU.add, eng="gpsimd")
                    I["Pm"], I["PTm"] = I["Mm"], I["Nm"]
                for lvl in range(6):
                    last = lvl == 5
                    for I in insts:
                        I["psl"] = self.bank()
                        if not last:
                            self.mm(I["psl"][:, 0:128], I["PTm"], I["Pm"])
                        self.mm(I["psl"][:, 128:256], I["Pm"], I["PTm"])
                    for I in insts:
                        I["PT2"] = I["R"]["N"].next()
                        self.cp(I["PT2"], I["psl"][:, 128:256], eng="scalar")
                        if not last:
                            I["P2"] = I["R"]["M"].next()
                            self.cp(I["P2"], I["psl"][:, 0:128], eng="vector")
                    for I in insts:
                        I["psx"] = self.bank()
                        self.mm(I["psx"][:, 0:128], I["PT2"], I["Xm"])
                    for I in insts:
                        X2 = I["R"]["X"].next()
                        self.tt(X2, I["psx"][:, 0:128], I["Xm"], ALU.add)
                        I["Xm"] = X2
                        I["PTm"] = I["PT2"]
                        if not last:
                            I["Pm"] = I["P2"]
                for I in insts:
                    I["psw"] = self.bank()
                    self.mm(I["psw"][:, 0:128], I["T"]["KB"], I["Xm"])
                    self.mm(I["psw"][:, 128:256], I["Xm"], I["T"]["VB"])
                for I in insts:
                    self.cp(I["O"]["WCT"], I["psw"][:, 0:128], eng="scalar")
                    self.cp(I["O"]["UC"], I["psw"][:, 128:256], eng="vector")
                    self.tt(I["O"]["QG"], qT[:, I["cs"]], I["T"]["ER"], ALU.mult, eng="gpsimd")

            def state(insts):
                for I in insts:
                    d, n, cs, O = I["d"], I["n"], I["cs"], I["O"]
                    Sst = gdn_state[:, h, d, :]
                    pss = self.bank()
                    self.mm(pss[:, 0:128], O["WCT"], Sst)
                    self.tt(O["VN"], O["UC"], pss[:, 0:128], ALU.subtract)
                    pso = self.bank()
                    self.mm(pso[:, 0:128], Sst, O["QG"], start=True, stop=False)
                    self.mm(pso[:, 0:128], O["VN"], O["ACT"], start=False, stop=True)
                    psu = self.bank()
                    self.mm(psu[:, 0:128], O["KG"], O["VN"])
                    OTn = V(OT.ap[:, cs], [kk + (n,) for kk in OT.keys])
                    self.tt(OTn, OTn, pso[:, 0:128], ALU.add)
                    self.stt(Sst, Sst, GL[:, d, n, h:h + 1], psu[:, 0:128], ALU.mult, ALU.add)

            groups = [group_insts(p) for p in range(ngroups)]
            pre(groups[0])
            for p in range(ngroups):
                if p + 1 < ngroups:
                    pre(groups[p + 1])
                state(groups[p])
            if lat and KG > 6:
                c = 40 + h
                self.dma(CV, V(pq.ap[c - 16][:, 0:Lx], [kk + (c,) for kk in pq.keys]))
                for t in range(Lx // TL):
                    sl = slice(t * TL, (t + 1) * TL)
                    sq, rt = SQr.next(), RTr.next()
                    self.tt(sq, OTa[:, sl], OTa[:, sl], ALU.mult, eng="gpsimd")
                    ps = self.bank()
                    self.mm(ps[:, 0:TL], ones_b, sq)
                    self.act(rt, ps[:, 0:TL], AF.Sqrt, bias=LN_EPS, scale=1.0 / 128.0)
                    self.recip(rt, rt)
                    self.tt(CV[:, sl], CV[:, sl], rt, ALU.mult, eng="gpsimd")
                self.stt(YB.re("p (r c) -> p c r", c=GW), OTa.re("p (c r) -> p c r", c=GW), gdnnw[:, 0:1],
                         CV.re("p (c r) -> p c r", c=GW), ALU.mult, ALU.mult)
                self.dma(E["ybT_d"][b, h].k(b, h), YB)

    def merge(self, b, E):
        A, P = self.A, self.P
        NB, L, CAP = self.NB, self.L, self.CAP
        ident_f, ones_b, mSU_b = E["ident_f"], E["ones_b"], E["mSU_b"]
        DEST, WT, CNT, ECAP, brt = E["DEST"], E["WT"], E["CNT"], E["ECAP"], E["brt"]
        wpa = A.alloc("wpa", (8, D), BF16)
        wpb = A.alloc("wpb", (8, D), BF16)
        wout = A.alloc("wout", (8, D), BF16)
        for (dst, src) in ((wpa, E["wpa_d"]), (wpb, E["wpb_d"]), (wout, E["wout_d"])):
            sv = src.re("(k p) n -> p k n", p=128)
            for k in range(8):
                self.dma(dst[:, k, :].k(k), sv[:, k, :], eng="gpsimd")
        wrt = A.alloc("wrt", (8, 36))
        self.dma(wrt, E["wrt_d"])
        g1bc = A.alloc("g1bc", D)
        sh2bc = A.alloc("sh2bc", D)
        sc2bc = A.alloc("sc2bc", D)
        ln1g = A.alloc("ln1g", D)
        ln1b = A.alloc("ln1b", D)
        bcs = E["bcs_d"]
        bk = [kk + (b, ct) for kk in bcs.keys for ct in range(8)]
        self.dma(g1bc, V(bcs.ap[b, 0], bk))
        self.dma(sh2bc, V(bcs.ap[b, 1], bk))
        self.dma(sc2bc, V(bcs.ap[b, 2], bk))
        self.dma(ln1g, E["lnbc_d"][:, 0, :])
        self.dma(ln1b, E["lnbc_d"][:, 1, :])
        YAt = Ring([A.alloc("YAt%d" % i, (8, 512), BF16) for i in range(1)])
        YBt = Ring([A.alloc("YBt%d" % i, (8, 512), BF16) for i in range(1)])
        SGa = Ring([A.alloc("SGa%d" % i, 512) for i in range(3)])
        SGb = Ring([A.alloc("SGb%d" % i, 512) for i in range(3)])
        M1r = Ring([A.alloc("M1r%d" % i, 512) for i in range(2)])
        M2r = Ring([A.alloc("M2r%d" % i, 512) for i in range(2)])
        MTr = Ring([A.alloc("MT%d" % i, (8, 512), BF16) for i in range(1)])
        T1r = Ring([A.alloc("T1%d" % i, D) for i in range(2)])
        Xtr = Ring([A.alloc("Xt%d" % i, D) for i in range(2)])
        X1r = Ring([A.alloc("X1%d" % i, D) for i in range(2)])
        H2r = Ring([A.alloc("H2%d" % i, D) for i in range(2)])
        H2br = Ring([A.alloc("H2b%d" % i, D, BF16) for i in range(3)])
        H2Tr = Ring([A.alloc("H2T%d" % i, (8, 128)) for i in range(2)])
        sm = Ring([A.alloc("rsm%d" % i, 256) for i in range(3)])
        yav = E["yaT_d"]
        ybv = E["ybT_d"]
        sgv = E["sg_d"]
        TLm = 512
        for g in range(L // TLm):
            ts_ = slice(g * TLm, (g + 1) * TLm)
            ya, yb = YAt.next(), YBt.next()
            for k in range(8):
                self.dma(ya[:, k, :].k(k), V(yav.ap[b, k][:, ts_], [kk + (b, k) for kk in yav.keys]))
                self.dma(yb[:, k, :].k(k), V(ybv.ap[b, k][:, ts_], [kk + (b, k) for kk in ybv.keys]))
            MT = MTr.next()
            for dc in range(8):
                sga, sgb = SGa.next(), SGb.next()
                self.dma(sga, V(sgv.ap[b, dc][:, ts_], [kk + (b, dc) for kk in sgv.keys]))
                self.dma(sgb, V(sgv.ap[b, 8 + dc][:, ts_], [kk + (b, 8 + dc) for kk in sgv.keys]))
                psa = self.bank()
                for k in range(8):
                    self.mm(psa, wpa[:, k, dc * 128:(dc + 1) * 128].k(k), ya[:, k, :].k(k), start=(k == 0), stop=(k == 7))
                psb_ = self.bank()
                for k in range(8):
                    self.mm(psb_, wpb[:, k, dc * 128:(dc + 1) * 128].k(k), yb[:, k, :].k(k), start=(k == 0), stop=(k == 7))
                m1, m2 = M1r.next(), M2r.next()
                self.tt(m1, psa, sga, ALU.mult)
                self.tt(m2, psb_, sgb, ALU.mult)
                self.tt(MT[:, dc, :].k(dc), m1, m2, ALU.add, eng="gpsimd")
            MTa = V(MT.ap, [kk + (dc,) for kk in MT.keys for dc in range(8)])
            for sub in range(4):
                tile = (b * L + g * TLm) // 128 + sub
                tok0 = g * TLm + sub * 128
                T1 = T1r.next()
                for half in range(2):
                    ps = self.bank()
                    for dc in range(8):
                        self.mm(ps, MTa[:, dc, sub * 128:(sub + 1) * 128], wout[:, dc, half * 512:(half + 1) * 512].k(dc),
                                start=(dc == 0), stop=(dc == 7))
                    self.tt(T1[:, half * 512:(half + 1) * 512].k(half), ps, g1bc[:, half * 512:(half + 1) * 512], ALU.mult)
                T1a = V(T1.ap, [kk + (hh,) for kk in T1.keys for hh in range(2)])
                xt = Xtr.next()
                self.dma(xt, E["x_d"][b][tok0:tok0 + 128, :])
                self.stt(T1a, xt, ALPHA, T1a, ALU.mult, ALU.add)
                mean, rstd = self.ln_stats(T1a)
                self.ts(T1a, T1a, mean, rstd, ALU.subtract, ALU.mult)
                X1 = X1r.next()
                self.tt(X1, T1a, ln1g, ALU.mult, eng="gpsimd")
                self.tt(X1, X1, ln1b, ALU.add, eng="gpsimd")
                self.dma(V(E["x1_d"].ap[tile * 128:(tile + 1) * 128, :], [kk + (tile,) for kk in E["x1_d"].keys]), X1)
                self.route(tile, X1, sh2bc, sc2bc, wrt, H2r, H2br, H2Tr, sm, E)

    def route(self, tile, X1, sh2bc, sc2bc, wrt, H2r, H2br, H2Tr, sm, E):
        CAP = self.CAP
        ident_f, ones_b, mSU_b = E["ident_f"], E["ones_b"], E["mSU_b"]
        DEST, WT, CNT, ECAP, brt = E["DEST"], E["WT"], E["CNT"], E["ECAP"], E["brt"]
        mean, rstd = self.ln_stats(X1)
        H2 = H2r.next()
        self.ts(H2, X1, mean, rstd, ALU.subtract, ALU.mult)
        self.tt(H2, H2, sc2bc, ALU.mult, eng="gpsimd")
        self.tt(H2, H2, sh2bc, ALU.add, eng="gpsimd")
        H2b = H2br.next()
        self.cp(H2b, H2, eng="scalar")
        H2T = H2Tr.next()
        for half in range(2):
            ps = self.bank()
            for kk in range(4):
                k = half * 4 + kk
                self.tr(ps[:, kk * 128:(kk + 1) * 128], H2[:, k * 128:(k + 1) * 128], ident_f)
            self.evac(H2T[:, half * 4:(half + 1) * 4, :].k(half), ps.re("p (a b) -> p a b", b=128))
        H2Ta = V(H2T.ap, [kk + (hh,) for kk in H2T.keys for hh in range(2)])
        psr = self.bank()
        for k in range(8):
            self.mm(psr[:, 0:36], H2Ta[:, k, :], wrt[:, k, :], start=(k == 0), stop=(k == 7))
        s = sm.next()
        LG = s[:, 0:36]
        gmax, ngmax, gsum, pg = s[:, 36:37], s[:, 37:38], s[:, 38:39], s[:, 39:40]
        GE, OHG = s[:, 40:44], s[:, 44:48]
        ML = s[:, 48:80]
        m8 = s[:, 80:88]
        d21, e21, den, w1, w2 = s[:, 88:89], s[:, 89:90], s[:, 90:91], s[:, 91:92], s[:, 92:93]
        OH1, OH2 = s[:, 96:128], s[:, 128:160]
        SL = s[:, 160:192]
        TMP = s[:, 192:224]
        d1f, d2f = s[:, 224:225], s[:, 225:226]
        Ab = V(s.ap[:, 232:248].bitcast(BF16), s.keys)
        self.tt(LG, psr[:, 0:36], brt, ALU.add)
        rmx = lambda o, i: self.P.op("vector", (lambda oo, ii: lambda e: e.reduce_max(out=oo, in_=ii, axis=AX.X))(o.ap, i.ap),
                                     reads=_keys(i), writes=_keys(o))
        rsm = lambda o, i: self.P.op("vector", (lambda oo, ii: lambda e: e.reduce_sum(out=oo, in_=ii, axis=AX.X))(o.ap, i.ap),
                                     reads=_keys(i), writes=_keys(o))
        rmx(gmax, LG[:, 0:4])
        self.ts(ngmax, gmax, -1.0, None, ALU.mult)
        self.act(GE, LG[:, 0:4], AF.Exp, bias=ngmax)
        rsm(gsum, GE)
        self.recip(pg, gsum)
        self.ts(OHG, LG[:, 0:4], gmax, None, ALU.is_equal)
        self.ts(OHG, OHG, 1.0, 1e9, ALU.subtract, ALU.mult)
        self.tt(ML.re("p (g e) -> p g e", e=8), LG[:, 4:36].re("p (g e) -> p g e", e=8),
                V(OHG.ap.unsqueeze(2).to_broadcast([128, 4, 8]), OHG.keys), ALU.add)
        ml, m8a = ML.ap, m8.ap
        self.P.op("vector", lambda e: e.max(out=m8a, in_=ml), reads=_keys(ML), writes=_keys(m8))
        self.tt(d21, m8[:, 1:2], m8[:, 0:1], ALU.subtract)
        self.act(e21, d21, AF.Exp)
        self.ts(den, e21, 1.0, None, ALU.add)
        self.recip(den, den)
        self.tt(w1, pg, den, ALU.mult)
        self.tt(w2, w1, e21, ALU.mult)
        self.cp(WT[:, tile, 0:1].k(tile), w1)
        self.cp(WT[:, tile, 1:2].k(tile), w2)
        self.ts(OH1, ML, m8[:, 0:1], None, ALU.is_equal)
        self.ts(OH2, ML, m8[:, 1:2], None, ALU.is_equal)
        self.tt(Ab, OH1, OH2, ALU.add)
        psk = self.bank()
        self.mm(psk[:, 0:32], mSU_b, Ab)
        self.mm(psk[:, 32:64], ones_b, Ab)
        self.tt(SL, psk[:, 0:32], CNT, ALU.add)
        self.stt(SL, SL, float(CAP - 1), ECAP, ALU.min, ALU.add)
        self.tt(CNT, CNT, psk[:, 32:64], ALU.add)
        self.tt(TMP, OH1, SL, ALU.mult)
        rsm(d1f, TMP)
        self.tt(TMP, OH2, SL, ALU.mult)
        rsm(d2f, TMP)
        self.cp(DEST[:, tile, 0:1].k(tile), d1f)
        self.cp(DEST[:, tile, 1:2].k(tile), d2f)
        xg = E["xg_d"]
        self.scatter(V(xg.ap, [kk + (tile, 0) for kk in xg.keys]), DEST[:, tile, 0:1].k(tile), H2b)
        self.scatter(V(xg.ap, [kk + (tile, 1) for kk in xg.keys]), DEST[:, tile, 1:2].k(tile), H2b)

    def experts(self, E):
        A = self.A
        CAP, NB, L = self.CAP, self.NB, self.L
        NT = NB * L // 128
        ident_b = E["ident_b"]
        xg, yg = E["xg_d"], E["yg_d"]
        xg_all = V(xg.ap, [kk + (t, j) for kk in xg.keys for t in range(NT) for j in range(2)])
        nst = CAP // 128
        Wg = Ring([A.alloc("Wg%d" % i, (8, D), BF16) for i in range(2)])
        Wu = Ring([A.alloc("Wu%d" % i, (8, D), BF16) for i in range(2)])
        Wd = Ring([A.alloc("Wd%d" % i, (8, D), BF16) for i in range(2)])
        Xr = Ring([A.alloc("Xr%d" % i, D, BF16) for i in range(3)])
        XT = Ring([A.alloc("XTe%d" % i, (8, CAP), BF16) for i in range(2)])
        AT = Ring([A.alloc("ATe%d" % i, (8, CAP), BF16) for i in range(2)])
        SGr = Ring([A.alloc("SGe%d" % i, 512) for i in range(3)])
        Yr = Ring([A.alloc("Ye%d" % i, D) for i in range(3)])
        segs = []
        o = 0
        while o < CAP:
            n = min(512, CAP - o)
            segs.append((o, n))
            o += n
        for e in range(NEXP):
            wg, wu, wd = Wg.next(), Wu.next(), Wd.next()
            for (dst, src) in ((wg, E["weg_d"]), (wu, E["weu_d"]), (wd, E["wed_d"])):
                sv = src[e].re("(k p) n -> p k n", p=128)
                for k in range(8):
                    self.dma(dst[:, k, :].k(k), sv[:, k, :], eng="gpsimd")
            xT = XT.next()
            for s_ in range(nst):
                xr = Xr.next()
                self.dma(xr, V(xg_all.ap[e * CAP + s_ * 128:e * CAP + (s_ + 1) * 128, :], xg_all.keys))
                for half in range(2):
                    ps = self.bank()
                    for kk in range(4):
                        k = half * 4 + kk
                        self.mm(ps[:, kk * 128:(kk + 1) * 128], xr[:, k * 128:(k + 1) * 128], ident_b)
                    self.evac(xT[:, half * 4:(half + 1) * 4, s_ * 128:(s_ + 1) * 128].k(half, s_),
                              ps.re("p (a b) -> p a b", b=128))
            xTa = V(xT.ap, [kk + (hh, ss) for kk in xT.keys for hh in range(2) for ss in range(nst)])
            aT = AT.next()
            for j in range(8):
                for (o, n) in segs:
                    psg = self.bank()
                    for k in range(8):
                        self.mm(psg[:, 0:n], wg[:, k, j * 128:(j + 1) * 128].k(k), xTa[:, k, o:o + n], start=(k == 0), stop=(k == 7))
                    psu = self.bank()
                    for k in range(8):
                        self.mm(psu[:, 0:n], wu[:, k, j * 128:(j + 1) * 128].k(k), xTa[:, k, o:o + n], start=(k == 0), stop=(k == 7))
                    sg_ = SGr.next()
                    self.act(sg_[:, 0:n], psg[:, 0:n], AF.Silu)
                    self.tt(aT[:, j, o:o + n].k(j, o), sg_[:, 0:n], psu[:, 0:n], ALU.mult)
            aTa = V(aT.ap, [kk + (j, o) for kk in aT.keys for j in range(8) for (o, n) in segs])
            for s_ in range(nst):
                y = Yr.next()
                for half in range(2):
                    ps = self.bank()
                    for j in range(8):
                        self.mm(ps, aTa[:, j, s_ * 128:(s_ + 1) * 128], wd[:, j, half * 512:(half + 1) * 512].k(j),
                                start=(j == 0), stop=(j == 7))
                    self.evac(y[:, half * 512:(half + 1) * 512].k(half), ps)
                ya = V(y.ap, [kk + (hh,) for kk in y.keys for hh in range(2)])
                r0 = e * CAP + s_ * 128
                self.dma(V(yg.ap[r0:r0 + 128, :], [kk + (e, s_) for kk in yg.keys]), ya)

    def combine(self, E):
        A = self.A
        CAP, NB, L = self.CAP, self.NB, self.L
        NT = NB * L // 128
        nst = CAP // 128
        DEST, WT = E["DEST"], E["WT"]
        yg, x1 = E["yg_d"], E["x1_d"]
        yg_all = V(yg.ap, [kk + (e, s_) for kk in yg.keys for e in range(NEXP) for s_ in range(nst)])
        ln2g = A.alloc("ln2g", D)
        ln2b = A.alloc("ln2b", D)
        self.dma(ln2g, E["lnbc_d"][:, 2, :])
        self.dma(ln2b, E["lnbc_d"][:, 3, :])
        g2bc = [A.alloc("g2bc%d" % b, D) for b in range(NB)]
        bcs = E["bcs_d"]
        for b in range(NB):
            self.dma(g2bc[b], V(bcs.ap[b, 3], [kk + (b, ct) for kk in bcs.keys for ct in range(8)]))
        Y1r = Ring([A.alloc("Y1g%d" % i, D) for i in range(2)])
        Y2r = Ring([A.alloc("Y2g%d" % i, D) for i in range(2)])
        X1r = Ring([A.alloc("X1c%d" % i, D) for i in range(2)])
        Or = Ring([A.alloc("Oc%d" % i, D) for i in range(2)])
        for tile in range(NT):
            b = tile * 128 // L
            Y1, Y2, X1, O = Y1r.next(), Y2r.next(), X1r.next(), Or.next()
            self.gather(Y1, yg_all, DEST[:, tile, 0:1].k(tile))
            self.gather(Y2, yg_all, DEST[:, tile, 1:2].k(tile))
            self.dma(X1, V(x1.ap[tile * 128:(tile + 1) * 128, :], [kk + (tile,) for kk in x1.keys]))
            self.ts(Y1, Y1, WT[:, tile, 0:1].k(tile), None, ALU.mult, eng="gpsimd")
            self.stt(Y1, Y2, WT[:, tile, 1:2].k(tile), Y1, ALU.mult, ALU.add)
            self.tt(Y1, Y1, g2bc[b], ALU.mult, eng="gpsimd")
            self.stt(Y1, X1, ALPHA, Y1, ALU.mult, ALU.add)
            mean, rstd = self.ln_stats(Y1)
            self.ts(Y1, Y1, mean, rstd, ALU.subtract, ALU.mult)
            self.tt(O, Y1, ln2g, ALU.mult, eng="gpsimd")
            self.tt(O, O, ln2b, ALU.add)
            self.dma(V(E["out_d"].ap[tile * 128:(tile + 1) * 128, :], [("out", tile)]), O, is_output=True)


def chunk_starts():
    st = [n * 128 for n in range(8)]
    st += [1024 + n * 128 for n in range(8)]
    st += [2048 + n * 128 for n in range(24)]
    st += [5152 + n * 128 for n in range(8)]
    st += [6176 + n * 128 for n in range(16)]
    return st


def host_layout(inp, NB, core, NBP):
    f = lambda a: np.ascontiguousarray(a, dtype=np.float32)
    w_in = inp["w_in"][0]
    b_in = inp["b_in"][0]
    st = chunk_starts()
    w_in_t = np.stack([w_in[:, s:s + 128].reshape(8, 128, 128).transpose(1, 0, 2) for s in st], 0)
    w_in_lg = w_in[:, 5120:5152].reshape(8, 128, 32).transpose(1, 0, 2)
    b_inT = np.zeros((128, 65), np.float32)
    for i, s in enumerate(st):
        b_inT[:, i] = b_in[s:s + 128]
    b_inT[0:32, 64] = b_in[5120:5152]
    bs = slice(core * NB, (core + 1) * NB)
    c = inp["c"][bs]
    cT = np.zeros((128, 8, NBP), np.float32)
    for b in range(NB):
        cT[:, :, b] = c[b].reshape(8, 128).T
    cT[:, :, NB] = inp["c_ctx"].reshape(8, 128).T
    b_mod = inp["b_mod"][0]
    fm = lambda v: v.reshape(-1, 128).T
    rep = lambda v: np.broadcast_to(v[None, :], (128, v.shape[0]))
    lw = np.stack([inp["lru_wa"][0, 0], inp["lru_wx"][0, 0], inp["lru_wa"][0, 1], inp["lru_wx"][0, 1]], 0)
    lb = np.stack([inp["lru_ba"][0, 0], inp["lru_bx"][0, 0], inp["lru_ba"][0, 1], inp["lru_bx"][0, 1]], 0)
    conv_a = np.concatenate([inp["conv_a_w"][0], inp["conv_a_b"][0][None]], 0)
    d = {
        "x": inp["x"][bs], "ctx": inp["ctx"][bs], "cT": cT,
        "w_mod": inp["w_mod"][0], "bmodT": fm(b_mod[0:2048]), "bmod_bc": rep(b_mod[2048:]),
        "w_in_t": w_in_t, "w_in_lg": w_in_lg, "b_inT": b_inT,
        "conv_a": conv_a.reshape(5, 8, 128).transpose(2, 1, 0),
        "lru_w": lw.transpose(2, 0, 1, 3),
        "lru_b": lb.reshape(4, 8, 128).transpose(2, 0, 1),
        "lru_lam": inp["lru_lambda"][0].reshape(2, 8, 128).transpose(2, 0, 1),
        "conv_qkv": inp["conv_qkv_w"][0].reshape(4, 24, 128).transpose(2, 1, 0),
        "gdn_rows": rep(np.concatenate([inp["gdn_dt_bias"][0].reshape(16), inp["gdn_a_log"][0].reshape(16)])),
        "gdn_nw": inp["gdn_norm_w"][0].reshape(128, 1),
        "w_pa": inp["w_pa"][0], "w_pb": inp["w_pb"][0], "w_out": inp["w_out"][0],
        "ln_bc": np.stack([rep(inp["ln1_g"][0]), rep(inp["ln1_b"][0]), rep(inp["ln2_g"][0]), rep(inp["ln2_b"][0])], 1),
        "w_rt": np.concatenate([inp["w_router_g"][0], inp["w_router_e"][0]], 1).reshape(8, 128, 36).transpose(1, 0, 2),
        "b_rt_bc": rep(np.concatenate([inp["b_router_g"][0], inp["b_router_e"][0]])),
        "w_eg": inp["w_e_gate"][0], "w_eu": inp["w_e_up"][0], "w_ed": inp["w_e_down"][0],
    }
    return {k: f(v) for k, v in d.items()}


_CACHE = {}


def run(inputs, n_cores, NB, L, CTX, CAP, debug=()):
    key = (NB, L, CTX, CAP, tuple(debug))
    if key not in _CACHE:
        _CACHE[key] = KB(NB, L, CTX, CAP, debug).build()
    nc = _CACHE[key]
    NBP = NB + 1 + ((NB + 1) % 2)
    inp = {k: np.asarray(v) for k, v in inputs.items()}
    shared = None
    in_maps = []
    for core in range(n_cores):
        m = host_layout(inp, NB, core, NBP) if shared is None else None
        if shared is None:
            shared = m
        else:
            m = dict(shared)
            bs = slice(core * NB, (core + 1) * NB)
            m["x"] = np.ascontiguousarray(inp["x"][bs], dtype=np.float32)
            m["ctx"] = np.ascontiguousarray(inp["ctx"][bs], dtype=np.float32)
            c = inp["c"][bs]
            cT = shared["cT"].copy()
            for b in range(NB):
                cT[:, :, b] = c[b].reshape(8, 128).T
            m["cT"] = cT
        in_maps.append(m)
    res = run_bass_kernel_spmd(nc, in_maps, core_ids=list(range(n_cores)))
    return res


def kernel(**inputs):
    n_cores, NB, L, CTX, CAP = 8, 2, 4096, 256, 640
    res = run(inputs, n_cores, NB, L, CTX, CAP)
    outs = [r["out"].reshape(NB, L, D) for r in res.results]
    return np.concatenate(outs, 0).astype(np.float32)
```

```python
import math
from contextlib import ExitStack
import numpy as np
import concourse.bass as bass
import concourse.mybir as mybir
from concourse.bass_utils import run_bass_kernel_spmd

F32 = mybir.dt.float32
F32R = mybir.dt.float32r
BF16 = mybir.dt.bfloat16
I32 = mybir.dt.int32
AF = mybir.ActivationFunctionType
ALU = mybir.AluOpType
AX = mybir.AxisListType

ENGS = ("sync", "scalar", "gpsimd", "vector", "tensor")
NDMA_SEM = 16
SAME_ENGINE_SYNC = True

D = 1024
GW = 64
ALPHA = 2.0 ** 0.25
LN_EPS = 1e-6
NEXP = 32


class Prog:
    def __init__(self, nc, stack):
        self.nc = nc
        self.q = {e: [] for e in ENGS}
        self.cnt = {e: 0 for e in ENGS}
        self.esem = {e: stack.enter_context(nc.semaphore("es_" + e)) for e in ENGS}
        self.dsem, self.dcnt, self.dnext = {}, {}, {}
        for e in ("sync", "gpsimd"):
            self.dsem[e] = [stack.enter_context(nc.semaphore("ds_%s%d" % (e, i))) for i in range(NDMA_SEM)]
            self.dcnt[e] = [0] * NDMA_SEM
            self.dnext[e] = 0
        self.semobj = {}
        for e in ENGS:
            self.semobj[("e", e)] = self.esem[e]
        for e in self.dsem:
            for i, s in enumerate(self.dsem[e]):
                self.semobj[("d", e, i)] = s
        self.seen = {e: {} for e in ENGS}
        self.last_w = {}
        self.readers = {}
        self.out_tokens = []

    def _deps(self, eng, reads, writes):
        toks = []
        for k in reads:
            t = self.last_w.get(k)
            if t is not None:
                toks.append(t)
        for k in writes:
            t = self.last_w.get(k)
            if t is not None:
                toks.append(t)
            toks.extend(self.readers.get(k, ()))
        need = {}
        for (sk, v) in toks:
            if sk == ("e", eng) and (eng == "tensor" or not SAME_ENGINE_SYNC):
                continue
            if self.seen[eng].get(sk, 0) >= v:
                continue
            if need.get(sk, 0) < v:
                need[sk] = v
        for sk, v in need.items():
            self.seen[eng][sk] = v
        return list(need.items())

    def _commit(self, tok, reads, writes):
        for k in reads:
            if k in writes:
                continue
            self.readers.setdefault(k, []).append(tok)
        for k in writes:
            self.last_w[k] = tok
            self.readers[k] = []

    def op(self, eng, fn, reads=(), writes=()):
        psr = [k for k in reads if k[0] == "psb"]
        if psr:
            writes = list(writes) + psr
        waits = self._deps(eng, reads, writes)
        self.cnt[eng] += 1
        tok = (("e", eng), self.cnt[eng])
        self.q[eng].append((waits, fn, self.esem[eng], 1))
        self._commit(tok, reads, writes)
        return tok

    def dma(self, eng, fn, reads=(), writes=(), is_output=False):
        i = self.dnext[eng]
        self.dnext[eng] = (i + 1) % NDMA_SEM
        sk = ("d", eng, i)
        waits = self._deps(eng, reads, writes)
        prev = self.dcnt[eng][i]
        if prev > 0 and self.seen[eng].get(sk, 0) < prev:
            self.seen[eng][sk] = prev
            waits.append((sk, prev))
        self.dcnt[eng][i] += 16
        tok = (sk, self.dcnt[eng][i])
        self.q[eng].append((waits, fn, self.dsem[eng][i], 16))
        self._commit(tok, reads, writes)
        if is_output:
            self.out_tokens.append(tok)
        return tok

    def barrier(self):
        toks = [(("e", e), self.cnt[e]) for e in ENGS if self.cnt[e] > 0]
        for e in self.dsem:
            for i in range(NDMA_SEM):
                if self.dcnt[e][i] > 0:
                    toks.append((("d", e, i), self.dcnt[e][i]))
        for eng in ENGS:
            waits = []
            for (sk, v) in toks:
                if sk == ("e", eng) and eng == "tensor":
                    continue
                if self.seen[eng].get(sk, 0) >= v:
                    continue
                self.seen[eng][sk] = v
                waits.append((sk, v))
            if waits:
                self.q[eng].append((waits, None, None, 0))
        self.last_w = {}
        self.readers = {}

    def finish(self):
        self.q["sync"].append((list(self.out_tokens), None, None, 0))

    def emit(self, block):
        def run(engname):
            def body(eng):
                for (waits, fn, sem, inc) in self.q[engname]:
                    for (sk, v) in waits:
                        eng.wait_ge(self.semobj[sk], v)
                    if fn is not None:
                        fn(eng).then_inc(sem, inc)
            return body
        block.sync(run("sync"))
        block.scalar(run("scalar"))
        block.gpsimd(run("gpsimd"))
        block.vector(run("vector"))
        block.tensor(run("tensor"))


class V:
    __slots__ = ("ap", "keys")

    def __init__(self, ap, keys):
        self.ap = ap
        self.keys = tuple(keys)

    def __getitem__(self, idx):
        return V(self.ap[idx], self.keys)

    def re(self, pat, **kw):
        return V(self.ap.rearrange(pat, **kw), self.keys)

    def bc(self, shape):
        return V(self.ap.to_broadcast(list(shape)), self.keys)

    def k(self, *sub):
        return V(self.ap, [kk + tuple(sub) for kk in self.keys])

    def cast(self, dt):
        return V(self.ap.bitcast(dt), self.keys)


def _keys(*vs):
    out = []
    for v in vs:
        if isinstance(v, V):
            out.extend(v.keys)
    return out


def _a(v):
    return v.ap if isinstance(v, V) else v


DT_SIZE = {F32: 4, BF16: 2, I32: 4}


class Arena:
    def __init__(self, nc, stack, nbytes):
        self.words = nbytes // 4
        self.t = stack.enter_context(nc.sbuf_tensor("arena", [128, self.words], F32))
        self.off = 0
        self.uid = 0

    def alloc(self, name, free_shape, dt=F32):
        if isinstance(free_shape, int):
            free_shape = (free_shape,)
        nel = 1
        for s in free_shape:
            nel *= s
        words = (nel * DT_SIZE[dt] + 31) // 32 * 8
        assert self.off + words <= self.words, "SBUF arena overflow at %s (%d + %d > %d)" % (name, self.off, words, self.words)
        ap = self.t[:, self.off:self.off + words]
        if dt != F32:
            ap = ap.bitcast(dt)
        ap = ap[:, 0:nel]
        if len(free_shape) == 2:
            ap = ap.rearrange("p (a b) -> p a b", b=free_shape[1])
        elif len(free_shape) == 3:
            ap = ap.rearrange("p (a b c) -> p a b c", b=free_shape[1], c=free_shape[2])
        elif len(free_shape) == 4:
            ap = ap.rearrange("p (a b c d) -> p a b c d", b=free_shape[1], c=free_shape[2], d=free_shape[3])
        self.off += words
        self.uid += 1
        return V(ap, [(name, self.uid)])

    def mark(self):
        return self.off

    def reset(self, m):
        self.off = m


class Ring:
    def __init__(self, items):
        self.items = items
        self.i = 0

    def next(self):
        v = self.items[self.i % len(self.items)]
        self.i += 1
        return v


class KB:
    def __init__(self, NB, L, CTX, CAP, debug=()):
        self.NB, self.L, self.CTX, self.CAP = NB, L, CTX, CAP
        self.R = L // GW
        self.debug = set(debug)
        self.nc = bass.Bass("TRN2", target_bir_lowering=False)
        self.alt = 0

    def din(self, name, shape, dt=F32):
        return V(self.nc.dram_tensor(name, list(shape), dt, kind="ExternalInput").ap(), [])

    def dscr(self, name, shape, dt):
        kind = "ExternalOutput" if name in self.debug else "Internal"
        return V(self.nc.dram_tensor(name, list(shape), dt, kind=kind).ap(), [(name,)])

    def mm(self, out, lhsT, rhs, start=True, stop=True):
        o, l, r = out.ap, lhsT.ap, rhs.ap
        self.P.op("tensor", lambda e: e.matmul(o, lhsT=l, rhs=r, start=start, stop=stop),
                  reads=_keys(lhsT, rhs), writes=_keys(out))

    def mmr(self, out, lhsT, rhs, start=True, stop=True):
        o, l, r = out.ap, lhsT.ap, rhs.ap
        self.P.op("tensor", lambda e: e.matmul(o, lhsT=l, rhs=r, start=start, stop=stop),
                  reads=_keys(lhsT, rhs), writes=_keys(out))

    def tr(self, out, in_, ident):
        o, i, d = out.ap, in_.ap, ident.ap
        self.P.op("tensor", lambda e: e.transpose(o, i, d), reads=_keys(in_, ident), writes=_keys(out))

    def act(self, out, in_, func, bias=0.0, scale=1.0, eng="scalar"):
        o, i, b, s = out.ap, in_.ap, _a(bias), _a(scale)
        self.P.op("scalar", lambda e: e.activation(out=o, in_=i, func=func, bias=b, scale=s),
                  reads=_keys(in_, bias, scale), writes=_keys(out))

    def ts(self, out, in0, s1, s2, op0, op1=None, eng="vector"):
        o, i, a, b = out.ap, in0.ap, _a(s1), _a(s2)
        if op1 is None:
            fn = lambda e: e.tensor_scalar(out=o, in0=i, scalar1=a, scalar2=None, op0=op0)
        else:
            fn = lambda e: e.tensor_scalar(out=o, in0=i, scalar1=a, scalar2=b, op0=op0, op1=op1)
        self.P.op(eng, fn, reads=_keys(in0, s1, s2), writes=_keys(out))

    def tt(self, out, in0, in1, op, eng="vector"):
        o, a, b = out.ap, in0.ap, in1.ap
        self.P.op(eng, lambda e: e.tensor_tensor(out=o, in0=a, in1=b, op=op), reads=_keys(in0, in1), writes=_keys(out))

    def stt(self, out, in0, scalar, in1, op0, op1):
        o, a, s, b = out.ap, in0.ap, _a(scalar), in1.ap
        self.P.op("vector", lambda e: e.scalar_tensor_tensor(out=o, in0=a, scalar=s, in1=b, op0=op0, op1=op1),
                  reads=_keys(in0, scalar, in1), writes=_keys(out))

    def cp(self, out, in_, eng="vector"):
        o, i = out.ap, in_.ap
        if eng == "scalar":
            self.P.op("scalar", lambda e: e.activation(out=o, in_=i, func=AF.Identity), reads=_keys(in_), writes=_keys(out))
        else:
            self.P.op(eng, lambda e: e.tensor_copy(out=o, in_=i), reads=_keys(in_), writes=_keys(out))

    def evac(self, out, in_):
        self.alt ^= 1
        self.cp(out, in_, eng="scalar" if self.alt else "vector")

    def memset(self, out, val, eng="gpsimd"):
        o = out.ap
        self.P.op(eng, lambda e: e.memset(o, val), writes=_keys(out))

    def scan(self, out, d0, d1, init):
        o, a, b, i = out.ap, d0.ap, d1.ap, _a(init)
        self.P.op("vector", lambda e: e.tensor_tensor_scan(out=o, data0=a, data1=b, initial=i, op0=ALU.mult, op1=ALU.add),
                  reads=_keys(d0, d1, init), writes=_keys(out))

    def dma(self, out, in_, eng="sync", is_output=False):
        o, i = out.ap, in_.ap
        if eng == "gpsimd":
            fn = lambda e: e.dma_start(out=o, in_=i, max_dma_last_dim=4096)
        else:
            fn = lambda e: e.dma_start(out=o, in_=i)
        self.P.dma(eng, fn, reads=_keys(in_), writes=_keys(out), is_output=is_output)

    def scatter(self, out_dram, idx, in_sb):
        o, x, i = out_dram.ap, idx.ap, in_sb.ap
        self.P.dma("gpsimd", lambda e: e.indirect_dma_start(out=o, out_offset=bass.IndirectOffsetOnAxis(ap=x, axis=0),
                                                            in_=i, in_offset=None),
                   reads=_keys(idx, in_sb), writes=_keys(out_dram))

    def gather(self, out_sb, in_dram, idx):
        o, x, i = out_sb.ap, idx.ap, in_dram.ap
        self.P.dma("gpsimd", lambda e: e.indirect_dma_start(out=o, out_offset=None, in_=i,
                                                            in_offset=bass.IndirectOffsetOnAxis(ap=x, axis=0)),
                   reads=_keys(idx, in_dram), writes=_keys(out_sb))

    def bank(self):
        return self.banks.next()

    def ln_stats(self, x):
        st = self.st_ring.next()
        mv = st[:, 12:14]
        self.P.op("vector", (lambda o, i: lambda e: e.bn_stats(out=o, in_=i))(st.ap[:, 0:6], x.ap[:, 0:512]),
                  reads=_keys(x), writes=_keys(st))
        self.P.op("vector", (lambda o, i: lambda e: e.bn_stats(out=o, in_=i))(st.ap[:, 6:12], x.ap[:, 512:1024]),
                  reads=_keys(x, st), writes=_keys(st))
        self.P.op("vector", (lambda o, i: lambda e: e.bn_aggr(out=o, in_=i))(mv.ap, st.ap[:, 0:12]),
                  reads=_keys(st), writes=_keys(st))
        self.act(st[:, 14:15], st[:, 13:14], AF.Sqrt, bias=LN_EPS)
        self.recip(st[:, 15:16], st[:, 14:15])
        return st[:, 12:13], st[:, 15:16]

    def recip(self, out, in_):
        o, i = out.ap, in_.ap
        self.P.op("vector", lambda e: e.reciprocal(out=o, in_=i), reads=_keys(in_), writes=_keys(out))

    def build(self):
        NB, L, CTX, CAP, R = self.NB, self.L, self.CTX, self.CAP, self.R
        nc = self.nc
        NBP = NB + 1 + ((NB + 1) % 2)
        NT = NB * L // 128
        x_d = self.din("x", [NB, L, D])
        ctx_d = self.din("ctx", [NB, CTX, D])
        cT_d = self.din("cT", [128, 8, NBP])
        wmod_d = self.din("w_mod", [D, 6 * D])
        bmodT_d = self.din("bmodT", [128, 16])
        bmodbc_d = self.din("bmod_bc", [128, 4 * D])
        wint_d = self.din("w_in_t", [64, 128, 8, 128])
        winlg_d = self.din("w_in_lg", [128, 8, 32])
        binT_d = self.din("b_inT", [128, 65])
        conva_d = self.din("conv_a", [128, 8, 5])
        lruw_d = self.din("lru_w", [128, 4, 8, 128])
        lrub_d = self.din("lru_b", [128, 4, 8])
        lrulam_d = self.din("lru_lam", [128, 2, 8])
        convq_d = self.din("conv_qkv", [128, 24, 4])
        gdnrows_d = self.din("gdn_rows", [128, 32])
        gdnnw_d = self.din("gdn_nw", [128, 1])
        wpa_d = self.din("w_pa", [D, D])
        wpb_d = self.din("w_pb", [D, D])
        wout_d = self.din("w_out", [D, D])
        lnbc_d = self.din("ln_bc", [128, 4, D])
        wrt_d = self.din("w_rt", [128, 8, 36])
        brt_d = self.din("b_rt_bc", [128, 36])
        weg_d = self.din("w_eg", [NEXP, D, D])
        weu_d = self.din("w_eu", [NEXP, D, D])
        wed_d = self.din("w_ed", [NEXP, D, D])
        out_d = V(nc.dram_tensor("out", [NB * L, D], F32, kind="ExternalOutput").ap(), [("out",)])
        yaT_d = self.dscr("yaT", [NB, 8, 128, L], BF16)
        ybT_d = self.dscr("ybT", [NB, 8, 128, L], BF16)
        sg_d = self.dscr("sg", [NB, 16, 128, L], F32)
        x1_d = self.dscr("x1s", [NB * L, D], F32)
        xg_d = self.dscr("xg", [NEXP * CAP, D], BF16)
        yg_d = self.dscr("yg", [NEXP * CAP, D], F32)
        bcs_d = self.dscr("bcs", [NB, 4, 128, D], F32)
        pq_d = self.dscr("pq", [32, 128, L], F32)
        dbg_d = {}

        st = ExitStack()
        with st:
            self.P = P = Prog(nc, st)
            cap_bytes = 194 * 1024
            self.A = A = Arena(nc, st, cap_bytes)
            banks = []
            for i in range(8):
                t = st.enter_context(nc.psum_tensor("psb%d" % i, [128, 512], F32))
                banks.append(V(t[:, :], [("psb", i)]))
            self.banks = Ring(banks)
            self.r32 = []
            for i in range(4):
                d_ = {}
                for nm in ("N", "M", "X"):
                    t = st.enter_context(nc.sbuf_tensor("r32_%s%d" % (nm, i), [128, 2 * 128], F32))
                    d_[nm] = [V(t[:, j * 128:(j + 1) * 128], [("r32", nm, i, j)]) for j in range(2)]
                self.r32.append(d_)

            ident_f = A.alloc("ident_f", 128)
            ones_f = A.alloc("ones_f", 128)
            mIL = A.alloc("mIL", 128)
            mSL = A.alloc("mSL", 128)
            mIU = A.alloc("mIU", 128)
            mSU = A.alloc("mSU", 128)
            ident_b = A.alloc("ident_b", 128, BF16)
            ones_b = A.alloc("ones_b", 128, BF16)
            mSU_b = A.alloc("mSU_b", 128, BF16)
            self.st_ring = Ring([A.alloc("lnst%d" % i, 16) for i in range(4)])

            def mask(dst, pat_step, cm, op):
                self.memset(dst, 1.0)
                o = dst.ap
                self.P.op("gpsimd", lambda e: e.affine_select(out=o, in_=o, pattern=[[pat_step, 128]], base=0,
                                                               channel_multiplier=cm, compare_op=op, fill=0.0),
                          reads=_keys(dst), writes=_keys(dst))
            self.memset(ones_f, 1.0)
            mask(ident_f, -1, 1, ALU.is_equal)
            mask(mIL, -1, 1, ALU.is_ge)
            mask(mSL, -1, 1, ALU.is_gt)
            mask(mIU, 1, -1, ALU.is_ge)
            mask(mSU, 1, -1, ALU.is_gt)
            self.cp(ident_b, ident_f, eng="gpsimd")
            self.cp(ones_b, ones_f, eng="gpsimd")
            self.cp(mSU_b, mSU, eng="gpsimd")

            modT = A.alloc("modT", (16, NBP))
            binT = A.alloc("binT", 65)
            conva = A.alloc("conva", (8, 5))
            lrub = A.alloc("lrub", (4, 8))
            cch = A.alloc("cch", (2, 8))
            convq = A.alloc("convq", (24, 4))
            gdnrows = A.alloc("gdnrows", 32)
            nega = A.alloc("nega", 16)
            gdnnw = A.alloc("gdnnw", 1)
            lruw = A.alloc("lruw", (4, 8, 128), BF16)
            lru_state = A.alloc("lru_state", (8, 2))
            gdn_state = A.alloc("gdn_state", (8, 2, 128))
            DEST = A.alloc("DEST", (NT, 2), I32)
            WT = A.alloc("WT", (NT, 2))
            CNT = A.alloc("CNT", 32)
            ECAP = A.alloc("ECAP", 32)
            brt = A.alloc("brt", 36)
            for (dst, src) in ((binT, binT_d), (conva, conva_d), (lrub, lrub_d), (cch, lrulam_d), (convq, convq_d),
                               (gdnrows, gdnrows_d), (gdnnw, gdnnw_d), (brt, brt_d)):
                self.dma(dst, src)
            self.dma(lruw, lruw_d, eng="gpsimd")
            self.act(cch, cch, AF.Exp, scale=-1.0)
            self.act(cch, cch, AF.Ln, bias=1.0)
            self.ts(cch, cch, -8.0, None, ALU.mult)
            self.act(nega, gdnrows[:, 16:32], AF.Exp)
            self.ts(nega, nega, -1.0, None, ALU.mult)
            self.memset(CNT, 0.0)
            ec = ECAP.ap
            self.P.op("gpsimd", lambda e: e.iota(ec, pattern=[[CAP, 32]], base=0, channel_multiplier=0,
                                                 allow_small_or_imprecise_dtypes=True), writes=_keys(ECAP))
            zt = A.alloc("zt", D, BF16)
            self.memset(zt, 0.0)
            for e_ in range(NEXP):
                self.dma(V(xg_d.ap[e_ * CAP:(e_ + 1) * CAP, :].rearrange("(n p) d -> p n d", p=128), xg_d.keys),
                         V(zt.ap.unsqueeze(1).to_broadcast([128, CAP // 128, D]), zt.keys))
            base_mark = A.mark()

            cT = A.alloc("cT", (8, NBP))
            self.dma(cT, cT_d)
            sT = A.alloc("sT", (8, NBP))
            self.act(sT, cT, AF.Silu)
            bmodT = A.alloc("bmodT", 16)
            self.dma(bmodT, bmodT_d)
            wmA = A.alloc("wmA", (8, 2048))
            wm_v = wmod_d.re("(k p) n -> p k n", p=128)
            for k in range(8):
                self.dma(wmA[:, k, :].k(k), wm_v[:, k, 0:2048])
            for cc in range(16):
                ps = self.bank()
                for k in range(8):
                    self.mm(ps[:, 0:NBP], wmA[:, k, cc * 128:(cc + 1) * 128].k(k), sT[:, k, :], start=(k == 0), stop=(k == 7))
                self.ts(modT[:, cc, :], ps[:, 0:NBP], bmodT[:, cc:cc + 1], 1.0 if cc >= 8 else 0.0, ALU.add, ALU.add)
            A.reset(A.mark() - 0)
            sbc = [A.alloc("sbc%d" % b, (8, 128)) for b in range(NB)]
            for b in range(NB):
                self.cp(sbc[b], sT[:, :, b:b + 1].bc([128, 8, 128]))
            wmB = Ring([A.alloc("wmB%d" % i, (8, 512)) for i in range(2)])
            bmB = Ring([A.alloc("bmB%d" % i, 512) for i in range(2)])
            stg = Ring([A.alloc("stg%d" % i, 512) for i in range(3)])
            for ct in range(8):
                w = wmB.next()
                bm = bmB.next()
                for k in range(8):
                    self.dma(w[:, k, :].k(k), wm_v[:, k, 2048 + ct * 512:2048 + (ct + 1) * 512])
                self.dma(bm, bmodbc_d[:, ct * 512:(ct + 1) * 512])
                for b in range(NB):
                    ps = self.bank()
                    for k in range(8):
                        self.mm(ps, sbc[b][:, k, :], w[:, k, :].k(k), start=(k == 0), stop=(k == 7))
                    sg_ = stg.next()
                    if ct // 2 == 2:
                        self.stt(sg_, ps, 1.0, bm, ALU.add, ALU.add)
                    else:
                        self.tt(sg_, ps, bm, ALU.add)
                    self.dma(bcs_d[b, ct // 2, :, (ct % 2) * 512:(ct % 2 + 1) * 512].k(b, ct), sg_)
            self.P.barrier()
            A.reset(base_mark)

            import os as _os
            KSTOP = int(_os.environ.get("KSTOP", "99"))
            for b in range(NB if KSTOP > 0 else 0):
                for which in ("ctx", "lat"):
                    if KSTOP == 1 and which == "lat":
                        continue
                    tabs, tab_mark = self.mixer(b, which, locals())
                    self.P.barrier()
                    A.reset(tab_mark)
                    if KSTOP > 2:
                        self.gdn(b, which == "lat", tabs, locals())
                    self.P.barrier()
                    A.reset(base_mark)
                if KSTOP > 3:
                    self.merge(b, locals())
                self.P.barrier()
                A.reset(base_mark)
            if KSTOP > 4:
                self.experts(locals())
            self.P.barrier()
            A.reset(base_mark)
            if KSTOP > 5:
                self.combine(locals())
            P.finish()
            with nc.Block() as block:
                P.emit(block)
        return nc

    def load_wchunk(self, wring, wint_d, c):
        w = wring.next()
        self.dma(w, wint_d[c], eng="gpsimd")
        return w

    def proj(self, w, M, hT, Lx, evac_fn):
        TL = min(512, Lx)
        for t in range(Lx // TL):
            ps = self.bank()
            for k in range(8):
                self.mm(ps[0:M, 0:TL], w[:, k, 0:M], hT[:, k, t * TL:(t + 1) * TL], start=(k == 0), stop=(k == 7))
            evac_fn(t, TL, ps[0:M, 0:TL])

    def mixer(self, b, which, E):
        A, P = self.A, self.P
        NB, L, CTX, R = self.NB, self.L, self.CTX, self.R
        lat = which == "lat"
        Lx = L if lat else CTX
        nch = Lx // 128
        NH = nch * 8
        col = b if lat else NB
        src = E["x_d"][b] if lat else E["ctx_d"][b]
        modT, binT, ident_f, ident_b, ones_f, ones_b = E["modT"], E["binT"], E["ident_f"], E["ident_b"], E["ones_f"], E["ones_b"]
        mIL, mIU = E["mIL"], E["mIU"]
        gdnrows, nega = E["gdnrows"], E["nega"]
        wint_d = E["wint_d"]
        tabs = {}
        for nm in ("BT", "NBT", "GC", "EKG", "GL", "BEG"):
            tabs[nm] = A.alloc(nm, (2, nch, 8))
        tab_mark = A.mark()
        hT = A.alloc("hT", (8, Lx), BF16)
        m0 = A.mark()
        xin = Ring([A.alloc("xin%d" % i, D) for i in range(2)])
        xnr = Ring([A.alloc("xn%d" % i, D) for i in range(2)])
        for t in range(Lx // 128):
            xt = xin.next()
            self.dma(xt, src[t * 128:(t + 1) * 128, :])
            mean, rstd = self.ln_stats(xt)
            xn = xnr.next()
            self.ts(xn, xt, mean, rstd, ALU.subtract, ALU.mult)
            for half in range(2):
                ps = self.bank()
                for kk in range(4):
                    k = half * 4 + kk
                    self.tr(ps[:, kk * 128:(kk + 1) * 128], xn[:, k * 128:(k + 1) * 128], ident_f)
                for kk in range(4):
                    k = half * 4 + kk
                    o = hT[:, k, t * 128:(t + 1) * 128].k(t)
                    if kk % 2 == 0:
                        self.act(o, ps[:, kk * 128:(kk + 1) * 128], AF.Identity, bias=modT[:, k, col:col + 1], scale=modT[:, 8 + k, col:col + 1])
                    else:
                        self.ts(o, ps[:, kk * 128:(kk + 1) * 128], modT[:, 8 + k, col:col + 1], modT[:, k, col:col + 1], ALU.mult, ALU.add)
        hT = V(hT.ap, [kk + (t,) for kk in hT.keys for t in range(Lx // 128)])
        self.P.barrier()
        A.reset(m0)
        wring = Ring([A.alloc("wch%d" % i, (8, 128), BF16) for i in range(2)])
        m1 = A.mark()

        conva, lrub, cch, lruw, lru_state = E["conva"], E["lrub"], E["cch"], E["lruw"], E["lru_state"]
        xa_pad = A.alloc("xa_pad", Lx + 3)
        u = A.alloc("u", Lx)
        u_bf = A.alloc("u_bf", Lx, BF16)
        Ab = A.alloc("Abuf", Lx)
        Ib = A.alloc("Ibuf", Lx)
        HF = A.alloc("HF", Lx)
        Tb = V(xa_pad.ap[:, 0:Lx], xa_pad.keys)
        HB = Tb
        YA = V(Ib.ap.bitcast(BF16)[:, 0:Lx], Ib.keys)
        TL = min(512, Lx)
        for n in range(8):
            w = self.load_wchunk(wring, wint_d, n)
            self.memset(xa_pad[:, 0:1], 0.0)
            self.memset(xa_pad[:, Lx + 1:Lx + 3], 0.0)
            self.proj(w, 128, hT, Lx, lambda t, tl, ps: self.act(xa_pad[:, 1 + t * tl:1 + (t + 1) * tl], ps, AF.Identity,
                                                                    bias=binT[:, n:n + 1]))
            self.ts(u, xa_pad[:, 0:Lx], conva[:, n, 0:1], conva[:, n, 4:5], ALU.mult, ALU.add)
            for j in range(1, 4):
                self.stt(u, xa_pad[:, j:j + Lx], conva[:, n, j:j + 1], u, ALU.mult, ALU.add)
            self.cp(u_bf, u, eng="scalar")
            for d in range(2):
                for t in range(Lx // TL):
                    sl = slice(t * TL, (t + 1) * TL)
                    ps = self.bank()
                    self.mm(ps[:, 0:TL], lruw[:, 2 * d, n, :], u_bf[:, sl])
                    self.act(Ab[:, sl], ps[:, 0:TL], AF.Sigmoid, bias=lrub[:, 2 * d, n:n + 1])
                    ps2 = self.bank()
                    self.mm(ps2[:, 0:TL], lruw[:, 2 * d + 1, n, :], u_bf[:, sl])
                    self.act(Ib[:, sl], ps2[:, 0:TL], AF.Sigmoid, bias=lrub[:, 2 * d + 1, n:n + 1])
                self.act(Ab, Ab, AF.Exp, scale=cch[:, d, n:n + 1])
                self.tt(Tb, Ab, Ab, ALU.mult, eng="gpsimd")
                self.ts(Tb, Tb, 0.99999994, -1.0, ALU.min, ALU.mult)
                self.act(Tb, Tb, AF.Sqrt, bias=1.0)
                self.tt(Ib, Ib, u, ALU.mult, eng="gpsimd")
                self.tt(Ib, Ib, Tb, ALU.mult)
                init = lru_state[:, n, d:d + 1] if lat else 0.0
                if d == 0:
                    self.scan(HF, Ab, Ib, init)
                else:
                    self.scan(HB[:, ::-1], Ab[:, ::-1], Ib[:, ::-1], init)
            if not lat:
                self.cp(lru_state[:, n, 0:1], HF[:, Lx - 1:Lx])
                self.cp(lru_state[:, n, 1:2], HB[:, 0:1])
            else:
                self.tt(HF, HF, HB, ALU.add, eng="gpsimd")
                w = self.load_wchunk(wring, wint_d, 8 + n)
                self.proj(w, 128, hT, Lx, lambda t, tl, ps: self.act(Ab[:, t * tl:(t + 1) * tl], ps, AF.Identity,
                                                                        bias=binT[:, 8 + n:9 + n]))
                self.tt(Tb, Ab, Ab, ALU.mult, eng="gpsimd")
                self.ts(Tb, Tb, 0.044715, 1.0, ALU.mult, ALU.add)
                self.tt(Tb, Tb, Ab, ALU.mult)
                self.act(Tb, Tb, AF.Sigmoid, scale=2.0 * math.sqrt(2.0 / math.pi))
                self.tt(Tb, Tb, Ab, ALU.mult, eng="gpsimd")
                self.tt(YA, Tb, HF, ALU.mult)
                self.dma(E["yaT_d"][b, n].k(b, n), YA)
        self.P.barrier()
        A.reset(m1)

        SG = Ring([A.alloc("SG%d" % i, Lx) for i in range(2)])
        if lat:
            for dc in range(16):
                w = self.load_wchunk(wring, wint_d, 48 + dc)
                sgb = SG.next()
                self.proj(w, 128, hT, Lx, lambda t, tl, ps: self.act(sgb[:, t * tl:(t + 1) * tl], ps, AF.Sigmoid,
                                                                        bias=binT[:, 48 + dc:49 + dc]))
                self.dma(E["sg_d"][b, dc].k(b, dc), sgb)
        pq = E["pq_d"]
        for c in range(16, 48 if lat else 40):
            w = self.load_wchunk(wring, wint_d, c)
            sgb = SG.next()
            func = AF.Silu if c >= 40 else AF.Identity
            self.proj(w, 128, hT, Lx, lambda t, tl, ps: self.act(self.cm_out(sgb, 0, Lx, lat, t, tl), self.cm_in(ps, lat),
                                                                    func, bias=binT[:, c:c + 1]))
            self.dma(V(pq.ap[c - 16][:, 0:Lx], [kk + (c,) for kk in pq.keys]), sgb)
        self.P.barrier()
        A.reset(m1)

        wlg = A.alloc("wlg", (8, 32), BF16)
        self.dma(wlg, E["winlg_d"], eng="gpsimd")
        lgT = A.alloc("lgT", Lx)
        for t in range(Lx // TL):
            ps = self.bank()
            for k in range(8):
                self.mm(ps[0:32, 0:TL], wlg[:, k, :], hT[:, k, t * TL:(t + 1) * TL], start=(k == 0), stop=(k == 7))
            self.act(self.cm_out(lgT, 0, Lx, lat, t, TL)[0:32], self.cm_in(ps[0:32, 0:TL], lat), AF.Identity, bias=binT[0:32, 64:65])
        lg_tm = A.alloc("lg_tm", (nch, 32))
        for n in range(nch):
            ps = self.bank()
            self.tr(ps[:, 0:32], lgT[0:32, n * 128:(n + 1) * 128], ident_f[0:32, 0:32])
            self.evac(lg_tm[:, n, :], ps[:, 0:32])
        XG = A.alloc("XG", (2, nch, 8))
        AXb = A.alloc("AXb", (2, nch, 8))
        Gt = A.alloc("Gt", (2, nch, 8))
        GTOT = A.alloc("GTOT", (2, nch, 8))
        BT, NBT, GC, EKG, GL, BEG = (tabs[nm] for nm in ("BT", "NBT", "GC", "EKG", "GL", "BEG"))
        lg4 = lg_tm.re("p n (j h) -> p j n h", h=8)
        dtb = gdnrows[:, 0:16].re("p (d h) -> p d h", h=8)
        self.tt(XG, lg4[:, 0:2], V(dtb.ap.unsqueeze(2).to_broadcast([128, 2, nch, 8]), dtb.keys), ALU.add)
        self.act(AXb, XG, AF.Abs)
        self.act(AXb, AXb, AF.Exp, scale=-1.0)
        self.act(AXb, AXb, AF.Ln, bias=1.0)
        self.stt(XG, XG, 0.0, AXb, ALU.max, ALU.add)
        ng = nega.re("p (d h) -> p d h", h=8)
        self.tt(Gt, XG, V(ng.ap.unsqueeze(2).to_broadcast([128, 2, nch, 8]), ng.keys), ALU.mult)
        self.act(BT, lg4[:, 2:4], AF.Sigmoid)
        self.ts(NBT, BT, -1.0, None, ALU.mult)
        for d in range(2):
            ps = self.bank()
            tri = mIU if d == 0 else mIL
            self.mm(ps[:, 0:NH], tri, Gt[:, d].re("p n h -> p (n h)"))
            self.evac(GC[:, d].re("p n h -> p (n h)"), ps[:, 0:NH])
            ps = self.bank()
            self.mm(ps[:, 0:NH], ones_f, Gt[:, d].re("p n h -> p (n h)"))
            self.evac(GTOT[:, d].re("p n h -> p (n h)"), ps[:, 0:NH])
        self.tt(EKG, GTOT, GC, ALU.subtract)
        self.act(EKG, EKG, AF.Exp)
        self.act(GL, GTOT, AF.Exp)
        self.act(BEG, GC, AF.Exp)
        self.tt(BEG, BEG, BT, ALU.mult)
        return tabs, tab_mark

    def cm_out(self, buf, off, Lx, lat, t, tl):
        if not lat:
            return buf[:, off + t * tl:off + (t + 1) * tl]
        rows = tl // GW
        v = buf[:, off:off + Lx].re("p (c r) -> p r c", c=GW)
        return v[:, t * rows:(t + 1) * rows, :]

    def cm_in(self, ps, lat):
        if not lat:
            return ps
        return ps.re("p (r c) -> p r c", c=GW)

    def gdn(self, b, lat, tabs, E):
        A, P = self.A, self.P
        NB, R = self.NB, self.R
        Lx = self.L if lat else self.CTX
        nch = Lx // 128
        TL = min(512, Lx)
        binT, ident_f, ident_b, ones_f, ones_b = E["binT"], E["ident_f"], E["ident_b"], E["ones_f"], E["ones_b"]
        mIL, mSL, mIU, mSU = E["mIL"], E["mSL"], E["mIU"], E["mSU"]
        gdnnw, convq, gdn_state = E["gdnnw"], E["convq"], E["gdn_state"]
        BT, NBT, GC, EKG, GL, BEG = (tabs[nm] for nm in ("BT", "NBT", "GC", "EKG", "GL", "BEG"))
        pq = E["pq_d"]
        if not lat:
            self.memset(gdn_state, 0.0)
        PAD = A.alloc("PAD", Lx + 16)
        CV = A.alloc("CV", Lx)
        qT = A.alloc("qT", Lx, BF16)
        kT = A.alloc("kT", Lx, BF16)
        vT = A.alloc("vT", Lx, BF16)
        OT = A.alloc("OT", Lx)
        OTa = V(OT.ap, [kk + (n,) for kk in OT.keys for n in range(nch)])
        k_tm = A.alloc("k_tm", (nch, 128), BF16)
        v_tm = A.alloc("v_tm", (nch, 128), BF16)
        YB = V(PAD.ap[:, 0:Lx // 2].bitcast(BF16), PAD.keys)
        SQr = Ring([A.alloc("SQ%d" % i, TL, BF16) for i in range(2)])
        RTr = Ring([A.alloc("RT%d" % i, TL) for i in range(2)])
        S_bf = [A.alloc("S_bf%d" % d, 128, BF16) for d in range(2)]

        GC_ = 2 if nch % 2 == 0 else 1
        NI = 2 * GC_
        TMP = [{nm: A.alloc("%s_t%d" % (nm, i), 128) for nm in ("DG", "M1", "M2", "ER", "E2i", "KB", "VB")} for i in range(NI)]
        RNG = [{nm: Ring(self.r32[i][nm]) for nm in ("N", "M", "X")} for i in range(NI)]
        OUTT = [[{nm: A.alloc("%s_o%d_%d" % (nm, par, i), 128, F32 if nm == "UC" else BF16)
                  for nm in ("ACT", "WCT", "UC", "QG", "KG", "VN")} for i in range(NI)] for par in range(2)]

        import os as _os
        KG = int(_os.environ.get("KG", "99"))
        for h in range(8 if KG > 0 else 0):
            for (qi, dst) in ((0, qT), (1, kT), (2, vT)):
                c = 16 + qi * 8 + h
                self.memset(PAD[:, 7:8], 0.0)
                self.memset(PAD[:, Lx + 8:Lx + 10], 0.0)
                self.dma(PAD[:, 8:8 + Lx], V(pq.ap[c - 16][:, 0:Lx], [kk + (c,) for kk in pq.keys]))
                cw = convq[:, qi * 8 + h, :]
                self.ts(CV, PAD[:, 7:7 + Lx], cw[:, 0:1], None, ALU.mult)
                for j in range(1, 4):
                    self.stt(CV, PAD[:, 7 + j:7 + j + Lx], cw[:, j:j + 1], CV, ALU.mult, ALU.add)
                self.act(CV, CV, AF.Silu)
                if qi < 2:
                    for t in range(Lx // TL):
                        sl = slice(t * TL, (t + 1) * TL)
                        sq, rt = SQr.next(), RTr.next()
                        self.tt(sq, CV[:, sl], CV[:, sl], ALU.mult, eng="gpsimd")
                        ps = self.bank()
                        self.mm(ps[:, 0:TL], ones_b, sq)
                        self.act(rt, ps[:, 0:TL], AF.Sqrt, bias=1e-6)
                        self.recip(rt, rt)
                        self.stt(dst[:, sl], CV[:, sl], (128.0 ** -0.5) if qi == 0 else 1.0, rt, ALU.mult, ALU.mult)
                else:
                    self.cp(dst, CV, eng="scalar")
            if KG < 2:
                continue
            for n in range(nch):
                ps = self.bank()
                self.mm(ps[:, 0:128], kT[:, n * 128:(n + 1) * 128], ident_b)
                self.mm(ps[:, 128:256], vT[:, n * 128:(n + 1) * 128], ident_b)
                self.cp(k_tm[:, n, :].k(n), ps[:, 0:128], eng="scalar")
                self.cp(v_tm[:, n, :].k(n), ps[:, 128:256], eng="vector")
            self.memset(OTa, 0.0)
            for d in range(2):
                self.cp(S_bf[d], gdn_state[:, h, d, :], eng="scalar")
            ngroups = nch // GC_

            def group_insts(p):
                L_ = []
                for j in range(GC_):
                    for d in range(2):
                        s_ = p * GC_ + j
                        n = s_ if d == 0 else nch - 1 - s_
                        i = j * 2 + d
                        L_.append({"d": d, "n": n, "cs": slice(n * 128, (n + 1) * 128), "T": TMP[i], "R": RNG[i],
                                   "O": OUTT[p % 2][i]})
                return L_

            def pre(insts):
                for I in insts:
                    d, n, T = I["d"], I["n"], I["T"]
                    ktn, vtn = k_tm[:, n, :].k(n), v_tm[:, n, :].k(n)
                    I["Gp"] = GC[:, d, n, h:h + 1]
                    self.ts(T["KB"], ktn, BEG[:, d, n, h:h + 1], None, ALU.mult, eng="gpsimd")
                    self.ts(I["O"]["KG"], ktn, EKG[:, d, n, h:h + 1], None, ALU.mult, eng="gpsimd")
                    self.ts(T["VB"], vtn, BT[:, d, n, h:h + 1], None, ALU.mult, eng="gpsimd")
                    self.ts(T["DG"], ident_f, I["Gp"], None, ALU.mult, eng="gpsimd")
                for I in insts:
                    I["psg"] = self.bank()
                    self.mm(I["psg"][:, 0:128], ones_f, I["T"]["DG"])
                for I in insts:
                    T, psg = I["T"], I["psg"]
                    self.ts(T["M1"], psg[:, 0:128], I["Gp"], 0.0, ALU.subtract, ALU.max)
                    self.ts(T["M2"], psg[:, 0:128], I["Gp"], 0.0, ALU.subtract, ALU.min)
                    self.act(T["ER"], psg[:, 0:128], AF.Exp)
                for I in insts:
                    T = I["T"]
                    self.act(T["M1"], T["M1"], AF.Exp, scale=-1.0)
                    self.act(T["M2"], T["M2"], AF.Exp)
                for I in insts:
                    T, d = I["T"], I["d"]
                    self.tt(T["M1"], T["M1"], mSL if d == 0 else mSU, ALU.mult, eng="gpsimd")
                    self.tt(T["E2i"], T["M2"], mIU if d == 0 else mIL, ALU.mult, eng="gpsimd")
                for I in insts:
                    cs = I["cs"]
                    I["psk"] = self.bank()
                    self.mm(I["psk"][:, 0:128], kT[:, cs], kT[:, cs])
                    self.mm(I["psk"][:, 128:256], kT[:, cs], qT[:, cs])
                for I in insts:
                    T, d, n = I["T"], I["d"], I["n"]
                    I["Nm"], I["Mm"], I["Xm"] = I["R"]["N"].next(), I["R"]["M"].next(), I["R"]["X"].next()
                    self.stt(I["Nm"], I["psk"][:, 0:128], NBT[:, d, n, h:h + 1], T["M1"], ALU.mult, ALU.mult)
                    self.tt(I["O"]["ACT"], I["psk"][:, 128:256], T["E2i"], ALU.mult)
                for I in insts:
                    I["pst"] = self.bank()
                    self.tr(I["pst"][:, 0:128], I["Nm"].cast(F32), ident_f)
                for I in insts:
                    self.cp(I["Mm"], I["pst"][:, 0:128], eng="scalar")
                    self.tt(I["Xm"], I["Mm"].cast(F32), ident_f, ALU.add)
                    I["Pm"], I["PTm"] = I["Mm"], I["Nm"]
                for lvl in range(6):
                    last = lvl == 5
                    for I in insts:
                        I["psl"] = self.bank()
                        if not last:
                            self.mm(I["psl"][:, 0:128], I["PTm"], I["Pm"])
                        self.mm(I["psl"][:, 128:256], I["Pm"], I["PTm"])
                    for I in insts:
                        I["PT2"] = I["R"]["N"].next()
                        self.cp(I["PT2"], I["psl"][:, 128:256], eng="scalar")
                        if not last:
                            I["P2"] = I["R"]["M"].next()
                            self.cp(I["P2"], I["psl"][:, 0:128], eng="vector")
                    for I in insts:
                        I["psx"] = self.bank()
                        self.mm(I["psx"][:, 0:128], I["PT2"], I["Xm"])
                    for I in insts:
                        X2 = I["R"]["X"].next()
                        self.tt(X2, I["psx"][:, 0:128], I["Xm"].cast(F32), ALU.add)
                        I["Xm"] = X2
                        I["PTm"] = I["PT2"]
                        if not last:
                            I["Pm"] = I["P2"]
                for I in insts:
                    I["psw"] = self.bank()
                    self.mm(I["psw"][:, 0:128], I["T"]["KB"], I["Xm"].cast(F32))
                    self.mm(I["psw"][:, 128:256], I["Xm"].cast(F32), I["T"]["VB"])
                for I in insts:
                    self.cp(I["O"]["WCT"], I["psw"][:, 0:128], eng="scalar")
                    self.cp(I["O"]["UC"], I["psw"][:, 128:256], eng="vector")
                    self.tt(I["O"]["QG"], qT[:, I["cs"]], I["T"]["ER"], ALU.mult, eng="gpsimd")

            def state(insts):
                for I in insts:
                    d, n, cs, O = I["d"], I["n"], I["cs"], I["O"]
                    Sst = gdn_state[:, h, d, :]
                    pss = self.bank()
                    self.mm(pss[:, 0:128], O["WCT"], S_bf[d])
                    self.tt(O["VN"], O["UC"], pss[:, 0:128], ALU.subtract)
                    pso = self.bank()
                    self.mm(pso[:, 0:128], S_bf[d], O["QG"], start=True, stop=False)
                    self.mm(pso[:, 0:128], O["VN"], O["ACT"], start=False, stop=True)
                    psu = self.bank()
                    self.mm(psu[:, 0:128], O["KG"], O["VN"])
                    OTn = V(OT.ap[:, cs], [kk + (n,) for kk in OT.keys])
                    self.tt(OTn, OTn, pso[:, 0:128], ALU.add)
                    self.stt(Sst, Sst, GL[:, d, n, h:h + 1], psu[:, 0:128], ALU.mult, ALU.add)
                    self.cp(S_bf[d], Sst, eng="scalar")

            groups = [group_insts(p) for p in range(ngroups)]
            pre(groups[0])
            for p in range(ngroups):
                if p + 1 < ngroups:
                    pre(groups[p + 1])
                state(groups[p])
            if lat and KG > 6:
                c = 40 + h
                self.dma(CV, V(pq.ap[c - 16][:, 0:Lx], [kk + (c,) for kk in pq.keys]))
                for t in range(Lx // TL):
                    sl = slice(t * TL, (t + 1) * TL)
                    sq, rt = SQr.next(), RTr.next()
                    self.tt(sq, OTa[:, sl], OTa[:, sl], ALU.mult, eng="gpsimd")
                    ps = self.bank()
                    self.mm(ps[:, 0:TL], ones_b, sq)
                    self.act(rt, ps[:, 0:TL], AF.Sqrt, bias=LN_EPS, scale=1.0 / 128.0)
                    self.recip(rt, rt)
                    self.tt(CV[:, sl], CV[:, sl], rt, ALU.mult, eng="gpsimd")
                self.stt(YB.re("p (r c) -> p c r", c=GW), OTa.re("p (c r) -> p c r", c=GW), gdnnw[:, 0:1],
                         CV.re("p (c r) -> p c r", c=GW), ALU.mult, ALU.mult)
                self.dma(E["ybT_d"][b, h].k(b, h), YB)

    def merge(self, b, E):
        A, P = self.A, self.P
        NB, L, CAP = self.NB, self.L, self.CAP
        ident_f, ones_b, mSU_b = E["ident_f"], E["ones_b"], E["mSU_b"]
        DEST, WT, CNT, ECAP, brt = E["DEST"], E["WT"], E["CNT"], E["ECAP"], E["brt"]
        wpa = A.alloc("wpa", (8, D), BF16)
        wpb = A.alloc("wpb", (8, D), BF16)
        wout = A.alloc("wout", (8, D), BF16)
        for (dst, src) in ((wpa, E["wpa_d"]), (wpb, E["wpb_d"]), (wout, E["wout_d"])):
            sv = src.re("(k p) n -> p k n", p=128)
            for k in range(8):
                self.dma(dst[:, k, :].k(k), sv[:, k, :], eng="gpsimd")
        wrt = A.alloc("wrt", (8, 36))
        self.dma(wrt, E["wrt_d"])
        g1bc = A.alloc("g1bc", D)
        sh2bc = A.alloc("sh2bc", D)
        sc2bc = A.alloc("sc2bc", D)
        ln1g = A.alloc("ln1g", D)
        ln1b = A.alloc("ln1b", D)
        bcs = E["bcs_d"]
        bk = [kk + (b, ct) for kk in bcs.keys for ct in range(8)]
        self.dma(g1bc, V(bcs.ap[b, 0], bk))
        self.dma(sh2bc, V(bcs.ap[b, 1], bk))
        self.dma(sc2bc, V(bcs.ap[b, 2], bk))
        self.dma(ln1g, E["lnbc_d"][:, 0, :])
        self.dma(ln1b, E["lnbc_d"][:, 1, :])
        YAt = Ring([A.alloc("YAt%d" % i, (8, 512), BF16) for i in range(1)])
        YBt = Ring([A.alloc("YBt%d" % i, (8, 512), BF16) for i in range(1)])
        SGa = Ring([A.alloc("SGa%d" % i, 512) for i in range(3)])
        SGb = Ring([A.alloc("SGb%d" % i, 512) for i in range(3)])
        M1r = Ring([A.alloc("M1r%d" % i, 512) for i in range(2)])
        M2r = Ring([A.alloc("M2r%d" % i, 512) for i in range(2)])
        MTr = Ring([A.alloc("MT%d" % i, (8, 512), BF16) for i in range(1)])
        T1r = Ring([A.alloc("T1%d" % i, D) for i in range(2)])
        Xtr = Ring([A.alloc("Xt%d" % i, D) for i in range(2)])
        X1r = Ring([A.alloc("X1%d" % i, D) for i in range(2)])
        H2r = Ring([A.alloc("H2%d" % i, D) for i in range(2)])
        H2br = Ring([A.alloc("H2b%d" % i, D, BF16) for i in range(3)])
        H2Tr = Ring([A.alloc("H2T%d" % i, (8, 128)) for i in range(2)])
        sm = Ring([A.alloc("rsm%d" % i, 256) for i in range(3)])
        yav = E["yaT_d"]
        ybv = E["ybT_d"]
        sgv = E["sg_d"]
        TLm = 512
        for g in range(L // TLm):
            ts_ = slice(g * TLm, (g + 1) * TLm)
            ya, yb = YAt.next(), YBt.next()
            for k in range(8):
                self.dma(ya[:, k, :].k(k), V(yav.ap[b, k][:, ts_], [kk + (b, k) for kk in yav.keys]))
                self.dma(yb[:, k, :].k(k), V(ybv.ap[b, k][:, ts_], [kk + (b, k) for kk in ybv.keys]))
            MT = MTr.next()
            for dc in range(8):
                sga, sgb = SGa.next(), SGb.next()
                self.dma(sga, V(sgv.ap[b, dc][:, ts_], [kk + (b, dc) for kk in sgv.keys]))
                self.dma(sgb, V(sgv.ap[b, 8 + dc][:, ts_], [kk + (b, 8 + dc) for kk in sgv.keys]))
                psa = self.bank()
                for k in range(8):
                    self.mm(psa, wpa[:, k, dc * 128:(dc + 1) * 128].k(k), ya[:, k, :].k(k), start=(k == 0), stop=(k == 7))
                psb_ = self.bank()
                for k in range(8):
                    self.mm(psb_, wpb[:, k, dc * 128:(dc + 1) * 128].k(k), yb[:, k, :].k(k), start=(k == 0), stop=(k == 7))
                m1, m2 = M1r.next(), M2r.next()
                self.tt(m1, psa, sga, ALU.mult)
                self.tt(m2, psb_, sgb, ALU.mult)
                self.tt(MT[:, dc, :].k(dc), m1, m2, ALU.add, eng="gpsimd")
            MTa = V(MT.ap, [kk + (dc,) for kk in MT.keys for dc in range(8)])
            for sub in range(4):
                tile = (b * L + g * TLm) // 128 + sub
                tok0 = g * TLm + sub * 128
                T1 = T1r.next()
                for half in range(2):
                    ps = self.bank()
                    for dc in range(8):
                        self.mm(ps, MTa[:, dc, sub * 128:(sub + 1) * 128], wout[:, dc, half * 512:(half + 1) * 512].k(dc),
                                start=(dc == 0), stop=(dc == 7))
                    self.tt(T1[:, half * 512:(half + 1) * 512].k(half), ps, g1bc[:, half * 512:(half + 1) * 512], ALU.mult)
                T1a = V(T1.ap, [kk + (hh,) for kk in T1.keys for hh in range(2)])
                xt = Xtr.next()
                self.dma(xt, E["x_d"][b][tok0:tok0 + 128, :])
                self.stt(T1a, xt, ALPHA, T1a, ALU.mult, ALU.add)
                mean, rstd = self.ln_stats(T1a)
                self.ts(T1a, T1a, mean, rstd, ALU.subtract, ALU.mult)
                X1 = X1r.next()
                self.tt(X1, T1a, ln1g, ALU.mult, eng="gpsimd")
                self.tt(X1, X1, ln1b, ALU.add, eng="gpsimd")
                self.dma(V(E["x1_d"].ap[tile * 128:(tile + 1) * 128, :], [kk + (tile,) for kk in E["x1_d"].keys]), X1)
                self.route(tile, X1, sh2bc, sc2bc, wrt, H2r, H2br, H2Tr, sm, E)

    def route(self, tile, X1, sh2bc, sc2bc, wrt, H2r, H2br, H2Tr, sm, E):
        CAP = self.CAP
        ident_f, ones_b, mSU_b = E["ident_f"], E["ones_b"], E["mSU_b"]
        DEST, WT, CNT, ECAP, brt = E["DEST"], E["WT"], E["CNT"], E["ECAP"], E["brt"]
        mean, rstd = self.ln_stats(X1)
        H2 = H2r.next()
        self.ts(H2, X1, mean, rstd, ALU.subtract, ALU.mult)
        self.tt(H2, H2, sc2bc, ALU.mult, eng="gpsimd")
        self.tt(H2, H2, sh2bc, ALU.add, eng="gpsimd")
        H2b = H2br.next()
        self.cp(H2b, H2, eng="scalar")
        H2T = H2Tr.next()
        for half in range(2):
            ps = self.bank()
            for kk in range(4):
                k = half * 4 + kk
                self.tr(ps[:, kk * 128:(kk + 1) * 128], H2[:, k * 128:(k + 1) * 128], ident_f)
            self.evac(H2T[:, half * 4:(half + 1) * 4, :].k(half), ps.re("p (a b) -> p a b", b=128))
        H2Ta = V(H2T.ap, [kk + (hh,) for kk in H2T.keys for hh in range(2)])
        psr = self.bank()
        for k in range(8):
            self.mm(psr[:, 0:36], H2Ta[:, k, :], wrt[:, k, :], start=(k == 0), stop=(k == 7))
        s = sm.next()
        LG = s[:, 0:36]
        gmax, ngmax, gsum, pg = s[:, 36:37], s[:, 37:38], s[:, 38:39], s[:, 39:40]
        GE, OHG = s[:, 40:44], s[:, 44:48]
        ML = s[:, 48:80]
        m8 = s[:, 80:88]
        d21, e21, den, w1, w2 = s[:, 88:89], s[:, 89:90], s[:, 90:91], s[:, 91:92], s[:, 92:93]
        OH1, OH2 = s[:, 96:128], s[:, 128:160]
        SL = s[:, 160:192]
        TMP = s[:, 192:224]
        d1f, d2f = s[:, 224:225], s[:, 225:226]
        Ab = V(s.ap[:, 232:248].bitcast(BF16), s.keys)
        self.tt(LG, psr[:, 0:36], brt, ALU.add)
        rmx = lambda o, i: self.P.op("vector", (lambda oo, ii: lambda e: e.reduce_max(out=oo, in_=ii, axis=AX.X))(o.ap, i.ap),
                                     reads=_keys(i), writes=_keys(o))
        rsm = lambda o, i: self.P.op("vector", (lambda oo, ii: lambda e: e.reduce_sum(out=oo, in_=ii, axis=AX.X))(o.ap, i.ap),
                                     reads=_keys(i), writes=_keys(o))
        rmx(gmax, LG[:, 0:4])
        self.ts(ngmax, gmax, -1.0, None, ALU.mult)
        self.act(GE, LG[:, 0:4], AF.Exp, bias=ngmax)
        rsm(gsum, GE)
        self.recip(pg, gsum)
        self.ts(OHG, LG[:, 0:4], gmax, None, ALU.is_equal)
        self.ts(OHG, OHG, 1.0, 1e9, ALU.subtract, ALU.mult)
        self.tt(ML.re("p (g e) -> p g e", e=8), LG[:, 4:36].re("p (g e) -> p g e", e=8),
                V(OHG.ap.unsqueeze(2).to_broadcast([128, 4, 8]), OHG.keys), ALU.add)
        ml, m8a = ML.ap, m8.ap
        self.P.op("vector", lambda e: e.max(out=m8a, in_=ml), reads=_keys(ML), writes=_keys(m8))
        self.tt(d21, m8[:, 1:2], m8[:, 0:1], ALU.subtract)
        self.act(e21, d21, AF.Exp)
        self.ts(den, e21, 1.0, None, ALU.add)
        self.recip(den, den)
        self.tt(w1, pg, den, ALU.mult)
        self.tt(w2, w1, e21, ALU.mult)
        self.cp(WT[:, tile, 0:1].k(tile), w1)
        self.cp(WT[:, tile, 1:2].k(tile), w2)
        self.ts(OH1, ML, m8[:, 0:1], None, ALU.is_equal)
        self.ts(OH2, ML, m8[:, 1:2], None, ALU.is_equal)
        self.tt(Ab, OH1, OH2, ALU.add)
        psk = self.bank()
        self.mm(psk[:, 0:32], mSU_b, Ab)
        self.mm(psk[:, 32:64], ones_b, Ab)
        self.tt(SL, psk[:, 0:32], CNT, ALU.add)
        self.stt(SL, SL, float(CAP - 1), ECAP, ALU.min, ALU.add)
        self.tt(CNT, CNT, psk[:, 32:64], ALU.add)
        self.tt(TMP, OH1, SL, ALU.mult)
        rsm(d1f, TMP)
        self.tt(TMP, OH2, SL, ALU.mult)
        rsm(d2f, TMP)
        self.cp(DEST[:, tile, 0:1].k(tile), d1f)
        self.cp(DEST[:, tile, 1:2].k(tile), d2f)
        xg = E["xg_d"]
        self.scatter(V(xg.ap, [kk + (tile, 0) for kk in xg.keys]), DEST[:, tile, 0:1].k(tile), H2b)
        self.scatter(V(xg.ap, [kk + (tile, 1) for kk in xg.keys]), DEST[:, tile, 1:2].k(tile), H2b)

    def experts(self, E):
        A = self.A
        CAP, NB, L = self.CAP, self.NB, self.L
        NT = NB * L // 128
        ident_b = E["ident_b"]
        xg, yg = E["xg_d"], E["yg_d"]
        xg_all = V(xg.ap, [kk + (t, j) for kk in xg.keys for t in range(NT) for j in range(2)])
        nst = CAP // 128
        Wg = Ring([A.alloc("Wg%d" % i, (8, D), BF16) for i in range(2)])
        Wu = Ring([A.alloc("Wu%d" % i, (8, D), BF16) for i in range(2)])
        Wd = Ring([A.alloc("Wd%d" % i, (8, D), BF16) for i in range(2)])
        Xr = Ring([A.alloc("Xr%d" % i, D, BF16) for i in range(3)])
        XT = Ring([A.alloc("XTe%d" % i, (8, CAP), BF16) for i in range(2)])
        AT = Ring([A.alloc("ATe%d" % i, (8, CAP), BF16) for i in range(2)])
        SGr = Ring([A.alloc("SGe%d" % i, 512) for i in range(3)])
        Yr = Ring([A.alloc("Ye%d" % i, D) for i in range(3)])
        segs = []
        o = 0
        while o < CAP:
            n = min(512, CAP - o)
            segs.append((o, n))
            o += n
        for e in range(NEXP):
            wg, wu, wd = Wg.next(), Wu.next(), Wd.next()
            for (dst, src) in ((wg, E["weg_d"]), (wu, E["weu_d"]), (wd, E["wed_d"])):
                sv = src[e].re("(k p) n -> p k n", p=128)
                for k in range(8):
                    self.dma(dst[:, k, :].k(k), sv[:, k, :], eng="gpsimd")
            xT = XT.next()
            for s_ in range(nst):
                xr = Xr.next()
                self.dma(xr, V(xg_all.ap[e * CAP + s_ * 128:e * CAP + (s_ + 1) * 128, :], xg_all.keys))
                for half in range(2):
                    ps = self.bank()
                    for kk in range(4):
                        k = half * 4 + kk
                        self.mm(ps[:, kk * 128:(kk + 1) * 128], xr[:, k * 128:(k + 1) * 128], ident_b)
                    self.evac(xT[:, half * 4:(half + 1) * 4, s_ * 128:(s_ + 1) * 128].k(half, s_),
                              ps.re("p (a b) -> p a b", b=128))
            xTa = V(xT.ap, [kk + (hh, ss) for kk in xT.keys for hh in range(2) for ss in range(nst)])
            aT = AT.next()
            for j in range(8):
                for (o, n) in segs:
                    psg = self.bank()
                    for k in range(8):
                        self.mm(psg[:, 0:n], wg[:, k, j * 128:(j + 1) * 128].k(k), xTa[:, k, o:o + n], start=(k == 0), stop=(k == 7))
                    psu = self.bank()
                    for k in range(8):
                        self.mm(psu[:, 0:n], wu[:, k, j * 128:(j + 1) * 128].k(k), xTa[:, k, o:o + n], start=(k == 0), stop=(k == 7))
                    sg_ = SGr.next()
                    self.act(sg_[:, 0:n], psg[:, 0:n], AF.Silu)
                    self.tt(aT[:, j, o:o + n].k(j, o), sg_[:, 0:n], psu[:, 0:n], ALU.mult)
            aTa = V(aT.ap, [kk + (j, o) for kk in aT.keys for j in range(8) for (o, n) in segs])
            for s_ in range(nst):
                y = Yr.next()
                for half in range(2):
                    ps = self.bank()
                    for j in range(8):
                        self.mm(ps, aTa[:, j, s_ * 128:(s_ + 1) * 128], wd[:, j, half * 512:(half + 1) * 512].k(j),
                                start=(j == 0), stop=(j == 7))
                    self.evac(y[:, half * 512:(half + 1) * 512].k(half), ps)
                ya = V(y.ap, [kk + (hh,) for kk in y.keys for hh in range(2)])
                r0 = e * CAP + s_ * 128
                self.dma(V(yg.ap[r0:r0 + 128, :], [kk + (e, s_) for kk in yg.keys]), ya)

    def combine(self, E):
        A = self.A
        CAP, NB, L = self.CAP, self.NB, self.L
        NT = NB * L // 128
        nst = CAP // 128
        DEST, WT = E["DEST"], E["WT"]
        yg, x1 = E["yg_d"], E["x1_d"]
        yg_all = V(yg.ap, [kk + (e, s_) for kk in yg.keys for e in range(NEXP) for s_ in range(nst)])
        ln2g = A.alloc("ln2g", D)
        ln2b = A.alloc("ln2b", D)
        self.dma(ln2g, E["lnbc_d"][:, 2, :])
        self.dma(ln2b, E["lnbc_d"][:, 3, :])
        g2bc = [A.alloc("g2bc%d" % b, D) for b in range(NB)]
        bcs = E["bcs_d"]
        for b in range(NB):
            self.dma(g2bc[b], V(bcs.ap[b, 3], [kk + (b, ct) for kk in bcs.keys for ct in range(8)]))
        Y1r = Ring([A.alloc("Y1g%d" % i, D) for i in range(2)])
        Y2r = Ring([A.alloc("Y2g%d" % i, D) for i in range(2)])
        X1r = Ring([A.alloc("X1c%d" % i, D) for i in range(2)])
        Or = Ring([A.alloc("Oc%d" % i, D) for i in range(2)])
        for tile in range(NT):
            b = tile * 128 // L
            Y1, Y2, X1, O = Y1r.next(), Y2r.next(), X1r.next(), Or.next()
            self.gather(Y1, yg_all, DEST[:, tile, 0:1].k(tile))
            self.gather(Y2, yg_all, DEST[:, tile, 1:2].k(tile))
            self.dma(X1, V(x1.ap[tile * 128:(tile + 1) * 128, :], [kk + (tile,) for kk in x1.keys]))
            self.ts(Y1, Y1, WT[:, tile, 0:1].k(tile), None, ALU.mult, eng="gpsimd")
            self.stt(Y1, Y2, WT[:, tile, 1:2].k(tile), Y1, ALU.mult, ALU.add)
            self.tt(Y1, Y1, g2bc[b], ALU.mult, eng="gpsimd")
            self.stt(Y1, X1, ALPHA, Y1, ALU.mult, ALU.add)
            mean, rstd = self.ln_stats(Y1)
            self.ts(Y1, Y1, mean, rstd, ALU.subtract, ALU.mult)
            self.tt(O, Y1, ln2g, ALU.mult, eng="gpsimd")
            self.tt(O, O, ln2b, ALU.add)
            self.dma(V(E["out_d"].ap[tile * 128:(tile + 1) * 128, :], [("out", tile)]), O, is_output=True)


def chunk_starts():
    st = [n * 128 for n in range(8)]
    st += [1024 + n * 128 for n in range(8)]
    st += [2048 + n * 128 for n in range(24)]
    st += [5152 + n * 128 for n in range(8)]
    st += [6176 + n * 128 for n in range(16)]
    return st


def host_layout(inp, NB, core, NBP):
    f = lambda a: np.ascontiguousarray(a, dtype=np.float32)
    w_in = inp["w_in"][0]
    b_in = inp["b_in"][0]
    st = chunk_starts()
    w_in_t = np.stack([w_in[:, s:s + 128].reshape(8, 128, 128).transpose(1, 0, 2) for s in st], 0)
    w_in_lg = w_in[:, 5120:5152].reshape(8, 128, 32).transpose(1, 0, 2)
    b_inT = np.zeros((128, 65), np.float32)
    for i, s in enumerate(st):
        b_inT[:, i] = b_in[s:s + 128]
    b_inT[0:32, 64] = b_in[5120:5152]
    bs = slice(core * NB, (core + 1) * NB)
    c = inp["c"][bs]
    cT = np.zeros((128, 8, NBP), np.float32)
    for b in range(NB):
        cT[:, :, b] = c[b].reshape(8, 128).T
    cT[:, :, NB] = inp["c_ctx"].reshape(8, 128).T
    b_mod = inp["b_mod"][0]
    fm = lambda v: v.reshape(-1, 128).T
    rep = lambda v: np.broadcast_to(v[None, :], (128, v.shape[0]))
    lw = np.stack([inp["lru_wa"][0, 0], inp["lru_wx"][0, 0], inp["lru_wa"][0, 1], inp["lru_wx"][0, 1]], 0)
    lb = np.stack([inp["lru_ba"][0, 0], inp["lru_bx"][0, 0], inp["lru_ba"][0, 1], inp["lru_bx"][0, 1]], 0)
    conv_a = np.concatenate([inp["conv_a_w"][0], inp["conv_a_b"][0][None]], 0)
    d = {
        "x": inp["x"][bs], "ctx": inp["ctx"][bs], "cT": cT,
        "w_mod": inp["w_mod"][0], "bmodT": fm(b_mod[0:2048]), "bmod_bc": rep(b_mod[2048:]),
        "w_in_t": w_in_t, "w_in_lg": w_in_lg, "b_inT": b_inT,
        "conv_a": conv_a.reshape(5, 8, 128).transpose(2, 1, 0),
        "lru_w": lw.transpose(2, 0, 1, 3),
        "lru_b": lb.reshape(4, 8, 128).transpose(2, 0, 1),
        "lru_lam": inp["lru_lambda"][0].reshape(2, 8, 128).transpose(2, 0, 1),
        "conv_qkv": inp["conv_qkv_w"][0].reshape(4, 24, 128).transpose(2, 1, 0),
        "gdn_rows": rep(np.concatenate([inp["gdn_dt_bias"][0].reshape(16), inp["gdn_a_log"][0].reshape(16)])),
        "gdn_nw": inp["gdn_norm_w"][0].reshape(128, 1),
        "w_pa": inp["w_pa"][0], "w_pb": inp["w_pb"][0], "w_out": inp["w_out"][0],
        "ln_bc": np.stack([rep(inp["ln1_g"][0]), rep(inp["ln1_b"][0]), rep(inp["ln2_g"][0]), rep(inp["ln2_b"][0])], 1),
        "w_rt": np.concatenate([inp["w_router_g"][0], inp["w_router_e"][0]], 1).reshape(8, 128, 36).transpose(1, 0, 2),
        "b_rt_bc": rep(np.concatenate([inp["b_router_g"][0], inp["b_router_e"][0]])),
        "w_eg": inp["w_e_gate"][0], "w_eu": inp["w_e_up"][0], "w_ed": inp["w_e_down"][0],
    }
    return {k: f(v) for k, v in d.items()}


_CACHE = {}


def run(inputs, n_cores, NB, L, CTX, CAP, debug=()):
    key = (NB, L, CTX, CAP, tuple(debug))
    if key not in _CACHE:
        _CACHE[key] = KB(NB, L, CTX, CAP, debug).build()
    nc = _CACHE[key]
    NBP = NB + 1 + ((NB + 1) % 2)
    inp = {k: np.asarray(v) for k, v in inputs.items()}
    shared = None
    in_maps = []
    for core in range(n_cores):
        m = host_layout(inp, NB, core, NBP) if shared is None else None
        if shared is None:
            shared = m
        else:
            m = dict(shared)
            bs = slice(core * NB, (core + 1) * NB)
            m["x"] = np.ascontiguousarray(inp["x"][bs], dtype=np.float32)
            m["ctx"] = np.ascontiguousarray(inp["ctx"][bs], dtype=np.float32)
            c = inp["c"][bs]
            cT = shared["cT"].copy()
            for b in range(NB):
                cT[:, :, b] = c[b].reshape(8, 128).T
            m["cT"] = cT
        in_maps.append(m)
    res = run_bass_kernel_spmd(nc, in_maps, core_ids=list(range(n_cores)))
    return res


def kernel(**inputs):
    n_cores, NB, L, CTX, CAP = 8, 2, 4096, 256, 640
    res = run(inputs, n_cores, NB, L, CTX, CAP)
    outs = [r["out"].reshape(NB, L, D) for r in res.results]
    return np.concatenate(outs, 0).astype(np.float32)
```

```python
import math
from contextlib import ExitStack
import numpy as np
import concourse.bass as bass
import concourse.mybir as mybir
from concourse.bass_utils import run_bass_kernel_spmd

F32 = mybir.dt.float32
F32R = mybir.dt.float32r
BF16 = mybir.dt.bfloat16
I32 = mybir.dt.int32
AF = mybir.ActivationFunctionType
ALU = mybir.AluOpType
AX = mybir.AxisListType

ENGS = ("sync", "scalar", "gpsimd", "vector", "tensor")
NDMA_SEM = 16
SAME_ENGINE_SYNC = True

D = 1024
GW = 64
ALPHA = 2.0 ** 0.25
LN_EPS = 1e-6
NEXP = 32
HI_LVL = 99


class Prog:
    def __init__(self, nc, stack):
        self.nc = nc
        self.q = {e: [] for e in ENGS}
        self.cnt = {e: 0 for e in ENGS}
        self.esem = {e: stack.enter_context(nc.semaphore("es_" + e)) for e in ENGS}
        self.dsem, self.dcnt, self.dnext = {}, {}, {}
        for e in ("sync", "gpsimd"):
            self.dsem[e] = [stack.enter_context(nc.semaphore("ds_%s%d" % (e, i))) for i in range(NDMA_SEM)]
            self.dcnt[e] = [0] * NDMA_SEM
            self.dnext[e] = 0
        self.semobj = {}
        for e in ENGS:
            self.semobj[("e", e)] = self.esem[e]
        for e in self.dsem:
            for i, s in enumerate(self.dsem[e]):
                self.semobj[("d", e, i)] = s
        self.seen = {e: {} for e in ENGS}
        self.last_w = {}
        self.readers = {}
        self.out_tokens = []

    def _deps(self, eng, reads, writes):
        toks = []
        for k in reads:
            t = self.last_w.get(k)
            if t is not None:
                toks.append(t)
        for k in writes:
            t = self.last_w.get(k)
            if t is not None:
                toks.append(t)
            toks.extend(self.readers.get(k, ()))
        need = {}
        for (sk, v) in toks:
            if sk == ("e", eng) and (eng == "tensor" or not SAME_ENGINE_SYNC):
                continue
            if self.seen[eng].get(sk, 0) >= v:
                continue
            if need.get(sk, 0) < v:
                need[sk] = v
        for sk, v in need.items():
            self.seen[eng][sk] = v
        return list(need.items())

    def _commit(self, tok, reads, writes):
        for k in reads:
            if k in writes:
                continue
            self.readers.setdefault(k, []).append(tok)
        for k in writes:
            self.last_w[k] = tok
            self.readers[k] = []

    def op(self, eng, fn, reads=(), writes=()):
        psr = [k for k in reads if k[0] == "psb"]
        if psr:
            writes = list(writes) + psr
        waits = self._deps(eng, reads, writes)
        self.cnt[eng] += 1
        tok = (("e", eng), self.cnt[eng])
        self.q[eng].append((waits, fn, self.esem[eng], 1))
        self._commit(tok, reads, writes)
        return tok

    def dma(self, eng, fn, reads=(), writes=(), is_output=False):
        i = self.dnext[eng]
        self.dnext[eng] = (i + 1) % NDMA_SEM
        sk = ("d", eng, i)
        waits = self._deps(eng, reads, writes)
        prev = self.dcnt[eng][i]
        if prev > 0 and self.seen[eng].get(sk, 0) < prev:
            self.seen[eng][sk] = prev
            waits.append((sk, prev))
        self.dcnt[eng][i] += 16
        tok = (sk, self.dcnt[eng][i])
        self.q[eng].append((waits, fn, self.dsem[eng][i], 16))
        self._commit(tok, reads, writes)
        if is_output:
            self.out_tokens.append(tok)
        return tok

    def barrier(self):
        toks = [(("e", e), self.cnt[e]) for e in ENGS if self.cnt[e] > 0]
        for e in self.dsem:
            for i in range(NDMA_SEM):
                if self.dcnt[e][i] > 0:
                    toks.append((("d", e, i), self.dcnt[e][i]))
        for eng in ENGS:
            waits = []
            for (sk, v) in toks:
                if sk == ("e", eng) and eng == "tensor":
                    continue
                if self.seen[eng].get(sk, 0) >= v:
                    continue
                self.seen[eng][sk] = v
                waits.append((sk, v))
            if waits:
                self.q[eng].append((waits, None, None, 0))
        self.last_w = {}
        self.readers = {}

    def finish(self):
        self.q["sync"].append((list(self.out_tokens), None, None, 0))

    def emit(self, block):
        def run(engname):
            def body(eng):
                for (waits, fn, sem, inc) in self.q[engname]:
                    for (sk, v) in waits:
                        eng.wait_ge(self.semobj[sk], v)
                    if fn is not None:
                        fn(eng).then_inc(sem, inc)
            return body
        block.sync(run("sync"))
        block.scalar(run("scalar"))
        block.gpsimd(run("gpsimd"))
        block.vector(run("vector"))
        block.tensor(run("tensor"))


class V:
    __slots__ = ("ap", "keys")

    def __init__(self, ap, keys):
        self.ap = ap
        self.keys = tuple(keys)

    def __getitem__(self, idx):
        return V(self.ap[idx], self.keys)

    def re(self, pat, **kw):
        return V(self.ap.rearrange(pat, **kw), self.keys)

    def bc(self, shape):
        return V(self.ap.to_broadcast(list(shape)), self.keys)

    def k(self, *sub):
        return V(self.ap, [kk + tuple(sub) for kk in self.keys])

    def cast(self, dt):
        return V(self.ap.bitcast(dt), self.keys)


def _keys(*vs):
    out = []
    for v in vs:
        if isinstance(v, V):
            out.extend(v.keys)
    return out


def _a(v):
    return v.ap if isinstance(v, V) else v


DT_SIZE = {F32: 4, BF16: 2, I32: 4}


class Arena:
    def __init__(self, nc, stack, nbytes):
        self.words = nbytes // 4
        self.t = stack.enter_context(nc.sbuf_tensor("arena", [128, self.words], F32))
        self.off = 0
        self.uid = 0

    def alloc(self, name, free_shape, dt=F32):
        if isinstance(free_shape, int):
            free_shape = (free_shape,)
        nel = 1
        for s in free_shape:
            nel *= s
        words = (nel * DT_SIZE[dt] + 31) // 32 * 8
        assert self.off + words <= self.words, "SBUF arena overflow at %s (%d + %d > %d)" % (name, self.off, words, self.words)
        ap = self.t[:, self.off:self.off + words]
        if dt != F32:
            ap = ap.bitcast(dt)
        ap = ap[:, 0:nel]
        if len(free_shape) == 2:
            ap = ap.rearrange("p (a b) -> p a b", b=free_shape[1])
        elif len(free_shape) == 3:
            ap = ap.rearrange("p (a b c) -> p a b c", b=free_shape[1], c=free_shape[2])
        elif len(free_shape) == 4:
            ap = ap.rearrange("p (a b c d) -> p a b c d", b=free_shape[1], c=free_shape[2], d=free_shape[3])
        self.off += words
        self.uid += 1
        return V(ap, [(name, self.uid)])

    def mark(self):
        return self.off

    def reset(self, m):
        self.off = m


class Ring:
    def __init__(self, items):
        self.items = items
        self.i = 0

    def next(self):
        v = self.items[self.i % len(self.items)]
        self.i += 1
        return v


class KB:
    def __init__(self, NB, L, CTX, CAP, debug=()):
        self.NB, self.L, self.CTX, self.CAP = NB, L, CTX, CAP
        self.R = L // GW
        self.debug = set(debug)
        self.nc = bass.Bass("TRN2", target_bir_lowering=False)
        self.alt = 0

    def din(self, name, shape, dt=F32):
        return V(self.nc.dram_tensor(name, list(shape), dt, kind="ExternalInput").ap(), [])

    def dscr(self, name, shape, dt):
        kind = "ExternalOutput" if name in self.debug else "Internal"
        return V(self.nc.dram_tensor(name, list(shape), dt, kind=kind).ap(), [(name,)])

    def mm(self, out, lhsT, rhs, start=True, stop=True):
        o, l, r = out.ap, lhsT.ap, rhs.ap
        self.P.op("tensor", lambda e: e.matmul(o, lhsT=l, rhs=r, start=start, stop=stop),
                  reads=_keys(lhsT, rhs), writes=_keys(out))

    def mmr(self, out, lhsT, rhs, start=True, stop=True):
        o, l, r = out.ap, lhsT.ap, rhs.ap
        self.P.op("tensor", lambda e: e.matmul(o, lhsT=l, rhs=r, start=start, stop=stop),
                  reads=_keys(lhsT, rhs), writes=_keys(out))

    def tr(self, out, in_, ident):
        o, i, d = out.ap, in_.ap, ident.ap
        self.P.op("tensor", lambda e: e.transpose(o, i, d), reads=_keys(in_, ident), writes=_keys(out))

    def act(self, out, in_, func, bias=0.0, scale=1.0, eng="scalar"):
        o, i, b, s = out.ap, in_.ap, _a(bias), _a(scale)
        self.P.op("scalar", lambda e: e.activation(out=o, in_=i, func=func, bias=b, scale=s),
                  reads=_keys(in_, bias, scale), writes=_keys(out))

    def ts(self, out, in0, s1, s2, op0, op1=None, eng="vector"):
        o, i, a, b = out.ap, in0.ap, _a(s1), _a(s2)
        if op1 is None:
            fn = lambda e: e.tensor_scalar(out=o, in0=i, scalar1=a, scalar2=None, op0=op0)
        else:
            fn = lambda e: e.tensor_scalar(out=o, in0=i, scalar1=a, scalar2=b, op0=op0, op1=op1)
        self.P.op(eng, fn, reads=_keys(in0, s1, s2), writes=_keys(out))

    def tt(self, out, in0, in1, op, eng="vector"):
        o, a, b = out.ap, in0.ap, in1.ap
        self.P.op(eng, lambda e: e.tensor_tensor(out=o, in0=a, in1=b, op=op), reads=_keys(in0, in1), writes=_keys(out))

    def stt(self, out, in0, scalar, in1, op0, op1):
        o, a, s, b = out.ap, in0.ap, _a(scalar), in1.ap
        self.P.op("vector", lambda e: e.scalar_tensor_tensor(out=o, in0=a, scalar=s, in1=b, op0=op0, op1=op1),
                  reads=_keys(in0, scalar, in1), writes=_keys(out))

    def cp(self, out, in_, eng="vector"):
        o, i = out.ap, in_.ap
        if eng == "scalar":
            self.P.op("scalar", lambda e: e.activation(out=o, in_=i, func=AF.Identity), reads=_keys(in_), writes=_keys(out))
        else:
            self.P.op(eng, lambda e: e.tensor_copy(out=o, in_=i), reads=_keys(in_), writes=_keys(out))

    def evac(self, out, in_):
        self.alt ^= 1
        self.cp(out, in_, eng="scalar" if self.alt else "vector")

    def memset(self, out, val, eng="gpsimd"):
        o = out.ap
        self.P.op(eng, lambda e: e.memset(o, val), writes=_keys(out))

    def scan(self, out, d0, d1, init):
        o, a, b, i = out.ap, d0.ap, d1.ap, _a(init)
        self.P.op("vector", lambda e: e.tensor_tensor_scan(out=o, data0=a, data1=b, initial=i, op0=ALU.mult, op1=ALU.add),
                  reads=_keys(d0, d1, init), writes=_keys(out))

    def dma(self, out, in_, eng="sync", is_output=False):
        o, i = out.ap, in_.ap
        if eng == "gpsimd":
            fn = lambda e: e.dma_start(out=o, in_=i, max_dma_last_dim=4096)
        else:
            fn = lambda e: e.dma_start(out=o, in_=i)
        self.P.dma(eng, fn, reads=_keys(in_), writes=_keys(out), is_output=is_output)

    def scatter(self, out_dram, idx, in_sb):
        o, x, i = out_dram.ap, idx.ap, in_sb.ap
        self.P.dma("gpsimd", lambda e: e.indirect_dma_start(out=o, out_offset=bass.IndirectOffsetOnAxis(ap=x, axis=0),
                                                            in_=i, in_offset=None),
                   reads=_keys(idx, in_sb), writes=_keys(out_dram))

    def gather(self, out_sb, in_dram, idx):
        o, x, i = out_sb.ap, idx.ap, in_dram.ap
        self.P.dma("gpsimd", lambda e: e.indirect_dma_start(out=o, out_offset=None, in_=i,
                                                            in_offset=bass.IndirectOffsetOnAxis(ap=x, axis=0)),
                   reads=_keys(idx, in_dram), writes=_keys(out_sb))

    def bank(self):
        return self.banks.next()

    def ln_stats(self, x):
        st = self.st_ring.next()
        mv = st[:, 12:14]
        self.P.op("vector", (lambda o, i: lambda e: e.bn_stats(out=o, in_=i))(st.ap[:, 0:6], x.ap[:, 0:512]),
                  reads=_keys(x), writes=_keys(st))
        self.P.op("vector", (lambda o, i: lambda e: e.bn_stats(out=o, in_=i))(st.ap[:, 6:12], x.ap[:, 512:1024]),
                  reads=_keys(x, st), writes=_keys(st))
        self.P.op("vector", (lambda o, i: lambda e: e.bn_aggr(out=o, in_=i))(mv.ap, st.ap[:, 0:12]),
                  reads=_keys(st), writes=_keys(st))
        self.act(st[:, 14:15], st[:, 13:14], AF.Sqrt, bias=LN_EPS)
        self.recip(st[:, 15:16], st[:, 14:15])
        return st[:, 12:13], st[:, 15:16]

    def recip(self, out, in_):
        o, i = out.ap, in_.ap
        self.P.op("vector", lambda e: e.reciprocal(out=o, in_=i), reads=_keys(in_), writes=_keys(out))

    def build(self):
        NB, L, CTX, CAP, R = self.NB, self.L, self.CTX, self.CAP, self.R
        nc = self.nc
        NBP = NB + 1 + ((NB + 1) % 2)
        NT = NB * L // 128
        x_d = self.din("x", [NB, L, D])
        ctx_d = self.din("ctx", [NB, CTX, D])
        cT_d = self.din("cT", [128, 8, NBP])
        wmod_d = self.din("w_mod", [D, 6 * D])
        bmodT_d = self.din("bmodT", [128, 16])
        bmodbc_d = self.din("bmod_bc", [128, 4 * D])
        wint_d = self.din("w_in_t", [64, 128, 8, 128])
        winlg_d = self.din("w_in_lg", [128, 8, 32])
        binT_d = self.din("b_inT", [128, 65])
        conva_d = self.din("conv_a", [128, 8, 5])
        lruw_d = self.din("lru_w", [128, 4, 8, 128])
        lrub_d = self.din("lru_b", [128, 4, 8])
        lrulam_d = self.din("lru_lam", [128, 2, 8])
        convq_d = self.din("conv_qkv", [128, 24, 4])
        gdnrows_d = self.din("gdn_rows", [128, 32])
        gdnnw_d = self.din("gdn_nw", [128, 1])
        wpa_d = self.din("w_pa", [D, D])
        wpb_d = self.din("w_pb", [D, D])
        wout_d = self.din("w_out", [D, D])
        lnbc_d = self.din("ln_bc", [128, 4, D])
        wrt_d = self.din("w_rt", [128, 8, 36])
        brt_d = self.din("b_rt_bc", [128, 36])
        weg_d = self.din("w_eg", [NEXP, D, D])
        weu_d = self.din("w_eu", [NEXP, D, D])
        wed_d = self.din("w_ed", [NEXP, D, D])
        out_d = V(nc.dram_tensor("out", [NB * L, D], F32, kind="ExternalOutput").ap(), [("out",)])
        yaT_d = self.dscr("yaT", [NB, 8, 128, L], BF16)
        ybT_d = self.dscr("ybT", [NB, 8, 128, L], BF16)
        sg_d = self.dscr("sg", [NB, 16, 128, L], F32)
        x1_d = self.dscr("x1s", [NB * L, D], F32)
        xg_d = self.dscr("xg", [NEXP * CAP, D], BF16)
        yg_d = self.dscr("yg", [NEXP * CAP, D], F32)
        bcs_d = self.dscr("bcs", [NB, 4, 128, D], F32)
        pq_d = self.dscr("pq", [32, 128, L], F32)
        dbg_d = {}

        st = ExitStack()
        with st:
            self.P = P = Prog(nc, st)
            cap_bytes = 194 * 1024
            self.A = A = Arena(nc, st, cap_bytes)
            banks = []
            for i in range(8):
                t = st.enter_context(nc.psum_tensor("psb%d" % i, [128, 512], F32))
                banks.append(V(t[:, :], [("psb", i)]))
            self.banks = Ring(banks)
            self.r32 = []
            for i in range(4):
                d_ = {}
                for nm in ("N", "M", "X"):
                    t = st.enter_context(nc.sbuf_tensor("r32_%s%d" % (nm, i), [128, 2 * 128], F32))
                    d_[nm] = [V(t[:, j * 128:(j + 1) * 128], [("r32", nm, i, j)]) for j in range(2)]
                self.r32.append(d_)

            ident_f = A.alloc("ident_f", 128)
            ones_f = A.alloc("ones_f", 128)
            mIL = A.alloc("mIL", 128)
            mSL = A.alloc("mSL", 128)
            mIU = A.alloc("mIU", 128)
            mSU = A.alloc("mSU", 128)
            ident_b = A.alloc("ident_b", 128, BF16)
            ones_b = A.alloc("ones_b", 128, BF16)
            mSU_b = A.alloc("mSU_b", 128, BF16)
            self.st_ring = Ring([A.alloc("lnst%d" % i, 16) for i in range(4)])

            def mask(dst, pat_step, cm, op):
                self.memset(dst, 1.0)
                o = dst.ap
                self.P.op("gpsimd", lambda e: e.affine_select(out=o, in_=o, pattern=[[pat_step, 128]], base=0,
                                                               channel_multiplier=cm, compare_op=op, fill=0.0),
                          reads=_keys(dst), writes=_keys(dst))
            self.memset(ones_f, 1.0)
            mask(ident_f, -1, 1, ALU.is_equal)
            mask(mIL, -1, 1, ALU.is_ge)
            mask(mSL, -1, 1, ALU.is_gt)
            mask(mIU, 1, -1, ALU.is_ge)
            mask(mSU, 1, -1, ALU.is_gt)
            self.cp(ident_b, ident_f, eng="gpsimd")
            self.cp(ones_b, ones_f, eng="gpsimd")
            self.cp(mSU_b, mSU, eng="gpsimd")

            modT = A.alloc("modT", (16, NBP))
            binT = A.alloc("binT", 65)
            conva = A.alloc("conva", (8, 5))
            lrub = A.alloc("lrub", (4, 8))
            cch = A.alloc("cch", (2, 8))
            convq = A.alloc("convq", (24, 4))
            gdnrows = A.alloc("gdnrows", 32)
            nega = A.alloc("nega", 16)
            gdnnw = A.alloc("gdnnw", 1)
            lruw = A.alloc("lruw", (4, 8, 128), BF16)
            lru_state = A.alloc("lru_state", (8, 2))
            gdn_state = A.alloc("gdn_state", (8, 2, 128))
            DEST = A.alloc("DEST", (NT, 2), I32)
            WT = A.alloc("WT", (NT, 2))
            CNT = A.alloc("CNT", 32)
            ECAP = A.alloc("ECAP", 32)
            brt = A.alloc("brt", 36)
            for (dst, src) in ((binT, binT_d), (conva, conva_d), (lrub, lrub_d), (cch, lrulam_d), (convq, convq_d),
                               (gdnrows, gdnrows_d), (gdnnw, gdnnw_d), (brt, brt_d)):
                self.dma(dst, src)
            self.dma(lruw, lruw_d, eng="gpsimd")
            self.act(cch, cch, AF.Exp, scale=-1.0)
            self.act(cch, cch, AF.Ln, bias=1.0)
            self.ts(cch, cch, -8.0, None, ALU.mult)
            self.act(nega, gdnrows[:, 16:32], AF.Exp)
            self.ts(nega, nega, -1.0, None, ALU.mult)
            self.memset(CNT, 0.0)
            ec = ECAP.ap
            self.P.op("gpsimd", lambda e: e.iota(ec, pattern=[[CAP, 32]], base=0, channel_multiplier=0,
                                                 allow_small_or_imprecise_dtypes=True), writes=_keys(ECAP))
            zt = A.alloc("zt", D, BF16)
            self.memset(zt, 0.0)
            for e_ in range(NEXP):
                self.dma(V(xg_d.ap[e_ * CAP:(e_ + 1) * CAP, :].rearrange("(n p) d -> p n d", p=128), xg_d.keys),
                         V(zt.ap.unsqueeze(1).to_broadcast([128, CAP // 128, D]), zt.keys))
            base_mark = A.mark()

            cT = A.alloc("cT", (8, NBP))
            self.dma(cT, cT_d)
            sT = A.alloc("sT", (8, NBP))
            self.act(sT, cT, AF.Silu)
            bmodT = A.alloc("bmodT", 16)
            self.dma(bmodT, bmodT_d)
            wmA = A.alloc("wmA", (8, 2048))
            wm_v = wmod_d.re("(k p) n -> p k n", p=128)
            for k in range(8):
                self.dma(wmA[:, k, :].k(k), wm_v[:, k, 0:2048])
            for cc in range(16):
                ps = self.bank()
                for k in range(8):
                    self.mm(ps[:, 0:NBP], wmA[:, k, cc * 128:(cc + 1) * 128].k(k), sT[:, k, :], start=(k == 0), stop=(k == 7))
                self.ts(modT[:, cc, :], ps[:, 0:NBP], bmodT[:, cc:cc + 1], 1.0 if cc >= 8 else 0.0, ALU.add, ALU.add)
            A.reset(A.mark() - 0)
            sbc = [A.alloc("sbc%d" % b, (8, 128)) for b in range(NB)]
            for b in range(NB):
                self.cp(sbc[b], sT[:, :, b:b + 1].bc([128, 8, 128]))
            wmB = Ring([A.alloc("wmB%d" % i, (8, 512)) for i in range(2)])
            bmB = Ring([A.alloc("bmB%d" % i, 512) for i in range(2)])
            stg = Ring([A.alloc("stg%d" % i, 512) for i in range(3)])
            for ct in range(8):
                w = wmB.next()
                bm = bmB.next()
                for k in range(8):
                    self.dma(w[:, k, :].k(k), wm_v[:, k, 2048 + ct * 512:2048 + (ct + 1) * 512])
                self.dma(bm, bmodbc_d[:, ct * 512:(ct + 1) * 512])
                for b in range(NB):
                    ps = self.bank()
                    for k in range(8):
                        self.mm(ps, sbc[b][:, k, :], w[:, k, :].k(k), start=(k == 0), stop=(k == 7))
                    sg_ = stg.next()
                    if ct // 2 == 2:
                        self.stt(sg_, ps, 1.0, bm, ALU.add, ALU.add)
                    else:
                        self.tt(sg_, ps, bm, ALU.add)
                    self.dma(bcs_d[b, ct // 2, :, (ct % 2) * 512:(ct % 2 + 1) * 512].k(b, ct), sg_)
            self.P.barrier()
            A.reset(base_mark)

            import os as _os
            KSTOP = int(_os.environ.get("KSTOP", "99"))
            for b in range(NB if KSTOP > 0 else 0):
                for which in ("ctx", "lat"):
                    if KSTOP == 1 and which == "lat":
                        continue
                    tabs, tab_mark = self.mixer(b, which, locals())
                    self.P.barrier()
                    A.reset(tab_mark)
                    if KSTOP > 2:
                        self.gdn(b, which == "lat", tabs, locals())
                    self.P.barrier()
                    A.reset(base_mark)
                if KSTOP > 3:
                    self.merge(b, locals())
                self.P.barrier()
                A.reset(base_mark)
            if KSTOP > 4:
                self.experts(locals())
            self.P.barrier()
            A.reset(base_mark)
            if KSTOP > 5:
                self.combine(locals())
            P.finish()
            with nc.Block() as block:
                P.emit(block)
        return nc

    def load_wchunk(self, wring, wint_d, c):
        w = wring.next()
        self.dma(w, wint_d[c], eng="gpsimd")
        return w

    def proj(self, w, M, hT, Lx, evac_fn):
        TL = min(512, Lx)
        for t in range(Lx // TL):
            ps = self.bank()
            for k in range(8):
                self.mm(ps[0:M, 0:TL], w[:, k, 0:M], hT[:, k, t * TL:(t + 1) * TL], start=(k == 0), stop=(k == 7))
            evac_fn(t, TL, ps[0:M, 0:TL])

    def mixer(self, b, which, E):
        A, P = self.A, self.P
        NB, L, CTX, R = self.NB, self.L, self.CTX, self.R
        lat = which == "lat"
        Lx = L if lat else CTX
        nch = Lx // 128
        NH = nch * 8
        col = b if lat else NB
        src = E["x_d"][b] if lat else E["ctx_d"][b]
        modT, binT, ident_f, ident_b, ones_f, ones_b = E["modT"], E["binT"], E["ident_f"], E["ident_b"], E["ones_f"], E["ones_b"]
        mIL, mIU = E["mIL"], E["mIU"]
        gdnrows, nega = E["gdnrows"], E["nega"]
        wint_d = E["wint_d"]
        tabs = {}
        for nm in ("BT", "NBT", "GC", "EKG", "GL", "BEG"):
            tabs[nm] = A.alloc(nm, (2, nch, 8))
        tab_mark = A.mark()
        hT = A.alloc("hT", (8, Lx), BF16)
        m0 = A.mark()
        xin = Ring([A.alloc("xin%d" % i, D) for i in range(2)])
        xnr = Ring([A.alloc("xn%d" % i, D) for i in range(2)])
        for t in range(Lx // 128):
            xt = xin.next()
            self.dma(xt, src[t * 128:(t + 1) * 128, :])
            mean, rstd = self.ln_stats(xt)
            xn = xnr.next()
            self.ts(xn, xt, mean, rstd, ALU.subtract, ALU.mult)
            for half in range(2):
                ps = self.bank()
                for kk in range(4):
                    k = half * 4 + kk
                    self.tr(ps[:, kk * 128:(kk + 1) * 128], xn[:, k * 128:(k + 1) * 128], ident_f)
                for kk in range(4):
                    k = half * 4 + kk
                    o = hT[:, k, t * 128:(t + 1) * 128].k(t)
                    if kk % 2 == 0:
                        self.act(o, ps[:, kk * 128:(kk + 1) * 128], AF.Identity, bias=modT[:, k, col:col + 1], scale=modT[:, 8 + k, col:col + 1])
                    else:
                        self.ts(o, ps[:, kk * 128:(kk + 1) * 128], modT[:, 8 + k, col:col + 1], modT[:, k, col:col + 1], ALU.mult, ALU.add)
        hT = V(hT.ap, [kk + (t,) for kk in hT.keys for t in range(Lx // 128)])
        self.P.barrier()
        A.reset(m0)
        wring = Ring([A.alloc("wch%d" % i, (8, 128), BF16) for i in range(2)])
        m1 = A.mark()

        conva, lrub, cch, lruw, lru_state = E["conva"], E["lrub"], E["cch"], E["lruw"], E["lru_state"]
        xa_pad = A.alloc("xa_pad", Lx + 3)
        u = A.alloc("u", Lx)
        u_bf = A.alloc("u_bf", Lx, BF16)
        Ab = A.alloc("Abuf", Lx)
        Ib = A.alloc("Ibuf", Lx)
        HF = A.alloc("HF", Lx)
        Tb = V(xa_pad.ap[:, 0:Lx], xa_pad.keys)
        HB = Tb
        YA = V(Ib.ap.bitcast(BF16)[:, 0:Lx], Ib.keys)
        TL = min(512, Lx)
        for n in range(8):
            w = self.load_wchunk(wring, wint_d, n)
            self.memset(xa_pad[:, 0:1], 0.0)
            self.memset(xa_pad[:, Lx + 1:Lx + 3], 0.0)
            self.proj(w, 128, hT, Lx, lambda t, tl, ps: self.act(xa_pad[:, 1 + t * tl:1 + (t + 1) * tl], ps, AF.Identity,
                                                                    bias=binT[:, n:n + 1]))
            self.ts(u, xa_pad[:, 0:Lx], conva[:, n, 0:1], conva[:, n, 4:5], ALU.mult, ALU.add)
            for j in range(1, 4):
                self.stt(u, xa_pad[:, j:j + Lx], conva[:, n, j:j + 1], u, ALU.mult, ALU.add)
            self.cp(u_bf, u, eng="scalar")
            for d in range(2):
                for t in range(Lx // TL):
                    sl = slice(t * TL, (t + 1) * TL)
                    ps = self.bank()
                    self.mm(ps[:, 0:TL], lruw[:, 2 * d, n, :], u_bf[:, sl])
                    self.act(Ab[:, sl], ps[:, 0:TL], AF.Sigmoid, bias=lrub[:, 2 * d, n:n + 1])
                    ps2 = self.bank()
                    self.mm(ps2[:, 0:TL], lruw[:, 2 * d + 1, n, :], u_bf[:, sl])
                    self.act(Ib[:, sl], ps2[:, 0:TL], AF.Sigmoid, bias=lrub[:, 2 * d + 1, n:n + 1])
                self.act(Ab, Ab, AF.Exp, scale=cch[:, d, n:n + 1])
                self.tt(Tb, Ab, Ab, ALU.mult, eng="gpsimd")
                self.ts(Tb, Tb, 0.99999994, -1.0, ALU.min, ALU.mult)
                self.act(Tb, Tb, AF.Sqrt, bias=1.0)
                self.tt(Ib, Ib, u, ALU.mult, eng="gpsimd")
                self.tt(Ib, Ib, Tb, ALU.mult)
                init = lru_state[:, n, d:d + 1] if lat else 0.0
                if d == 0:
                    self.scan(HF, Ab, Ib, init)
                else:
                    self.scan(HB[:, ::-1], Ab[:, ::-1], Ib[:, ::-1], init)
            if not lat:
                self.cp(lru_state[:, n, 0:1], HF[:, Lx - 1:Lx])
                self.cp(lru_state[:, n, 1:2], HB[:, 0:1])
            else:
                self.tt(HF, HF, HB, ALU.add, eng="gpsimd")
                w = self.load_wchunk(wring, wint_d, 8 + n)
                self.proj(w, 128, hT, Lx, lambda t, tl, ps: self.act(Ab[:, t * tl:(t + 1) * tl], ps, AF.Identity,
                                                                        bias=binT[:, 8 + n:9 + n]))
                self.tt(Tb, Ab, Ab, ALU.mult, eng="gpsimd")
                self.ts(Tb, Tb, 0.044715, 1.0, ALU.mult, ALU.add)
                self.tt(Tb, Tb, Ab, ALU.mult)
                self.act(Tb, Tb, AF.Sigmoid, scale=2.0 * math.sqrt(2.0 / math.pi))
                self.tt(Tb, Tb, Ab, ALU.mult, eng="gpsimd")
                self.tt(YA, Tb, HF, ALU.mult)
                self.dma(E["yaT_d"][b, n].k(b, n), YA)
        self.P.barrier()
        A.reset(m1)

        SG = Ring([A.alloc("SG%d" % i, Lx) for i in range(2)])
        if lat:
            for dc in range(16):
                w = self.load_wchunk(wring, wint_d, 48 + dc)
                sgb = SG.next()
                self.proj(w, 128, hT, Lx, lambda t, tl, ps: self.act(sgb[:, t * tl:(t + 1) * tl], ps, AF.Sigmoid,
                                                                        bias=binT[:, 48 + dc:49 + dc]))
                self.dma(E["sg_d"][b, dc].k(b, dc), sgb)
        pq = E["pq_d"]
        for c in range(16, 48 if lat else 40):
            w = self.load_wchunk(wring, wint_d, c)
            sgb = SG.next()
            func = AF.Silu if c >= 40 else AF.Identity
            self.proj(w, 128, hT, Lx, lambda t, tl, ps: self.act(self.cm_out(sgb, 0, Lx, lat, t, tl), self.cm_in(ps, lat),
                                                                    func, bias=binT[:, c:c + 1]))
            self.dma(V(pq.ap[c - 16][:, 0:Lx], [kk + (c,) for kk in pq.keys]), sgb)
        self.P.barrier()
        A.reset(m1)

        wlg = A.alloc("wlg", (8, 32), BF16)
        self.dma(wlg, E["winlg_d"], eng="gpsimd")
        lgT = A.alloc("lgT", Lx)
        for t in range(Lx // TL):
            ps = self.bank()
            for k in range(8):
                self.mm(ps[0:32, 0:TL], wlg[:, k, :], hT[:, k, t * TL:(t + 1) * TL], start=(k == 0), stop=(k == 7))
            self.act(self.cm_out(lgT, 0, Lx, lat, t, TL)[0:32], self.cm_in(ps[0:32, 0:TL], lat), AF.Identity, bias=binT[0:32, 64:65])
        lg_tm = A.alloc("lg_tm", (nch, 32))
        for n in range(nch):
            ps = self.bank()
            self.tr(ps[:, 0:32], lgT[0:32, n * 128:(n + 1) * 128], ident_f[0:32, 0:32])
            self.evac(lg_tm[:, n, :], ps[:, 0:32])
        XG = A.alloc("XG", (2, nch, 8))
        AXb = A.alloc("AXb", (2, nch, 8))
        Gt = A.alloc("Gt", (2, nch, 8))
        GTOT = A.alloc("GTOT", (2, nch, 8))
        BT, NBT, GC, EKG, GL, BEG = (tabs[nm] for nm in ("BT", "NBT", "GC", "EKG", "GL", "BEG"))
        lg4 = lg_tm.re("p n (j h) -> p j n h", h=8)
        dtb = gdnrows[:, 0:16].re("p (d h) -> p d h", h=8)
        self.tt(XG, lg4[:, 0:2], V(dtb.ap.unsqueeze(2).to_broadcast([128, 2, nch, 8]), dtb.keys), ALU.add)
        self.act(AXb, XG, AF.Abs)
        self.act(AXb, AXb, AF.Exp, scale=-1.0)
        self.act(AXb, AXb, AF.Ln, bias=1.0)
        self.stt(XG, XG, 0.0, AXb, ALU.max, ALU.add)
        ng = nega.re("p (d h) -> p d h", h=8)
        self.tt(Gt, XG, V(ng.ap.unsqueeze(2).to_broadcast([128, 2, nch, 8]), ng.keys), ALU.mult)
        self.act(BT, lg4[:, 2:4], AF.Sigmoid)
        self.ts(NBT, BT, -1.0, None, ALU.mult)
        for d in range(2):
            ps = self.bank()
            tri = mIU if d == 0 else mIL
            self.mm(ps[:, 0:NH], tri, Gt[:, d].re("p n h -> p (n h)"))
            self.evac(GC[:, d].re("p n h -> p (n h)"), ps[:, 0:NH])
            ps = self.bank()
            self.mm(ps[:, 0:NH], ones_f, Gt[:, d].re("p n h -> p (n h)"))
            self.evac(GTOT[:, d].re("p n h -> p (n h)"), ps[:, 0:NH])
        self.tt(EKG, GTOT, GC, ALU.subtract)
        self.act(EKG, EKG, AF.Exp)
        self.act(GL, GTOT, AF.Exp)
        self.act(BEG, GC, AF.Exp)
        self.tt(BEG, BEG, BT, ALU.mult)
        return tabs, tab_mark

    def cm_out(self, buf, off, Lx, lat, t, tl):
        if not lat:
            return buf[:, off + t * tl:off + (t + 1) * tl]
        rows = tl // GW
        v = buf[:, off:off + Lx].re("p (c r) -> p r c", c=GW)
        return v[:, t * rows:(t + 1) * rows, :]

    def cm_in(self, ps, lat):
        if not lat:
            return ps
        return ps.re("p (r c) -> p r c", c=GW)

    def gdn(self, b, lat, tabs, E):
        A, P = self.A, self.P
        NB, R = self.NB, self.R
        Lx = self.L if lat else self.CTX
        nch = Lx // 128
        TL = min(512, Lx)
        binT, ident_f, ident_b, ones_f, ones_b = E["binT"], E["ident_f"], E["ident_b"], E["ones_f"], E["ones_b"]
        mIL, mSL, mIU, mSU = E["mIL"], E["mSL"], E["mIU"], E["mSU"]
        gdnnw, convq, gdn_state = E["gdnnw"], E["convq"], E["gdn_state"]
        BT, NBT, GC, EKG, GL, BEG = (tabs[nm] for nm in ("BT", "NBT", "GC", "EKG", "GL", "BEG"))
        pq = E["pq_d"]
        if not lat:
            self.memset(gdn_state, 0.0)
        PAD = A.alloc("PAD", Lx + 16)
        CV = A.alloc("CV", Lx)
        qT = A.alloc("qT", Lx, BF16)
        kT = A.alloc("kT", Lx, BF16)
        vT = A.alloc("vT", Lx, BF16)
        OT = A.alloc("OT", Lx)
        OTa = V(OT.ap, [kk + (n,) for kk in OT.keys for n in range(nch)])
        k_tm = A.alloc("k_tm", (nch, 128), BF16)
        v_tm = A.alloc("v_tm", (nch, 128), BF16)
        YB = V(PAD.ap[:, 0:Lx // 2].bitcast(BF16), PAD.keys)
        SQr = Ring([A.alloc("SQ%d" % i, TL, BF16) for i in range(2)])
        RTr = Ring([A.alloc("RT%d" % i, TL) for i in range(2)])
        S_bf = [A.alloc("S_bf%d" % d, 128, BF16) for d in range(2)]

        GC_ = 2 if nch % 2 == 0 else 1
        NI = 2 * GC_
        TMP = [{nm: A.alloc("%s_t%d" % (nm, i), 128) for nm in ("DG", "M1", "M2", "ER", "KB", "VB")} for i in range(NI)]
        BFR = [{nm: Ring([A.alloc("%s_b%d_%d" % (nm, i, j), 128, BF16) for j in range(2)]) for nm in ("N", "M", "X")} for i in range(NI)]
        MB1 = [A.alloc("MB1_%d" % d, 128) for d in range(2)]
        MB2 = [A.alloc("MB2_%d" % d, 128) for d in range(2)]
        self.ts(MB1[0], mSL, -1e4, 1e4, ALU.mult, ALU.add, eng="gpsimd")
        self.ts(MB1[1], mSU, -1e4, 1e4, ALU.mult, ALU.add, eng="gpsimd")
        self.ts(MB2[0], mIU, 1e4, -1e4, ALU.mult, ALU.add, eng="gpsimd")
        self.ts(MB2[1], mIL, 1e4, -1e4, ALU.mult, ALU.add, eng="gpsimd")
        RNG = [{nm: Ring(self.r32[i][nm]) for nm in ("N", "M", "X")} for i in range(NI)]
        OUTT = [[{nm: A.alloc("%s_o%d_%d" % (nm, par, i), 128, F32 if nm == "UC" else BF16)
                  for nm in ("ACT", "WCT", "UC", "QG", "KG", "VN")} for i in range(NI)] for par in range(2)]

        import os as _os
        KG = int(_os.environ.get("KG", "99"))
        for h in range(8 if KG > 0 else 0):
            for (qi, dst) in ((0, qT), (1, kT), (2, vT)):
                c = 16 + qi * 8 + h
                self.memset(PAD[:, 7:8], 0.0)
                self.memset(PAD[:, Lx + 8:Lx + 10], 0.0)
                self.dma(PAD[:, 8:8 + Lx], V(pq.ap[c - 16][:, 0:Lx], [kk + (c,) for kk in pq.keys]))
                cw = convq[:, qi * 8 + h, :]
                self.ts(CV, PAD[:, 7:7 + Lx], cw[:, 0:1], None, ALU.mult)
                for j in range(1, 4):
                    self.stt(CV, PAD[:, 7 + j:7 + j + Lx], cw[:, j:j + 1], CV, ALU.mult, ALU.add)
                self.act(CV, CV, AF.Silu)
                if qi < 2:
                    for t in range(Lx // TL):
                        sl = slice(t * TL, (t + 1) * TL)
                        sq, rt = SQr.next(), RTr.next()
                        self.tt(sq, CV[:, sl], CV[:, sl], ALU.mult, eng="gpsimd")
                        ps = self.bank()
                        self.mm(ps[:, 0:TL], ones_b, sq)
                        self.act(rt, ps[:, 0:TL], AF.Sqrt, bias=1e-6)
                        self.recip(rt, rt)
                        self.stt(dst[:, sl], CV[:, sl], (128.0 ** -0.5) if qi == 0 else 1.0, rt, ALU.mult, ALU.mult)
                else:
                    self.cp(dst, CV, eng="scalar")
            if KG < 2:
                continue
            for n in range(nch):
                ps = self.bank()
                self.mm(ps[:, 0:128], kT[:, n * 128:(n + 1) * 128], ident_b)
                self.mm(ps[:, 128:256], vT[:, n * 128:(n + 1) * 128], ident_b)
                self.cp(k_tm[:, n, :].k(n), ps[:, 0:128], eng="scalar")
                self.cp(v_tm[:, n, :].k(n), ps[:, 128:256], eng="vector")
            self.memset(OTa, 0.0)
            for d in range(2):
                self.cp(S_bf[d], gdn_state[:, h, d, :], eng="scalar")
            ngroups = nch // GC_

            def group_insts(p):
                L_ = []
                for j in range(GC_):
                    for d in range(2):
                        s_ = p * GC_ + j
                        n = s_ if d == 0 else nch - 1 - s_
                        i = j * 2 + d
                        L_.append({"d": d, "n": n, "cs": slice(n * 128, (n + 1) * 128), "T": TMP[i], "R": RNG[i],
                                   "B": BFR[i], "O": OUTT[p % 2][i]})
                return L_

            def pre(insts):
                for I in insts:
                    d, n, T = I["d"], I["n"], I["T"]
                    ktn, vtn = k_tm[:, n, :].k(n), v_tm[:, n, :].k(n)
                    I["Gp"] = GC[:, d, n, h:h + 1]
                    self.act(T["KB"], ktn, AF.Identity, scale=BEG[:, d, n, h:h + 1])
                    self.act(I["O"]["KG"], ktn, AF.Identity, scale=EKG[:, d, n, h:h + 1])
                    self.act(T["VB"], vtn, AF.Identity, scale=BT[:, d, n, h:h + 1])
                    self.ts(T["DG"], ident_f, I["Gp"], None, ALU.mult, eng="gpsimd")
                for I in insts:
                    I["psg"] = self.bank()
                    self.mm(I["psg"][:, 0:128], ones_f, I["T"]["DG"])
                for I in insts:
                    T, psg = I["T"], I["psg"]
                    self.stt(T["M1"], psg[:, 0:128], I["Gp"], MB1[I["d"]], ALU.subtract, ALU.max)
                    self.stt(T["M2"], psg[:, 0:128], I["Gp"], MB2[I["d"]], ALU.subtract, ALU.min)
                    self.act(T["ER"], psg[:, 0:128], AF.Exp)
                for I in insts:
                    T = I["T"]
                    self.act(T["M1"], T["M1"], AF.Exp, scale=-1.0)
                    self.act(T["M2"], T["M2"], AF.Exp)
                for I in insts:
                    cs = I["cs"]
                    I["psk"] = self.bank()
                    self.mm(I["psk"][:, 0:128], kT[:, cs], kT[:, cs])
                    self.mm(I["psk"][:, 128:256], kT[:, cs], qT[:, cs])
                for I in insts:
                    T, d, n = I["T"], I["d"], I["n"]
                    I["Nm"], I["Mm"], I["Xm"] = I["R"]["N"].next(), I["R"]["M"].next(), I["R"]["X"].next()
                    self.stt(I["Nm"], I["psk"][:, 0:128], NBT[:, d, n, h:h + 1], T["M1"], ALU.mult, ALU.mult)
                    self.tt(I["O"]["ACT"], I["psk"][:, 128:256], T["M2"], ALU.mult)
                for I in insts:
                    I["pst"] = self.bank()
                    self.tr(I["pst"][:, 0:128], I["Nm"].cast(F32), ident_f)
                for I in insts:
                    self.cp(I["Mm"], I["pst"][:, 0:128], eng="scalar")
                    self.tt(I["Xm"], I["Mm"].cast(F32), ident_f, ALU.add)
                    I["Pm"], I["PTm"] = I["Mm"], I["Nm"]
                    I["Xop"] = I["Xm"]
                for lvl in range(6):
                    last = lvl == 5
                    hi = lvl >= HI_LVL
                    nxt_hi = lvl >= HI_LVL - 1
                    for I in insts:
                        I["psl"] = self.bank()
                        if not last:
                            self.mm(I["psl"][:, 0:128], I["PTm"], I["Pm"])
                        self.mm(I["psl"][:, 128:256], I["Pm"], I["PTm"])
                    for I in insts:
                        if hi:
                            I["PT2"] = I["B"]["N"].next()
                            self.cp(I["PT2"], I["psl"][:, 128:256], eng="scalar")
                            I["PT2n"] = I["PT2"]
                        else:
                            I["PT2"] = I["R"]["N"].next()
                            self.cp(I["PT2"], I["psl"][:, 128:256], eng="scalar")
                            I["PT2n"] = I["PT2"]
                            if nxt_hi:
                                I["PT2n"] = I["B"]["N"].next()
                                self.cp(I["PT2n"], I["psl"][:, 128:256], eng="scalar")
                        if not last:
                            I["P2"] = (I["B"]["M"] if nxt_hi else I["R"]["M"]).next()
                            self.cp(I["P2"], I["psl"][:, 0:128], eng="vector")
                    for I in insts:
                        I["psx"] = self.bank()
                        self.mm(I["psx"][:, 0:128], I["PT2"], I["Xop"])
                    for I in insts:
                        X2 = I["R"]["X"].next()
                        self.tt(X2, I["psx"][:, 0:128], I["Xm"], ALU.add)
                        I["Xm"] = X2
                        I["Xop"] = X2
                        if nxt_hi and not last:
                            I["Xop"] = I["B"]["X"].next()
                            self.cp(I["Xop"], X2, eng="scalar")
                        I["PTm"] = I["PT2n"]
                        if not last:
                            I["Pm"] = I["P2"]
                for I in insts:
                    I["psw"] = self.bank()
                    self.mm(I["psw"][:, 0:128], I["T"]["KB"], I["Xm"].cast(F32))
                    self.mm(I["psw"][:, 128:256], I["Xm"].cast(F32), I["T"]["VB"])
                for I in insts:
                    self.cp(I["O"]["WCT"], I["psw"][:, 0:128], eng="scalar")
                    self.cp(I["O"]["UC"], I["psw"][:, 128:256], eng="vector")
                    self.tt(I["O"]["QG"], qT[:, I["cs"]], I["T"]["ER"], ALU.mult, eng="gpsimd")

            def state(insts):
                for I in insts:
                    d, n, cs, O = I["d"], I["n"], I["cs"], I["O"]
                    Sst = gdn_state[:, h, d, :]
                    pss = self.bank()
                    self.mm(pss[:, 0:128], O["WCT"], S_bf[d])
                    self.tt(O["VN"], O["UC"], pss[:, 0:128], ALU.subtract)
                    pso = self.bank()
                    self.mm(pso[:, 0:128], S_bf[d], O["QG"], start=True, stop=False)
                    self.mm(pso[:, 0:128], O["VN"], O["ACT"], start=False, stop=True)
                    psu = self.bank()
                    self.mm(psu[:, 0:128], O["KG"], O["VN"])
                    OTn = V(OT.ap[:, cs], [kk + (n,) for kk in OT.keys])
                    self.tt(OTn, OTn, pso[:, 0:128], ALU.add)
                    self.stt(Sst, Sst, GL[:, d, n, h:h + 1], psu[:, 0:128], ALU.mult, ALU.add)
                    self.cp(S_bf[d], Sst, eng="scalar")

            groups = [group_insts(p) for p in range(ngroups)]
            pre(groups[0])
            for p in range(ngroups):
                if p + 1 < ngroups:
                    pre(groups[p + 1])
                state(groups[p])
            if lat and KG > 6:
                c = 40 + h
                self.dma(CV, V(pq.ap[c - 16][:, 0:Lx], [kk + (c,) for kk in pq.keys]))
                for t in range(Lx // TL):
                    sl = slice(t * TL, (t + 1) * TL)
                    sq, rt = SQr.next(), RTr.next()
                    self.tt(sq, OTa[:, sl], OTa[:, sl], ALU.mult, eng="gpsimd")
                    ps = self.bank()
                    self.mm(ps[:, 0:TL], ones_b, sq)
                    self.act(rt, ps[:, 0:TL], AF.Sqrt, bias=LN_EPS, scale=1.0 / 128.0)
                    self.recip(rt, rt)
                    self.tt(CV[:, sl], CV[:, sl], rt, ALU.mult, eng="gpsimd")
                self.stt(YB.re("p (r c) -> p c r", c=GW), OTa.re("p (c r) -> p c r", c=GW), gdnnw[:, 0:1],
                         CV.re("p (c r) -> p c r", c=GW), ALU.mult, ALU.mult)
                self.dma(E["ybT_d"][b, h].k(b, h), YB)

    def merge(self, b, E):
        A, P = self.A, self.P
        NB, L, CAP = self.NB, self.L, self.CAP
        ident_f, ones_b, mSU_b = E["ident_f"], E["ones_b"], E["mSU_b"]
        DEST, WT, CNT, ECAP, brt = E["DEST"], E["WT"], E["CNT"], E["ECAP"], E["brt"]
        wpa = A.alloc("wpa", (8, D), BF16)
        wpb = A.alloc("wpb", (8, D), BF16)
        wout = A.alloc("wout", (8, D), BF16)
        for (dst, src) in ((wpa, E["wpa_d"]), (wpb, E["wpb_d"]), (wout, E["wout_d"])):
            sv = src.re("(k p) n -> p k n", p=128)
            for k in range(8):
                self.dma(dst[:, k, :].k(k), sv[:, k, :], eng="gpsimd")
        wrt = A.alloc("wrt", (8, 36))
        self.dma(wrt, E["wrt_d"])
        g1bc = A.alloc("g1bc", D)
        sh2bc = A.alloc("sh2bc", D)
        sc2bc = A.alloc("sc2bc", D)
        ln1g = A.alloc("ln1g", D)
        ln1b = A.alloc("ln1b", D)
        bcs = E["bcs_d"]
        bk = [kk + (b, ct) for kk in bcs.keys for ct in range(8)]
        self.dma(g1bc, V(bcs.ap[b, 0], bk))
        self.dma(sh2bc, V(bcs.ap[b, 1], bk))
        self.dma(sc2bc, V(bcs.ap[b, 2], bk))
        self.dma(ln1g, E["lnbc_d"][:, 0, :])
        self.dma(ln1b, E["lnbc_d"][:, 1, :])
        YAt = Ring([A.alloc("YAt%d" % i, (8, 512), BF16) for i in range(1)])
        YBt = Ring([A.alloc("YBt%d" % i, (8, 512), BF16) for i in range(1)])
        SGa = Ring([A.alloc("SGa%d" % i, 512) for i in range(3)])
        SGb = Ring([A.alloc("SGb%d" % i, 512) for i in range(3)])
        M1r = Ring([A.alloc("M1r%d" % i, 512) for i in range(2)])
        M2r = Ring([A.alloc("M2r%d" % i, 512) for i in range(2)])
        MTr = Ring([A.alloc("MT%d" % i, (8, 512), BF16) for i in range(1)])
        T1r = Ring([A.alloc("T1%d" % i, D) for i in range(2)])
        Xtr = Ring([A.alloc("Xt%d" % i, D) for i in range(2)])
        X1r = Ring([A.alloc("X1%d" % i, D) for i in range(2)])
        H2r = Ring([A.alloc("H2%d" % i, D) for i in range(2)])
        H2br = Ring([A.alloc("H2b%d" % i, D, BF16) for i in range(3)])
        H2Tr = Ring([A.alloc("H2T%d" % i, (8, 128)) for i in range(2)])
        sm = Ring([A.alloc("rsm%d" % i, 256) for i in range(3)])
        yav = E["yaT_d"]
        ybv = E["ybT_d"]
        sgv = E["sg_d"]
        TLm = 512
        for g in range(L // TLm):
            ts_ = slice(g * TLm, (g + 1) * TLm)
            ya, yb = YAt.next(), YBt.next()
            for k in range(8):
                self.dma(ya[:, k, :].k(k), V(yav.ap[b, k][:, ts_], [kk + (b, k) for kk in yav.keys]))
                self.dma(yb[:, k, :].k(k), V(ybv.ap[b, k][:, ts_], [kk + (b, k) for kk in ybv.keys]))
            MT = MTr.next()
            for dc in range(8):
                sga, sgb = SGa.next(), SGb.next()
                self.dma(sga, V(sgv.ap[b, dc][:, ts_], [kk + (b, dc) for kk in sgv.keys]))
                self.dma(sgb, V(sgv.ap[b, 8 + dc][:, ts_], [kk + (b, 8 + dc) for kk in sgv.keys]))
                psa = self.bank()
                for k in range(8):
                    self.mm(psa, wpa[:, k, dc * 128:(dc + 1) * 128].k(k), ya[:, k, :].k(k), start=(k == 0), stop=(k == 7))
                psb_ = self.bank()
                for k in range(8):
                    self.mm(psb_, wpb[:, k, dc * 128:(dc + 1) * 128].k(k), yb[:, k, :].k(k), start=(k == 0), stop=(k == 7))
                m1, m2 = M1r.next(), M2r.next()
                self.tt(m1, psa, sga, ALU.mult)
                self.tt(m2, psb_, sgb, ALU.mult)
                self.tt(MT[:, dc, :].k(dc), m1, m2, ALU.add, eng="gpsimd")
            MTa = V(MT.ap, [kk + (dc,) for kk in MT.keys for dc in range(8)])
            for sub in range(4):
                tile = (b * L + g * TLm) // 128 + sub
                tok0 = g * TLm + sub * 128
                T1 = T1r.next()
                for half in range(2):
                    ps = self.bank()
                    for dc in range(8):
                        self.mm(ps, MTa[:, dc, sub * 128:(sub + 1) * 128], wout[:, dc, half * 512:(half + 1) * 512].k(dc),
                                start=(dc == 0), stop=(dc == 7))
                    self.tt(T1[:, half * 512:(half + 1) * 512].k(half), ps, g1bc[:, half * 512:(half + 1) * 512], ALU.mult)
                T1a = V(T1.ap, [kk + (hh,) for kk in T1.keys for hh in range(2)])
                xt = Xtr.next()
                self.dma(xt, E["x_d"][b][tok0:tok0 + 128, :])
                self.stt(T1a, xt, ALPHA, T1a, ALU.mult, ALU.add)
                mean, rstd = self.ln_stats(T1a)
                self.ts(T1a, T1a, mean, rstd, ALU.subtract, ALU.mult)
                X1 = X1r.next()
                self.tt(X1, T1a, ln1g, ALU.mult, eng="gpsimd")
                self.tt(X1, X1, ln1b, ALU.add, eng="gpsimd")
                self.dma(V(E["x1_d"].ap[tile * 128:(tile + 1) * 128, :], [kk + (tile,) for kk in E["x1_d"].keys]), X1)
                self.route(tile, X1, sh2bc, sc2bc, wrt, H2r, H2br, H2Tr, sm, E)

    def route(self, tile, X1, sh2bc, sc2bc, wrt, H2r, H2br, H2Tr, sm, E):
        CAP = self.CAP
        ident_f, ones_b, mSU_b = E["ident_f"], E["ones_b"], E["mSU_b"]
        DEST, WT, CNT, ECAP, brt = E["DEST"], E["WT"], E["CNT"], E["ECAP"], E["brt"]
        mean, rstd = self.ln_stats(X1)
        H2 = H2r.next()
        self.ts(H2, X1, mean, rstd, ALU.subtract, ALU.mult)
        self.tt(H2, H2, sc2bc, ALU.mult, eng="gpsimd")
        self.tt(H2, H2, sh2bc, ALU.add, eng="gpsimd")
        H2b = H2br.next()
        self.cp(H2b, H2, eng="scalar")
        H2T = H2Tr.next()
        for half in range(2):
            ps = self.bank()
            for kk in range(4):
                k = half * 4 + kk
                self.tr(ps[:, kk * 128:(kk + 1) * 128], H2[:, k * 128:(k + 1) * 128], ident_f)
            self.evac(H2T[:, half * 4:(half + 1) * 4, :].k(half), ps.re("p (a b) -> p a b", b=128))
        H2Ta = V(H2T.ap, [kk + (hh,) for kk in H2T.keys for hh in range(2)])
        psr = self.bank()
        for k in range(8):
            self.mm(psr[:, 0:36], H2Ta[:, k, :], wrt[:, k, :], start=(k == 0), stop=(k == 7))
        s = sm.next()
        LG = s[:, 0:36]
        gmax, ngmax, gsum, pg = s[:, 36:37], s[:, 37:38], s[:, 38:39], s[:, 39:40]
        GE, OHG = s[:, 40:44], s[:, 44:48]
        ML = s[:, 48:80]
        m8 = s[:, 80:88]
        d21, e21, den, w1, w2 = s[:, 88:89], s[:, 89:90], s[:, 90:91], s[:, 91:92], s[:, 92:93]
        OH1, OH2 = s[:, 96:128], s[:, 128:160]
        SL = s[:, 160:192]
        TMP = s[:, 192:224]
        d1f, d2f = s[:, 224:225], s[:, 225:226]
        Ab = V(s.ap[:, 232:248].bitcast(BF16), s.keys)
        self.tt(LG, psr[:, 0:36], brt, ALU.add)
        rmx = lambda o, i: self.P.op("vector", (lambda oo, ii: lambda e: e.reduce_max(out=oo, in_=ii, axis=AX.X))(o.ap, i.ap),
                                     reads=_keys(i), writes=_keys(o))
        rsm = lambda o, i: self.P.op("vector", (lambda oo, ii: lambda e: e.reduce_sum(out=oo, in_=ii, axis=AX.X))(o.ap, i.ap),
                                     reads=_keys(i), writes=_keys(o))
        rmx(gmax, LG[:, 0:4])
        self.ts(ngmax, gmax, -1.0, None, ALU.mult)
        self.act(GE, LG[:, 0:4], AF.Exp, bias=ngmax)
        rsm(gsum, GE)
        self.recip(pg, gsum)
        self.ts(OHG, LG[:, 0:4], gmax, None, ALU.is_equal)
        self.ts(OHG, OHG, 1.0, 1e9, ALU.subtract, ALU.mult)
        self.tt(ML.re("p (g e) -> p g e", e=8), LG[:, 4:36].re("p (g e) -> p g e", e=8),
                V(OHG.ap.unsqueeze(2).to_broadcast([128, 4, 8]), OHG.keys), ALU.add)
        ml, m8a = ML.ap, m8.ap
        self.P.op("vector", lambda e: e.max(out=m8a, in_=ml), reads=_keys(ML), writes=_keys(m8))
        self.tt(d21, m8[:, 1:2], m8[:, 0:1], ALU.subtract)
        self.act(e21, d21, AF.Exp)
        self.ts(den, e21, 1.0, None, ALU.add)
        self.recip(den, den)
        self.tt(w1, pg, den, ALU.mult)
        self.tt(w2, w1, e21, ALU.mult)
        self.cp(WT[:, tile, 0:1].k(tile), w1)
        self.cp(WT[:, tile, 1:2].k(tile), w2)
        self.ts(OH1, ML, m8[:, 0:1], None, ALU.is_equal)
        self.ts(OH2, ML, m8[:, 1:2], None, ALU.is_equal)
        self.tt(Ab, OH1, OH2, ALU.add)
        psk = self.bank()
        self.mm(psk[:, 0:32], mSU_b, Ab)
        self.mm(psk[:, 32:64], ones_b, Ab)
        self.tt(SL, psk[:, 0:32], CNT, ALU.add)
        self.stt(SL, SL, float(CAP - 1), ECAP, ALU.min, ALU.add)
        self.tt(CNT, CNT, psk[:, 32:64], ALU.add)
        self.tt(TMP, OH1, SL, ALU.mult)
        rsm(d1f, TMP)
        self.tt(TMP, OH2, SL, ALU.mult)
        rsm(d2f, TMP)
        self.cp(DEST[:, tile, 0:1].k(tile), d1f)
        self.cp(DEST[:, tile, 1:2].k(tile), d2f)
        xg = E["xg_d"]
        self.scatter(V(xg.ap, [kk + (tile, 0) for kk in xg.keys]), DEST[:, tile, 0:1].k(tile), H2b)
        self.scatter(V(xg.ap, [kk + (tile, 1) for kk in xg.keys]), DEST[:, tile, 1:2].k(tile), H2b)

    def experts(self, E):
        A = self.A
        CAP, NB, L = self.CAP, self.NB, self.L
        NT = NB * L // 128
        ident_b = E["ident_b"]
        xg, yg = E["xg_d"], E["yg_d"]
        xg_all = V(xg.ap, [kk + (t, j) for kk in xg.keys for t in range(NT) for j in range(2)])
        nst = CAP // 128
        Wg = Ring([A.alloc("Wg%d" % i, (8, D), BF16) for i in range(2)])
        Wu = Ring([A.alloc("Wu%d" % i, (8, D), BF16) for i in range(2)])
        Wd = Ring([A.alloc("Wd%d" % i, (8, D), BF16) for i in range(2)])
        Xr = Ring([A.alloc("Xr%d" % i, D, BF16) for i in range(3)])
        XT = Ring([A.alloc("XTe%d" % i, (8, CAP), BF16) for i in range(2)])
        AT = Ring([A.alloc("ATe%d" % i, (8, CAP), BF16) for i in range(2)])
        SGr = Ring([A.alloc("SGe%d" % i, 512) for i in range(3)])
        Yr = Ring([A.alloc("Ye%d" % i, D) for i in range(3)])
        segs = []
        o = 0
        while o < CAP:
            n = min(512, CAP - o)
            segs.append((o, n))
            o += n
        for e in range(NEXP):
            wg, wu, wd = Wg.next(), Wu.next(), Wd.next()
            for (dst, src) in ((wg, E["weg_d"]), (wu, E["weu_d"]), (wd, E["wed_d"])):
                sv = src[e].re("(k p) n -> p k n", p=128)
                for k in range(8):
                    self.dma(dst[:, k, :].k(k), sv[:, k, :], eng="gpsimd")
            xT = XT.next()
            for s_ in range(nst):
                xr = Xr.next()
                self.dma(xr, V(xg_all.ap[e * CAP + s_ * 128:e * CAP + (s_ + 1) * 128, :], xg_all.keys))
                for half in range(2):
                    ps = self.bank()
                    for kk in range(4):
                        k = half * 4 + kk
                        self.mm(ps[:, kk * 128:(kk + 1) * 128], xr[:, k * 128:(k + 1) * 128], ident_b)
                    self.evac(xT[:, half * 4:(half + 1) * 4, s_ * 128:(s_ + 1) * 128].k(half, s_),
                              ps.re("p (a b) -> p a b", b=128))
            xTa = V(xT.ap, [kk + (hh, ss) for kk in xT.keys for hh in range(2) for ss in range(nst)])
            aT = AT.next()
            for j in range(8):
                for (o, n) in segs:
                    psg = self.bank()
                    for k in range(8):
                        self.mm(psg[:, 0:n], wg[:, k, j * 128:(j + 1) * 128].k(k), xTa[:, k, o:o + n], start=(k == 0), stop=(k == 7))
                    psu = self.bank()
                    for k in range(8):
                        self.mm(psu[:, 0:n], wu[:, k, j * 128:(j + 1) * 128].k(k), xTa[:, k, o:o + n], start=(k == 0), stop=(k == 7))
                    sg_ = SGr.next()
                    self.act(sg_[:, 0:n], psg[:, 0:n], AF.Silu)
                    self.tt(aT[:, j, o:o + n].k(j, o), sg_[:, 0:n], psu[:, 0:n], ALU.mult)
            aTa = V(aT.ap, [kk + (j, o) for kk in aT.keys for j in range(8) for (o, n) in segs])
            for s_ in range(nst):
                y = Yr.next()
                for half in range(2):
                    ps = self.bank()
                    for j in range(8):
                        self.mm(ps, aTa[:, j, s_ * 128:(s_ + 1) * 128], wd[:, j, half * 512:(half + 1) * 512].k(j),
                                start=(j == 0), stop=(j == 7))
                    self.evac(y[:, half * 512:(half + 1) * 512].k(half), ps)
                ya = V(y.ap, [kk + (hh,) for kk in y.keys for hh in range(2)])
                r0 = e * CAP + s_ * 128
                self.dma(V(yg.ap[r0:r0 + 128, :], [kk + (e, s_) for kk in yg.keys]), ya)

    def combine(self, E):
        A = self.A
        CAP, NB, L = self.CAP, self.NB, self.L
        NT = NB * L // 128
        nst = CAP // 128
        DEST, WT = E["DEST"], E["WT"]
        yg, x1 = E["yg_d"], E["x1_d"]
        yg_all = V(yg.ap, [kk + (e, s_) for kk in yg.keys for e in range(NEXP) for s_ in range(nst)])
        ln2g = A.alloc("ln2g", D)
        ln2b = A.alloc("ln2b", D)
        self.dma(ln2g, E["lnbc_d"][:, 2, :])
        self.dma(ln2b, E["lnbc_d"][:, 3, :])
        g2bc = [A.alloc("g2bc%d" % b, D) for b in range(NB)]
        bcs = E["bcs_d"]
        for b in range(NB):
            self.dma(g2bc[b], V(bcs.ap[b, 3], [kk + (b, ct) for kk in bcs.keys for ct in range(8)]))
        Y1r = Ring([A.alloc("Y1g%d" % i, D) for i in range(2)])
        Y2r = Ring([A.alloc("Y2g%d" % i, D) for i in range(2)])
        X1r = Ring([A.alloc("X1c%d" % i, D) for i in range(2)])
        Or = Ring([A.alloc("Oc%d" % i, D) for i in range(2)])
        for tile in range(NT):
            b = tile * 128 // L
            Y1, Y2, X1, O = Y1r.next(), Y2r.next(), X1r.next(), Or.next()
            self.gather(Y1, yg_all, DEST[:, tile, 0:1].k(tile))
            self.gather(Y2, yg_all, DEST[:, tile, 1:2].k(tile))
            self.dma(X1, V(x1.ap[tile * 128:(tile + 1) * 128, :], [kk + (tile,) for kk in x1.keys]))
            self.ts(Y1, Y1, WT[:, tile, 0:1].k(tile), None, ALU.mult, eng="gpsimd")
            self.stt(Y1, Y2, WT[:, tile, 1:2].k(tile), Y1, ALU.mult, ALU.add)
            self.tt(Y1, Y1, g2bc[b], ALU.mult, eng="gpsimd")
            self.stt(Y1, X1, ALPHA, Y1, ALU.mult, ALU.add)
            mean, rstd = self.ln_stats(Y1)
            self.ts(Y1, Y1, mean, rstd, ALU.subtract, ALU.mult)
            self.tt(O, Y1, ln2g, ALU.mult, eng="gpsimd")
            self.tt(O, O, ln2b, ALU.add)
            self.dma(V(E["out_d"].ap[tile * 128:(tile + 1) * 128, :], [("out", tile)]), O, is_output=True)


def chunk_starts():
    st = [n * 128 for n in range(8)]
    st += [1024 + n * 128 for n in range(8)]
    st += [2048 + n * 128 for n in range(24)]
    st += [5152 + n * 128 for n in range(8)]
    st += [6176 + n * 128 for n in range(16)]
    return st


def host_layout(inp, NB, core, NBP):
    f = lambda a: np.ascontiguousarray(a, dtype=np.float32)
    w_in = inp["w_in"][0]
    b_in = inp["b_in"][0]
    st = chunk_starts()
    w_in_t = np.stack([w_in[:, s:s + 128].reshape(8, 128, 128).transpose(1, 0, 2) for s in st], 0)
    w_in_lg = w_in[:, 5120:5152].reshape(8, 128, 32).transpose(1, 0, 2)
    b_inT = np.zeros((128, 65), np.float32)
    for i, s in enumerate(st):
        b_inT[:, i] = b_in[s:s + 128]
    b_inT[0:32, 64] = b_in[5120:5152]
    bs = slice(core * NB, (core + 1) * NB)
    c = inp["c"][bs]
    cT = np.zeros((128, 8, NBP), np.float32)
    for b in range(NB):
        cT[:, :, b] = c[b].reshape(8, 128).T
    cT[:, :, NB] = inp["c_ctx"].reshape(8, 128).T
    b_mod = inp["b_mod"][0]
    fm = lambda v: v.reshape(-1, 128).T
    rep = lambda v: np.broadcast_to(v[None, :], (128, v.shape[0]))
    lw = np.stack([inp["lru_wa"][0, 0], inp["lru_wx"][0, 0], inp["lru_wa"][0, 1], inp["lru_wx"][0, 1]], 0)
    lb = np.stack([inp["lru_ba"][0, 0], inp["lru_bx"][0, 0], inp["lru_ba"][0, 1], inp["lru_bx"][0, 1]], 0)
    conv_a = np.concatenate([inp["conv_a_w"][0], inp["conv_a_b"][0][None]], 0)
    d = {
        "x": inp["x"][bs], "ctx": inp["ctx"][bs], "cT": cT,
        "w_mod": inp["w_mod"][0], "bmodT": fm(b_mod[0:2048]), "bmod_bc": rep(b_mod[2048:]),
        "w_in_t": w_in_t, "w_in_lg": w_in_lg, "b_inT": b_inT,
        "conv_a": conv_a.reshape(5, 8, 128).transpose(2, 1, 0),
        "lru_w": lw.transpose(2, 0, 1, 3),
        "lru_b": lb.reshape(4, 8, 128).transpose(2, 0, 1),
        "lru_lam": inp["lru_lambda"][0].reshape(2, 8, 128).transpose(2, 0, 1),
        "conv_qkv": inp["conv_qkv_w"][0].reshape(4, 24, 128).transpose(2, 1, 0),
        "gdn_rows": rep(np.concatenate([inp["gdn_dt_bias"][0].reshape(16), inp["gdn_a_log"][0].reshape(16)])),
        "gdn_nw": inp["gdn_norm_w"][0].reshape(128, 1),
        "w_pa": inp["w_pa"][0], "w_pb": inp["w_pb"][0], "w_out": inp["w_out"][0],
        "ln_bc": np.stack([rep(inp["ln1_g"][0]), rep(inp["ln1_b"][0]), rep(inp["ln2_g"][0]), rep(inp["ln2_b"][0])], 1),
        "w_rt": np.concatenate([inp["w_router_g"][0], inp["w_router_e"][0]], 1).reshape(8, 128, 36).transpose(1, 0, 2),
        "b_rt_bc": rep(np.concatenate([inp["b_router_g"][0], inp["b_router_e"][0]])),
        "w_eg": inp["w_e_gate"][0], "w_eu": inp["w_e_up"][0], "w_ed": inp["w_e_down"][0],
    }
    return {k: f(v) for k, v in d.items()}


_CACHE = {}


def run(inputs, n_cores, NB, L, CTX, CAP, debug=()):
    key = (NB, L, CTX, CAP, tuple(debug))
    if key not in _CACHE:
        _CACHE[key] = KB(NB, L, CTX, CAP, debug).build()
    nc = _CACHE[key]
    NBP = NB + 1 + ((NB + 1) % 2)
    inp = {k: np.asarray(v) for k, v in inputs.items()}
    shared = None
    in_maps = []
    for core in range(n_cores):
        m = host_layout(inp, NB, core, NBP) if shared is None else None
        if shared is None:
            shared = m
        else:
            m = dict(shared)
            bs = slice(core * NB, (core + 1) * NB)
            m["x"] = np.ascontiguousarray(inp["x"][bs], dtype=np.float32)
            m["ctx"] = np.ascontiguousarray(inp["ctx"][bs], dtype=np.float32)
            c = inp["c"][bs]
            cT = shared["cT"].copy()
            for b in range(NB):
                cT[:, :, b] = c[b].reshape(8, 128).T
            m["cT"] = cT
        in_maps.append(m)
    res = run_bass_kernel_spmd(nc, in_maps, core_ids=list(range(n_cores)))
    return res


def kernel(**inputs):
    n_cores, NB, L, CTX, CAP = 8, 2, 4096, 256, 640
    res = run(inputs, n_cores, NB, L, CTX, CAP)
    outs = [r["out"].reshape(NB, L, D) for r in res.results]
    return np.concatenate(outs, 0).astype(np.float32)
```

```python
import math
from contextlib import ExitStack
import numpy as np
import concourse.bass as bass
import concourse.mybir as mybir
from concourse.bass_utils import run_bass_kernel_spmd

F32 = mybir.dt.float32
F32R = mybir.dt.float32r
BF16 = mybir.dt.bfloat16
I32 = mybir.dt.int32
AF = mybir.ActivationFunctionType
ALU = mybir.AluOpType
AX = mybir.AxisListType

ENGS = ("sync", "scalar", "gpsimd", "vector", "tensor")
NDMA_SEM = 16
SAME_ENGINE_SYNC = True

D = 1024
GW = 64
ALPHA = 2.0 ** 0.25
LN_EPS = 1e-6
NEXP = 32
HI_LVL = 99


class Prog:
    def __init__(self, nc, stack):
        self.nc = nc
        self.q = {e: [] for e in ENGS}
        self.cnt = {e: 0 for e in ENGS}
        self.esem = {e: stack.enter_context(nc.semaphore("es_" + e)) for e in ENGS}
        self.dsem, self.dcnt, self.dnext = {}, {}, {}
        for e in ("sync", "gpsimd"):
            self.dsem[e] = [stack.enter_context(nc.semaphore("ds_%s%d" % (e, i))) for i in range(NDMA_SEM)]
            self.dcnt[e] = [0] * NDMA_SEM
            self.dnext[e] = 0
        self.semobj = {}
        for e in ENGS:
            self.semobj[("e", e)] = self.esem[e]
        for e in self.dsem:
            for i, s in enumerate(self.dsem[e]):
                self.semobj[("d", e, i)] = s
        self.seen = {e: {} for e in ENGS}
        self.last_w = {}
        self.readers = {}
        self.out_tokens = []

    def _deps(self, eng, reads, writes):
        toks = []
        for k in reads:
            t = self.last_w.get(k)
            if t is not None:
                toks.append(t)
        for k in writes:
            t = self.last_w.get(k)
            if t is not None:
                toks.append(t)
            toks.extend(self.readers.get(k, ()))
        need = {}
        for (sk, v) in toks:
            if sk == ("e", eng) and (eng == "tensor" or not SAME_ENGINE_SYNC):
                continue
            if self.seen[eng].get(sk, 0) >= v:
                continue
            if need.get(sk, 0) < v:
                need[sk] = v
        for sk, v in need.items():
            self.seen[eng][sk] = v
        return list(need.items())

    def _commit(self, tok, reads, writes):
        for k in reads:
            if k in writes:
                continue
            self.readers.setdefault(k, []).append(tok)
        for k in writes:
            self.last_w[k] = tok
            self.readers[k] = []

    def op(self, eng, fn, reads=(), writes=()):
        psr = [k for k in reads if k[0] == "psb"]
        if psr:
            writes = list(writes) + psr
        waits = self._deps(eng, reads, writes)
        self.cnt[eng] += 1
        tok = (("e", eng), self.cnt[eng])
        self.q[eng].append((waits, fn, self.esem[eng], 1))
        self._commit(tok, reads, writes)
        return tok

    def dma(self, eng, fn, reads=(), writes=(), is_output=False):
        i = self.dnext[eng]
        self.dnext[eng] = (i + 1) % NDMA_SEM
        sk = ("d", eng, i)
        waits = self._deps(eng, reads, writes)
        prev = self.dcnt[eng][i]
        if prev > 0 and self.seen[eng].get(sk, 0) < prev:
            self.seen[eng][sk] = prev
            waits.append((sk, prev))
        self.dcnt[eng][i] += 16
        tok = (sk, self.dcnt[eng][i])
        self.q[eng].append((waits, fn, self.dsem[eng][i], 16))
        self._commit(tok, reads, writes)
        if is_output:
            self.out_tokens.append(tok)
        return tok

    def barrier(self):
        toks = [(("e", e), self.cnt[e]) for e in ENGS if self.cnt[e] > 0]
        for e in self.dsem:
            for i in range(NDMA_SEM):
                if self.dcnt[e][i] > 0:
                    toks.append((("d", e, i), self.dcnt[e][i]))
        for eng in ENGS:
            waits = []
            for (sk, v) in toks:
                if sk == ("e", eng) and eng == "tensor":
                    continue
                if self.seen[eng].get(sk, 0) >= v:
                    continue
                self.seen[eng][sk] = v
                waits.append((sk, v))
            if waits:
                self.q[eng].append((waits, None, None, 0))
        self.last_w = {}
        self.readers = {}

    def finish(self):
        self.q["sync"].append((list(self.out_tokens), None, None, 0))

    def emit(self, block):
        def run(engname):
            def body(eng):
                for (waits, fn, sem, inc) in self.q[engname]:
                    for (sk, v) in waits:
                        eng.wait_ge(self.semobj[sk], v)
                    if fn is not None:
                        fn(eng).then_inc(sem, inc)
            return body
        block.sync(run("sync"))
        block.scalar(run("scalar"))
        block.gpsimd(run("gpsimd"))
        block.vector(run("vector"))
        block.tensor(run("tensor"))


class V:
    __slots__ = ("ap", "keys")

    def __init__(self, ap, keys):
        self.ap = ap
        self.keys = tuple(keys)

    def __getitem__(self, idx):
        return V(self.ap[idx], self.keys)

    def re(self, pat, **kw):
        return V(self.ap.rearrange(pat, **kw), self.keys)

    def bc(self, shape):
        return V(self.ap.to_broadcast(list(shape)), self.keys)

    def k(self, *sub):
        return V(self.ap, [kk + tuple(sub) for kk in self.keys])

    def cast(self, dt):
        return V(self.ap.bitcast(dt), self.keys)


def _keys(*vs):
    out = []
    for v in vs:
        if isinstance(v, V):
            out.extend(v.keys)
    return out


def _a(v):
    return v.ap if isinstance(v, V) else v


DT_SIZE = {F32: 4, BF16: 2, I32: 4}


class Arena:
    def __init__(self, nc, stack, nbytes):
        self.words = nbytes // 4
        self.t = stack.enter_context(nc.sbuf_tensor("arena", [128, self.words], F32))
        self.off = 0
        self.uid = 0

    def alloc(self, name, free_shape, dt=F32):
        if isinstance(free_shape, int):
            free_shape = (free_shape,)
        nel = 1
        for s in free_shape:
            nel *= s
        words = (nel * DT_SIZE[dt] + 31) // 32 * 8
        assert self.off + words <= self.words, "SBUF arena overflow at %s (%d + %d > %d)" % (name, self.off, words, self.words)
        ap = self.t[:, self.off:self.off + words]
        if dt != F32:
            ap = ap.bitcast(dt)
        ap = ap[:, 0:nel]
        if len(free_shape) == 2:
            ap = ap.rearrange("p (a b) -> p a b", b=free_shape[1])
        elif len(free_shape) == 3:
            ap = ap.rearrange("p (a b c) -> p a b c", b=free_shape[1], c=free_shape[2])
        elif len(free_shape) == 4:
            ap = ap.rearrange("p (a b c d) -> p a b c d", b=free_shape[1], c=free_shape[2], d=free_shape[3])
        self.off += words
        self.uid += 1
        return V(ap, [(name, self.uid)])

    def mark(self):
        return self.off

    def reset(self, m):
        self.off = m


class Ring:
    def __init__(self, items):
        self.items = items
        self.i = 0

    def next(self):
        v = self.items[self.i % len(self.items)]
        self.i += 1
        return v


def lockstep(gens):
    alive = list(gens)
    while alive:
        for g_ in list(alive):
            try:
                next(g_)
            except StopIteration:
                alive.remove(g_)


class KB:
    def __init__(self, NB, L, CTX, CAP, debug=()):
        self.NB, self.L, self.CTX, self.CAP = NB, L, CTX, CAP
        self.R = L // GW
        self.debug = set(debug)
        self.nc = bass.Bass("TRN2", target_bir_lowering=False)
        self.alt = 0

    def din(self, name, shape, dt=F32):
        return V(self.nc.dram_tensor(name, list(shape), dt, kind="ExternalInput").ap(), [])

    def dscr(self, name, shape, dt):
        kind = "ExternalOutput" if name in self.debug else "Internal"
        return V(self.nc.dram_tensor(name, list(shape), dt, kind=kind).ap(), [(name,)])

    def mm(self, out, lhsT, rhs, start=True, stop=True):
        o, l, r = out.ap, lhsT.ap, rhs.ap
        self.P.op("tensor", lambda e: e.matmul(o, lhsT=l, rhs=r, start=start, stop=stop),
                  reads=_keys(lhsT, rhs), writes=_keys(out))

    def mmr(self, out, lhsT, rhs, start=True, stop=True):
        o, l, r = out.ap, lhsT.ap, rhs.ap
        self.P.op("tensor", lambda e: e.matmul(o, lhsT=l, rhs=r, start=start, stop=stop),
                  reads=_keys(lhsT, rhs), writes=_keys(out))

    def tr(self, out, in_, ident):
        o, i, d = out.ap, in_.ap, ident.ap
        self.P.op("tensor", lambda e: e.transpose(o, i, d), reads=_keys(in_, ident), writes=_keys(out))

    def act(self, out, in_, func, bias=0.0, scale=1.0, eng="scalar"):
        o, i, b, s = out.ap, in_.ap, _a(bias), _a(scale)
        self.P.op("scalar", lambda e: e.activation(out=o, in_=i, func=func, bias=b, scale=s),
                  reads=_keys(in_, bias, scale), writes=_keys(out))

    def ts(self, out, in0, s1, s2, op0, op1=None, eng="vector"):
        o, i, a, b = out.ap, in0.ap, _a(s1), _a(s2)
        if op1 is None:
            fn = lambda e: e.tensor_scalar(out=o, in0=i, scalar1=a, scalar2=None, op0=op0)
        else:
            fn = lambda e: e.tensor_scalar(out=o, in0=i, scalar1=a, scalar2=b, op0=op0, op1=op1)
        self.P.op(eng, fn, reads=_keys(in0, s1, s2), writes=_keys(out))

    def tt(self, out, in0, in1, op, eng="vector"):
        o, a, b = out.ap, in0.ap, in1.ap
        self.P.op(eng, lambda e: e.tensor_tensor(out=o, in0=a, in1=b, op=op), reads=_keys(in0, in1), writes=_keys(out))

    def stt(self, out, in0, scalar, in1, op0, op1):
        o, a, s, b = out.ap, in0.ap, _a(scalar), in1.ap
        self.P.op("vector", lambda e: e.scalar_tensor_tensor(out=o, in0=a, scalar=s, in1=b, op0=op0, op1=op1),
                  reads=_keys(in0, scalar, in1), writes=_keys(out))

    def cp(self, out, in_, eng="vector"):
        o, i = out.ap, in_.ap
        if eng == "scalar":
            self.P.op("scalar", lambda e: e.activation(out=o, in_=i, func=AF.Identity), reads=_keys(in_), writes=_keys(out))
        else:
            self.P.op(eng, lambda e: e.tensor_copy(out=o, in_=i), reads=_keys(in_), writes=_keys(out))

    def evac(self, out, in_):
        self.alt ^= 1
        self.cp(out, in_, eng="scalar" if self.alt else "vector")

    def memset(self, out, val, eng="gpsimd"):
        o = out.ap
        self.P.op(eng, lambda e: e.memset(o, val), writes=_keys(out))

    def scan(self, out, d0, d1, init):
        o, a, b, i = out.ap, d0.ap, d1.ap, _a(init)
        self.P.op("vector", lambda e: e.tensor_tensor_scan(out=o, data0=a, data1=b, initial=i, op0=ALU.mult, op1=ALU.add),
                  reads=_keys(d0, d1, init), writes=_keys(out))

    def dma(self, out, in_, eng="sync", is_output=False):
        o, i = out.ap, in_.ap
        if eng == "gpsimd":
            fn = lambda e: e.dma_start(out=o, in_=i, max_dma_last_dim=4096)
        else:
            fn = lambda e: e.dma_start(out=o, in_=i)
        self.P.dma(eng, fn, reads=_keys(in_), writes=_keys(out), is_output=is_output)

    def scatter(self, out_dram, idx, in_sb):
        o, x, i = out_dram.ap, idx.ap, in_sb.ap
        self.P.dma("gpsimd", lambda e: e.indirect_dma_start(out=o, out_offset=bass.IndirectOffsetOnAxis(ap=x, axis=0),
                                                            in_=i, in_offset=None),
                   reads=_keys(idx, in_sb), writes=_keys(out_dram))

    def gather(self, out_sb, in_dram, idx):
        o, x, i = out_sb.ap, idx.ap, in_dram.ap
        self.P.dma("gpsimd", lambda e: e.indirect_dma_start(out=o, out_offset=None, in_=i,
                                                            in_offset=bass.IndirectOffsetOnAxis(ap=x, axis=0)),
                   reads=_keys(idx, in_dram), writes=_keys(out_sb))

    def bank(self):
        return self.banks.next()

    def ln_stats(self, x):
        st = self.st_ring.next()
        mv = st[:, 12:14]
        self.P.op("vector", (lambda o, i: lambda e: e.bn_stats(out=o, in_=i))(st.ap[:, 0:6], x.ap[:, 0:512]),
                  reads=_keys(x), writes=_keys(st))
        self.P.op("vector", (lambda o, i: lambda e: e.bn_stats(out=o, in_=i))(st.ap[:, 6:12], x.ap[:, 512:1024]),
                  reads=_keys(x, st), writes=_keys(st))
        self.P.op("vector", (lambda o, i: lambda e: e.bn_aggr(out=o, in_=i))(mv.ap, st.ap[:, 0:12]),
                  reads=_keys(st), writes=_keys(st))
        self.act(st[:, 14:15], st[:, 13:14], AF.Sqrt, bias=LN_EPS)
        self.recip(st[:, 15:16], st[:, 14:15])
        return st[:, 12:13], st[:, 15:16]

    def recip(self, out, in_):
        o, i = out.ap, in_.ap
        self.P.op("vector", lambda e: e.reciprocal(out=o, in_=i), reads=_keys(in_), writes=_keys(out))

    def build(self):
        NB, L, CTX, CAP, R = self.NB, self.L, self.CTX, self.CAP, self.R
        nc = self.nc
        NBP = NB + 1 + ((NB + 1) % 2)
        NT = NB * L // 128
        x_d = self.din("x", [NB, L, D])
        ctx_d = self.din("ctx", [NB, CTX, D])
        cT_d = self.din("cT", [128, 8, NBP])
        wmod_d = self.din("w_mod", [D, 6 * D])
        bmodT_d = self.din("bmodT", [128, 16])
        bmodbc_d = self.din("bmod_bc", [128, 4 * D])
        wint_d = self.din("w_in_t", [64, 128, 8, 128])
        winlg_d = self.din("w_in_lg", [128, 8, 32])
        binT_d = self.din("b_inT", [128, 65])
        conva_d = self.din("conv_a", [128, 8, 5])
        lruw_d = self.din("lru_w", [128, 4, 8, 128])
        lrub_d = self.din("lru_b", [128, 4, 8])
        lrulam_d = self.din("lru_lam", [128, 2, 8])
        convq_d = self.din("conv_qkv", [128, 24, 4])
        gdnrows_d = self.din("gdn_rows", [128, 32])
        gdnnw_d = self.din("gdn_nw", [128, 1])
        wpa_d = self.din("w_pa", [D, D])
        wpb_d = self.din("w_pb", [D, D])
        wout_d = self.din("w_out", [D, D])
        lnbc_d = self.din("ln_bc", [128, 4, D])
        wrt_d = self.din("w_rt", [128, 8, 36])
        brt_d = self.din("b_rt_bc", [128, 36])
        weg_d = self.din("w_eg", [NEXP, D, D])
        weu_d = self.din("w_eu", [NEXP, D, D])
        wed_d = self.din("w_ed", [NEXP, D, D])
        out_d = V(nc.dram_tensor("out", [NB * L, D], F32, kind="ExternalOutput").ap(), [("out",)])
        yaT_d = self.dscr("yaT", [NB, 8, 128, L], BF16)
        ybT_d = self.dscr("ybT", [NB, 8, 128, L], BF16)
        sg_d = self.dscr("sg", [NB, 16, 128, L], F32)
        x1_d = self.dscr("x1s", [NB * L, D], F32)
        xg_d = self.dscr("xg", [NEXP * CAP, D], BF16)
        yg_d = self.dscr("yg", [NEXP * CAP, D], F32)
        bcs_d = self.dscr("bcs", [NB, 4, 128, D], F32)
        pq_d = self.dscr("pq", [32, 128, L], F32)
        dbg_d = {}

        st = ExitStack()
        with st:
            self.P = P = Prog(nc, st)
            cap_bytes = 194 * 1024
            self.A = A = Arena(nc, st, cap_bytes)
            banks = []
            for i in range(8):
                t = st.enter_context(nc.psum_tensor("psb%d" % i, [128, 512], F32))
                banks.append(V(t[:, :], [("psb", i)]))
            self.banks = Ring(banks)
            self.r32 = []
            for i in range(4):
                d_ = {}
                for nm in ("N", "M", "X"):
                    t = st.enter_context(nc.sbuf_tensor("r32_%s%d" % (nm, i), [128, 2 * 128], F32))
                    d_[nm] = [V(t[:, j * 128:(j + 1) * 128], [("r32", nm, i, j)]) for j in range(2)]
                self.r32.append(d_)

            ident_f = A.alloc("ident_f", 128)
            ones_f = A.alloc("ones_f", 128)
            mIL = A.alloc("mIL", 128)
            mSL = A.alloc("mSL", 128)
            mIU = A.alloc("mIU", 128)
            mSU = A.alloc("mSU", 128)
            ident_b = A.alloc("ident_b", 128, BF16)
            ones_b = A.alloc("ones_b", 128, BF16)
            mSU_b = A.alloc("mSU_b", 128, BF16)
            self.st_ring = Ring([A.alloc("lnst%d" % i, 16) for i in range(4)])

            def mask(dst, pat_step, cm, op):
                self.memset(dst, 1.0)
                o = dst.ap
                self.P.op("gpsimd", lambda e: e.affine_select(out=o, in_=o, pattern=[[pat_step, 128]], base=0,
                                                               channel_multiplier=cm, compare_op=op, fill=0.0),
                          reads=_keys(dst), writes=_keys(dst))
            self.memset(ones_f, 1.0)
            mask(ident_f, -1, 1, ALU.is_equal)
            mask(mIL, -1, 1, ALU.is_ge)
            mask(mSL, -1, 1, ALU.is_gt)
            mask(mIU, 1, -1, ALU.is_ge)
            mask(mSU, 1, -1, ALU.is_gt)
            self.cp(ident_b, ident_f, eng="gpsimd")
            self.cp(ones_b, ones_f, eng="gpsimd")
            self.cp(mSU_b, mSU, eng="gpsimd")

            modT = A.alloc("modT", (16, NBP))
            binT = A.alloc("binT", 65)
            conva = A.alloc("conva", (8, 5))
            lrub = A.alloc("lrub", (4, 8))
            cch = A.alloc("cch", (2, 8))
            convq = A.alloc("convq", (24, 4))
            gdnrows = A.alloc("gdnrows", 32)
            nega = A.alloc("nega", 16)
            gdnnw = A.alloc("gdnnw", 1)
            lruw = A.alloc("lruw", (4, 8, 128), BF16)
            lru_state = A.alloc("lru_state", (8, 2))
            gdn_state = A.alloc("gdn_state", (8, 2, 128))
            DEST = A.alloc("DEST", (NT, 2), I32)
            WT = A.alloc("WT", (NT, 2))
            CNT = A.alloc("CNT", 32)
            ECAP = A.alloc("ECAP", 32)
            brt = A.alloc("brt", 36)
            for (dst, src) in ((binT, binT_d), (conva, conva_d), (lrub, lrub_d), (cch, lrulam_d), (convq, convq_d),
                               (gdnrows, gdnrows_d), (gdnnw, gdnnw_d), (brt, brt_d)):
                self.dma(dst, src)
            self.dma(lruw, lruw_d, eng="gpsimd")
            self.act(cch, cch, AF.Exp, scale=-1.0)
            self.act(cch, cch, AF.Ln, bias=1.0)
            self.ts(cch, cch, -8.0, None, ALU.mult)
            self.act(nega, gdnrows[:, 16:32], AF.Exp)
            self.ts(nega, nega, -1.0, None, ALU.mult)
            self.memset(CNT, 0.0)
            ec = ECAP.ap
            self.P.op("gpsimd", lambda e: e.iota(ec, pattern=[[CAP, 32]], base=0, channel_multiplier=0,
                                                 allow_small_or_imprecise_dtypes=True), writes=_keys(ECAP))
            zt = A.alloc("zt", D, BF16)
            self.memset(zt, 0.0)
            for e_ in range(NEXP):
                self.dma(V(xg_d.ap[e_ * CAP:(e_ + 1) * CAP, :].rearrange("(n p) d -> p n d", p=128), xg_d.keys),
                         V(zt.ap.unsqueeze(1).to_broadcast([128, CAP // 128, D]), zt.keys))
            base_mark = A.mark()

            cT = A.alloc("cT", (8, NBP))
            self.dma(cT, cT_d)
            sT = A.alloc("sT", (8, NBP))
            self.act(sT, cT, AF.Silu)
            bmodT = A.alloc("bmodT", 16)
            self.dma(bmodT, bmodT_d)
            wmA = A.alloc("wmA", (8, 2048))
            wm_v = wmod_d.re("(k p) n -> p k n", p=128)
            for k in range(8):
                self.dma(wmA[:, k, :].k(k), wm_v[:, k, 0:2048])
            for cc in range(16):
                ps = self.bank()
                for k in range(8):
                    self.mm(ps[:, 0:NBP], wmA[:, k, cc * 128:(cc + 1) * 128].k(k), sT[:, k, :], start=(k == 0), stop=(k == 7))
                self.ts(modT[:, cc, :], ps[:, 0:NBP], bmodT[:, cc:cc + 1], 1.0 if cc >= 8 else 0.0, ALU.add, ALU.add)
            A.reset(A.mark() - 0)
            sbc = [A.alloc("sbc%d" % b, (8, 128)) for b in range(NB)]
            for b in range(NB):
                self.cp(sbc[b], sT[:, :, b:b + 1].bc([128, 8, 128]))
            wmB = Ring([A.alloc("wmB%d" % i, (8, 512)) for i in range(2)])
            bmB = Ring([A.alloc("bmB%d" % i, 512) for i in range(2)])
            stg = Ring([A.alloc("stg%d" % i, 512) for i in range(3)])
            for ct in range(8):
                w = wmB.next()
                bm = bmB.next()
                for k in range(8):
                    self.dma(w[:, k, :].k(k), wm_v[:, k, 2048 + ct * 512:2048 + (ct + 1) * 512])
                self.dma(bm, bmodbc_d[:, ct * 512:(ct + 1) * 512])
                for b in range(NB):
                    ps = self.bank()
                    for k in range(8):
                        self.mm(ps, sbc[b][:, k, :], w[:, k, :].k(k), start=(k == 0), stop=(k == 7))
                    sg_ = stg.next()
                    if ct // 2 == 2:
                        self.stt(sg_, ps, 1.0, bm, ALU.add, ALU.add)
                    else:
                        self.tt(sg_, ps, bm, ALU.add)
                    self.dma(bcs_d[b, ct // 2, :, (ct % 2) * 512:(ct % 2 + 1) * 512].k(b, ct), sg_)
            self.P.barrier()
            A.reset(base_mark)

            import os as _os
            KSTOP = int(_os.environ.get("KSTOP", "99"))
            for b in range(NB if KSTOP > 0 else 0):
                for which in ("ctx", "lat"):
                    if KSTOP == 1 and which == "lat":
                        continue
                    tabs, tab_mark = self.mixer(b, which, locals())
                    self.P.barrier()
                    A.reset(tab_mark)
                    if KSTOP > 2:
                        self.gdn(b, which == "lat", tabs, locals())
                    self.P.barrier()
                    A.reset(base_mark)
                if KSTOP > 3:
                    self.merge(b, locals())
                self.P.barrier()
                A.reset(base_mark)
            if KSTOP > 4:
                self.experts(locals())
            self.P.barrier()
            A.reset(base_mark)
            if KSTOP > 5:
                self.combine(locals())
            P.finish()
            with nc.Block() as block:
                P.emit(block)
        return nc

    def load_wchunk(self, wring, wint_d, c):
        w = wring.next()
        self.dma(w, wint_d[c], eng="gpsimd")
        return w

    def proj(self, w, M, hT, Lx, evac_fn):
        TL = min(512, Lx)
        for t in range(Lx // TL):
            ps = self.bank()
            for k in range(8):
                self.mm(ps[0:M, 0:TL], w[:, k, 0:M], hT[:, k, t * TL:(t + 1) * TL], start=(k == 0), stop=(k == 7))
            evac_fn(t, TL, ps[0:M, 0:TL])

    def mixer(self, b, which, E):
        A, P = self.A, self.P
        NB, L, CTX, R = self.NB, self.L, self.CTX, self.R
        lat = which == "lat"
        Lx = L if lat else CTX
        nch = Lx // 128
        NH = nch * 8
        col = b if lat else NB
        src = E["x_d"][b] if lat else E["ctx_d"][b]
        modT, binT, ident_f, ident_b, ones_f, ones_b = E["modT"], E["binT"], E["ident_f"], E["ident_b"], E["ones_f"], E["ones_b"]
        mIL, mIU = E["mIL"], E["mIU"]
        gdnrows, nega = E["gdnrows"], E["nega"]
        wint_d = E["wint_d"]
        tabs = {}
        for nm in ("BT", "NBT", "GC", "EKG", "GL", "BEG"):
            tabs[nm] = A.alloc(nm, (2, nch, 8))
        tab_mark = A.mark()
        hT = A.alloc("hT", (8, Lx), BF16)
        m0 = A.mark()
        xin = Ring([A.alloc("xin%d" % i, D) for i in range(2)])
        xnr = Ring([A.alloc("xn%d" % i, D) for i in range(2)])
        def ht_gen(t):
            xt = xin.next()
            self.dma(xt, src[t * 128:(t + 1) * 128, :])
            yield
            mean, rstd = self.ln_stats(xt)
            yield
            xn = xnr.next()
            self.ts(xn, xt, mean, rstd, ALU.subtract, ALU.mult)
            yield
            for half in range(2):
                ps = self.bank()
                for kk in range(4):
                    k = half * 4 + kk
                    self.tr(ps[:, kk * 128:(kk + 1) * 128], xn[:, k * 128:(k + 1) * 128], ident_f)
                yield
                for kk in range(4):
                    k = half * 4 + kk
                    o = hT[:, k, t * 128:(t + 1) * 128].k(t)
                    if kk % 2 == 0:
                        self.act(o, ps[:, kk * 128:(kk + 1) * 128], AF.Identity, bias=modT[:, k, col:col + 1], scale=modT[:, 8 + k, col:col + 1])
                    else:
                        self.ts(o, ps[:, kk * 128:(kk + 1) * 128], modT[:, 8 + k, col:col + 1], modT[:, k, col:col + 1], ALU.mult, ALU.add)
                yield

        for t2 in range(0, Lx // 128, 2):
            lockstep([ht_gen(t2), ht_gen(t2 + 1)])
        hT = V(hT.ap, [kk + (t,) for kk in hT.keys for t in range(Lx // 128)])
        self.P.barrier()
        A.reset(m0)
        wring = Ring([A.alloc("wch%d" % i, (8, 128), BF16) for i in range(2)])
        m1 = A.mark()

        conva, lrub, cch, lruw, lru_state = E["conva"], E["lrub"], E["cch"], E["lruw"], E["lru_state"]
        xa_pad = A.alloc("xa_pad", Lx + 3)
        u = A.alloc("u", Lx)
        u_bf = A.alloc("u_bf", Lx, BF16)
        Ab = A.alloc("Abuf", Lx)
        Ib = A.alloc("Ibuf", Lx)
        HF = A.alloc("HF", Lx)
        Tb = V(xa_pad.ap[:, 0:Lx], xa_pad.keys)
        HB = Tb
        YA = V(Ib.ap.bitcast(BF16)[:, 0:Lx], Ib.keys)
        TL = min(512, Lx)
        for n in range(8):
            w = self.load_wchunk(wring, wint_d, n)
            self.memset(xa_pad[:, 0:1], 0.0)
            self.memset(xa_pad[:, Lx + 1:Lx + 3], 0.0)
            self.proj(w, 128, hT, Lx, lambda t, tl, ps: self.act(xa_pad[:, 1 + t * tl:1 + (t + 1) * tl], ps, AF.Identity,
                                                                    bias=binT[:, n:n + 1]))
            self.ts(u, xa_pad[:, 0:Lx], conva[:, n, 0:1], conva[:, n, 4:5], ALU.mult, ALU.add)
            for j in range(1, 4):
                self.stt(u, xa_pad[:, j:j + Lx], conva[:, n, j:j + 1], u, ALU.mult, ALU.add)
            self.cp(u_bf, u, eng="scalar")
            for d in range(2):
                for t in range(Lx // TL):
                    sl = slice(t * TL, (t + 1) * TL)
                    ps = self.bank()
                    self.mm(ps[:, 0:TL], lruw[:, 2 * d, n, :], u_bf[:, sl])
                    self.act(Ab[:, sl], ps[:, 0:TL], AF.Sigmoid, bias=lrub[:, 2 * d, n:n + 1])
                    ps2 = self.bank()
                    self.mm(ps2[:, 0:TL], lruw[:, 2 * d + 1, n, :], u_bf[:, sl])
                    self.act(Ib[:, sl], ps2[:, 0:TL], AF.Sigmoid, bias=lrub[:, 2 * d + 1, n:n + 1])
                self.act(Ab, Ab, AF.Exp, scale=cch[:, d, n:n + 1])
                self.tt(Tb, Ab, Ab, ALU.mult, eng="gpsimd")
                self.ts(Tb, Tb, 0.99999994, -1.0, ALU.min, ALU.mult)
                self.act(Tb, Tb, AF.Sqrt, bias=1.0)
                self.tt(Ib, Ib, u, ALU.mult, eng="gpsimd")
                self.tt(Ib, Ib, Tb, ALU.mult)
                init = lru_state[:, n, d:d + 1] if lat else 0.0
                if d == 0:
                    self.scan(HF, Ab, Ib, init)
                else:
                    self.scan(HB[:, ::-1], Ab[:, ::-1], Ib[:, ::-1], init)
            if not lat:
                self.cp(lru_state[:, n, 0:1], HF[:, Lx - 1:Lx])
                self.cp(lru_state[:, n, 1:2], HB[:, 0:1])
            else:
                self.tt(HF, HF, HB, ALU.add, eng="gpsimd")
                w = self.load_wchunk(wring, wint_d, 8 + n)
                self.proj(w, 128, hT, Lx, lambda t, tl, ps: self.act(Ab[:, t * tl:(t + 1) * tl], ps, AF.Identity,
                                                                        bias=binT[:, 8 + n:9 + n]))
                self.tt(Tb, Ab, Ab, ALU.mult, eng="gpsimd")
                self.ts(Tb, Tb, 0.044715, 1.0, ALU.mult, ALU.add)
                self.tt(Tb, Tb, Ab, ALU.mult)
                self.act(Tb, Tb, AF.Sigmoid, scale=2.0 * math.sqrt(2.0 / math.pi))
                self.tt(Tb, Tb, Ab, ALU.mult, eng="gpsimd")
                self.tt(YA, Tb, HF, ALU.mult)
                self.dma(E["yaT_d"][b, n].k(b, n), YA)
        self.P.barrier()
        A.reset(m1)

        SG = Ring([A.alloc("SG%d" % i, Lx) for i in range(2)])
        if lat:
            for dc in range(16):
                w = self.load_wchunk(wring, wint_d, 48 + dc)
                sgb = SG.next()
                self.proj(w, 128, hT, Lx, lambda t, tl, ps: self.act(sgb[:, t * tl:(t + 1) * tl], ps, AF.Sigmoid,
                                                                        bias=binT[:, 48 + dc:49 + dc]))
                self.dma(E["sg_d"][b, dc].k(b, dc), sgb)
        pq = E["pq_d"]
        for c in range(16, 48 if lat else 40):
            w = self.load_wchunk(wring, wint_d, c)
            sgb = SG.next()
            func = AF.Silu if c >= 40 else AF.Identity
            self.proj(w, 128, hT, Lx, lambda t, tl, ps: self.act(self.cm_out(sgb, 0, Lx, lat, t, tl), self.cm_in(ps, lat),
                                                                    func, bias=binT[:, c:c + 1]))
            self.dma(V(pq.ap[c - 16][:, 0:Lx], [kk + (c,) for kk in pq.keys]), sgb)
        self.P.barrier()
        A.reset(m1)

        wlg = A.alloc("wlg", (8, 32), BF16)
        self.dma(wlg, E["winlg_d"], eng="gpsimd")
        lgT = A.alloc("lgT", Lx)
        for t in range(Lx // TL):
            ps = self.bank()
            for k in range(8):
                self.mm(ps[0:32, 0:TL], wlg[:, k, :], hT[:, k, t * TL:(t + 1) * TL], start=(k == 0), stop=(k == 7))
            self.act(self.cm_out(lgT, 0, Lx, lat, t, TL)[0:32], self.cm_in(ps[0:32, 0:TL], lat), AF.Identity, bias=binT[0:32, 64:65])
        lg_tm = A.alloc("lg_tm", (nch, 32))
        for n in range(nch):
            ps = self.bank()
            self.tr(ps[:, 0:32], lgT[0:32, n * 128:(n + 1) * 128], ident_f[0:32, 0:32])
            self.evac(lg_tm[:, n, :], ps[:, 0:32])
        XG = A.alloc("XG", (2, nch, 8))
        AXb = A.alloc("AXb", (2, nch, 8))
        Gt = A.alloc("Gt", (2, nch, 8))
        GTOT = A.alloc("GTOT", (2, nch, 8))
        BT, NBT, GC, EKG, GL, BEG = (tabs[nm] for nm in ("BT", "NBT", "GC", "EKG", "GL", "BEG"))
        lg4 = lg_tm.re("p n (j h) -> p j n h", h=8)
        dtb = gdnrows[:, 0:16].re("p (d h) -> p d h", h=8)
        self.tt(XG, lg4[:, 0:2], V(dtb.ap.unsqueeze(2).to_broadcast([128, 2, nch, 8]), dtb.keys), ALU.add)
        self.act(AXb, XG, AF.Abs)
        self.act(AXb, AXb, AF.Exp, scale=-1.0)
        self.act(AXb, AXb, AF.Ln, bias=1.0)
        self.stt(XG, XG, 0.0, AXb, ALU.max, ALU.add)
        ng = nega.re("p (d h) -> p d h", h=8)
        self.tt(Gt, XG, V(ng.ap.unsqueeze(2).to_broadcast([128, 2, nch, 8]), ng.keys), ALU.mult)
        self.act(BT, lg4[:, 2:4], AF.Sigmoid)
        self.ts(NBT, BT, -1.0, None, ALU.mult)
        for d in range(2):
            ps = self.bank()
            tri = mIU if d == 0 else mIL
            self.mm(ps[:, 0:NH], tri, Gt[:, d].re("p n h -> p (n h)"))
            self.evac(GC[:, d].re("p n h -> p (n h)"), ps[:, 0:NH])
            ps = self.bank()
            self.mm(ps[:, 0:NH], ones_f, Gt[:, d].re("p n h -> p (n h)"))
            self.evac(GTOT[:, d].re("p n h -> p (n h)"), ps[:, 0:NH])
        self.tt(EKG, GTOT, GC, ALU.subtract)
        self.act(EKG, EKG, AF.Exp)
        self.act(GL, GTOT, AF.Exp)
        self.act(BEG, GC, AF.Exp)
        self.tt(BEG, BEG, BT, ALU.mult)
        return tabs, tab_mark

    def cm_out(self, buf, off, Lx, lat, t, tl):
        if not lat:
            return buf[:, off + t * tl:off + (t + 1) * tl]
        rows = tl // GW
        v = buf[:, off:off + Lx].re("p (c r) -> p r c", c=GW)
        return v[:, t * rows:(t + 1) * rows, :]

    def cm_in(self, ps, lat):
        if not lat:
            return ps
        return ps.re("p (r c) -> p r c", c=GW)

    def gdn(self, b, lat, tabs, E):
        A, P = self.A, self.P
        NB, R = self.NB, self.R
        Lx = self.L if lat else self.CTX
        nch = Lx // 128
        TL = min(512, Lx)
        binT, ident_f, ident_b, ones_f, ones_b = E["binT"], E["ident_f"], E["ident_b"], E["ones_f"], E["ones_b"]
        mIL, mSL, mIU, mSU = E["mIL"], E["mSL"], E["mIU"], E["mSU"]
        gdnnw, convq, gdn_state = E["gdnnw"], E["convq"], E["gdn_state"]
        BT, NBT, GC, EKG, GL, BEG = (tabs[nm] for nm in ("BT", "NBT", "GC", "EKG", "GL", "BEG"))
        pq = E["pq_d"]
        if not lat:
            self.memset(gdn_state, 0.0)
        PAD = A.alloc("PAD", Lx + 16)
        CV = A.alloc("CV", Lx)
        qT = A.alloc("qT", Lx, BF16)
        kT = A.alloc("kT", Lx, BF16)
        vT = A.alloc("vT", Lx, BF16)
        OT = A.alloc("OT", Lx)
        OTa = V(OT.ap, [kk + (n,) for kk in OT.keys for n in range(nch)])
        k_tm = A.alloc("k_tm", (nch, 128), BF16)
        v_tm = A.alloc("v_tm", (nch, 128), BF16)
        YB = V(PAD.ap[:, 0:Lx // 2].bitcast(BF16), PAD.keys)
        SQr = Ring([A.alloc("SQ%d" % i, TL, BF16) for i in range(2)])
        RTr = Ring([A.alloc("RT%d" % i, TL) for i in range(2)])
        S_bf = [A.alloc("S_bf%d" % d, 128, BF16) for d in range(2)]

        GC_ = 2 if nch % 2 == 0 else 1
        NI = 2 * GC_
        TMP = [{nm: A.alloc("%s_t%d" % (nm, i), 128) for nm in ("DG", "M1", "M2", "ER", "KB", "VB")} for i in range(NI)]
        BFR = [{nm: Ring([A.alloc("%s_b%d_%d" % (nm, i, j), 128, BF16) for j in range(2)]) for nm in ("N", "M", "X")} for i in range(NI)]
        MB1 = [A.alloc("MB1_%d" % d, 128) for d in range(2)]
        MB2 = [A.alloc("MB2_%d" % d, 128) for d in range(2)]
        self.ts(MB1[0], mSL, -1e4, 1e4, ALU.mult, ALU.add, eng="gpsimd")
        self.ts(MB1[1], mSU, -1e4, 1e4, ALU.mult, ALU.add, eng="gpsimd")
        self.ts(MB2[0], mIU, 1e4, -1e4, ALU.mult, ALU.add, eng="gpsimd")
        self.ts(MB2[1], mIL, 1e4, -1e4, ALU.mult, ALU.add, eng="gpsimd")
        RNG = [{nm: Ring(self.r32[i][nm]) for nm in ("N", "M", "X")} for i in range(NI)]
        OUTT = [[{nm: A.alloc("%s_o%d_%d" % (nm, par, i), 128, F32 if nm == "UC" else BF16)
                  for nm in ("ACT", "WCT", "UC", "QG", "KG", "VN")} for i in range(NI)] for par in range(2)]

        import os as _os
        KG = int(_os.environ.get("KG", "99"))
        for h in range(8 if KG > 0 else 0):
            for (qi, dst) in ((0, qT), (1, kT), (2, vT)):
                c = 16 + qi * 8 + h
                self.memset(PAD[:, 7:8], 0.0)
                self.memset(PAD[:, Lx + 8:Lx + 10], 0.0)
                self.dma(PAD[:, 8:8 + Lx], V(pq.ap[c - 16][:, 0:Lx], [kk + (c,) for kk in pq.keys]))
                cw = convq[:, qi * 8 + h, :]
                self.ts(CV, PAD[:, 7:7 + Lx], cw[:, 0:1], None, ALU.mult)
                for j in range(1, 4):
                    self.stt(CV, PAD[:, 7 + j:7 + j + Lx], cw[:, j:j + 1], CV, ALU.mult, ALU.add)
                self.act(CV, CV, AF.Silu)
                if qi < 2:
                    for t in range(Lx // TL):
                        sl = slice(t * TL, (t + 1) * TL)
                        sq, rt = SQr.next(), RTr.next()
                        self.tt(sq, CV[:, sl], CV[:, sl], ALU.mult, eng="gpsimd")
                        ps = self.bank()
                        self.mm(ps[:, 0:TL], ones_b, sq)
                        self.act(rt, ps[:, 0:TL], AF.Sqrt, bias=1e-6)
                        self.recip(rt, rt)
                        self.stt(dst[:, sl], CV[:, sl], (128.0 ** -0.5) if qi == 0 else 1.0, rt, ALU.mult, ALU.mult)
                else:
                    self.cp(dst, CV, eng="scalar")
            if KG < 2:
                continue
            for n in range(nch):
                ps = self.bank()
                self.mm(ps[:, 0:128], kT[:, n * 128:(n + 1) * 128], ident_b)
                self.mm(ps[:, 128:256], vT[:, n * 128:(n + 1) * 128], ident_b)
                self.cp(k_tm[:, n, :].k(n), ps[:, 0:128], eng="scalar")
                self.cp(v_tm[:, n, :].k(n), ps[:, 128:256], eng="vector")
            self.memset(OTa, 0.0)
            for d in range(2):
                self.cp(S_bf[d], gdn_state[:, h, d, :], eng="scalar")
            ngroups = nch // GC_

            def group_insts(p):
                L_ = []
                for j in range(GC_):
                    for d in range(2):
                        s_ = p * GC_ + j
                        n = s_ if d == 0 else nch - 1 - s_
                        i = j * 2 + d
                        L_.append({"d": d, "n": n, "cs": slice(n * 128, (n + 1) * 128), "T": TMP[i], "R": RNG[i],
                                   "B": BFR[i], "O": OUTT[p % 2][i]})
                return L_

            def pre(insts):
                for I in insts:
                    d, n, T = I["d"], I["n"], I["T"]
                    ktn, vtn = k_tm[:, n, :].k(n), v_tm[:, n, :].k(n)
                    I["Gp"] = GC[:, d, n, h:h + 1]
                    self.act(T["KB"], ktn, AF.Identity, scale=BEG[:, d, n, h:h + 1])
                    self.act(I["O"]["KG"], ktn, AF.Identity, scale=EKG[:, d, n, h:h + 1])
                    self.act(T["VB"], vtn, AF.Identity, scale=BT[:, d, n, h:h + 1])
                    self.ts(T["DG"], ident_f, I["Gp"], None, ALU.mult, eng="gpsimd")
                for I in insts:
                    I["psg"] = self.bank()
                    self.mm(I["psg"][:, 0:128], ones_f, I["T"]["DG"])
                for I in insts:
                    T, psg = I["T"], I["psg"]
                    self.stt(T["M1"], psg[:, 0:128], I["Gp"], MB1[I["d"]], ALU.subtract, ALU.max)
                    self.stt(T["M2"], psg[:, 0:128], I["Gp"], MB2[I["d"]], ALU.subtract, ALU.min)
                    self.act(T["ER"], psg[:, 0:128], AF.Exp)
                for I in insts:
                    T = I["T"]
                    self.act(T["M1"], T["M1"], AF.Exp, scale=-1.0)
                    self.act(T["M2"], T["M2"], AF.Exp)
                for I in insts:
                    cs = I["cs"]
                    I["psk"] = self.bank()
                    self.mm(I["psk"][:, 0:128], kT[:, cs], kT[:, cs])
                    self.mm(I["psk"][:, 128:256], kT[:, cs], qT[:, cs])
                for I in insts:
                    T, d, n = I["T"], I["d"], I["n"]
                    I["Nm"], I["Mm"], I["Xm"] = I["R"]["N"].next(), I["R"]["M"].next(), I["R"]["X"].next()
                    self.stt(I["Nm"], I["psk"][:, 0:128], NBT[:, d, n, h:h + 1], T["M1"], ALU.mult, ALU.mult)
                    self.tt(I["O"]["ACT"], I["psk"][:, 128:256], T["M2"], ALU.mult)
                for I in insts:
                    I["pst"] = self.bank()
                    self.tr(I["pst"][:, 0:128], I["Nm"].cast(F32), ident_f)
                for I in insts:
                    self.cp(I["Mm"], I["pst"][:, 0:128], eng="scalar")
                    self.tt(I["Xm"], I["Mm"].cast(F32), ident_f, ALU.add)
                    I["Pm"], I["PTm"] = I["Mm"], I["Nm"]
                    I["Xop"] = I["Xm"]
                for lvl in range(6):
                    last = lvl == 5
                    hi = lvl >= HI_LVL
                    nxt_hi = lvl >= HI_LVL - 1
                    for I in insts:
                        I["psl"] = self.bank()
                        if not last:
                            self.mm(I["psl"][:, 0:128], I["PTm"], I["Pm"])
                        self.mm(I["psl"][:, 128:256], I["Pm"], I["PTm"])
                    for I in insts:
                        if hi:
                            I["PT2"] = I["B"]["N"].next()
                            self.cp(I["PT2"], I["psl"][:, 128:256], eng="scalar")
                            I["PT2n"] = I["PT2"]
                        else:
                            I["PT2"] = I["R"]["N"].next()
                            self.cp(I["PT2"], I["psl"][:, 128:256], eng="scalar")
                            I["PT2n"] = I["PT2"]
                            if nxt_hi:
                                I["PT2n"] = I["B"]["N"].next()
                                self.cp(I["PT2n"], I["psl"][:, 128:256], eng="scalar")
                        if not last:
                            I["P2"] = (I["B"]["M"] if nxt_hi else I["R"]["M"]).next()
                            self.cp(I["P2"], I["psl"][:, 0:128], eng="vector")
                    for I in insts:
                        I["psx"] = self.bank()
                        self.mm(I["psx"][:, 0:128], I["PT2"], I["Xop"])
                    for I in insts:
                        X2 = I["R"]["X"].next()
                        self.tt(X2, I["psx"][:, 0:128], I["Xm"], ALU.add)
                        I["Xm"] = X2
                        I["Xop"] = X2
                        if nxt_hi and not last:
                            I["Xop"] = I["B"]["X"].next()
                            self.cp(I["Xop"], X2, eng="scalar")
                        I["PTm"] = I["PT2n"]
                        if not last:
                            I["Pm"] = I["P2"]
                for I in insts:
                    I["psw"] = self.bank()
                    self.mm(I["psw"][:, 0:128], I["T"]["KB"], I["Xm"].cast(F32))
                    self.mm(I["psw"][:, 128:256], I["Xm"].cast(F32), I["T"]["VB"])
                for I in insts:
                    self.cp(I["O"]["WCT"], I["psw"][:, 0:128], eng="scalar")
                    self.cp(I["O"]["UC"], I["psw"][:, 128:256], eng="vector")
                    self.tt(I["O"]["QG"], qT[:, I["cs"]], I["T"]["ER"], ALU.mult, eng="gpsimd")

            def state(insts):
                for I in insts:
                    d, n, cs, O = I["d"], I["n"], I["cs"], I["O"]
                    Sst = gdn_state[:, h, d, :]
                    pss = self.bank()
                    self.mm(pss[:, 0:128], O["WCT"], S_bf[d])
                    self.tt(O["VN"], O["UC"], pss[:, 0:128], ALU.subtract)
                    pso = self.bank()
                    self.mm(pso[:, 0:128], S_bf[d], O["QG"], start=True, stop=False)
                    self.mm(pso[:, 0:128], O["VN"], O["ACT"], start=False, stop=True)
                    psu = self.bank()
                    self.mm(psu[:, 0:128], O["KG"], O["VN"])
                    OTn = V(OT.ap[:, cs], [kk + (n,) for kk in OT.keys])
                    self.tt(OTn, OTn, pso[:, 0:128], ALU.add)
                    self.stt(Sst, Sst, GL[:, d, n, h:h + 1], psu[:, 0:128], ALU.mult, ALU.add)
                    self.cp(S_bf[d], Sst, eng="scalar")

            groups = [group_insts(p) for p in range(ngroups)]
            pre(groups[0])
            for p in range(ngroups):
                if p + 1 < ngroups:
                    pre(groups[p + 1])
                state(groups[p])
            if lat and KG > 6:
                c = 40 + h
                self.dma(CV, V(pq.ap[c - 16][:, 0:Lx], [kk + (c,) for kk in pq.keys]))
                for t in range(Lx // TL):
                    sl = slice(t * TL, (t + 1) * TL)
                    sq, rt = SQr.next(), RTr.next()
                    self.tt(sq, OTa[:, sl], OTa[:, sl], ALU.mult, eng="gpsimd")
                    ps = self.bank()
                    self.mm(ps[:, 0:TL], ones_b, sq)
                    self.act(rt, ps[:, 0:TL], AF.Sqrt, bias=LN_EPS, scale=1.0 / 128.0)
                    self.recip(rt, rt)
                    self.tt(CV[:, sl], CV[:, sl], rt, ALU.mult, eng="gpsimd")
                self.stt(YB.re("p (r c) -> p c r", c=GW), OTa.re("p (c r) -> p c r", c=GW), gdnnw[:, 0:1],
                         CV.re("p (c r) -> p c r", c=GW), ALU.mult, ALU.mult)
                self.dma(E["ybT_d"][b, h].k(b, h), YB)

    def merge(self, b, E):
        A, P = self.A, self.P
        NB, L, CAP = self.NB, self.L, self.CAP
        ident_f, ones_b, mSU_b = E["ident_f"], E["ones_b"], E["mSU_b"]
        DEST, WT, CNT, ECAP, brt = E["DEST"], E["WT"], E["CNT"], E["ECAP"], E["brt"]
        wpa = A.alloc("wpa", (8, D), BF16)
        wpb = A.alloc("wpb", (8, D), BF16)
        wout = A.alloc("wout", (8, D), BF16)
        for (dst, src) in ((wpa, E["wpa_d"]), (wpb, E["wpb_d"]), (wout, E["wout_d"])):
            sv = src.re("(k p) n -> p k n", p=128)
            for k in range(8):
                self.dma(dst[:, k, :].k(k), sv[:, k, :], eng="gpsimd")
        wrt = A.alloc("wrt", (8, 36))
        self.dma(wrt, E["wrt_d"])
        g1bc = A.alloc("g1bc", D)
        sh2bc = A.alloc("sh2bc", D)
        sc2bc = A.alloc("sc2bc", D)
        ln1g = A.alloc("ln1g", D)
        ln1b = A.alloc("ln1b", D)
        bcs = E["bcs_d"]
        bk = [kk + (b, ct) for kk in bcs.keys for ct in range(8)]
        self.dma(g1bc, V(bcs.ap[b, 0], bk))
        self.dma(sh2bc, V(bcs.ap[b, 1], bk))
        self.dma(sc2bc, V(bcs.ap[b, 2], bk))
        self.dma(ln1g, E["lnbc_d"][:, 0, :])
        self.dma(ln1b, E["lnbc_d"][:, 1, :])
        YAt = Ring([A.alloc("YAt%d" % i, (8, 512), BF16) for i in range(1)])
        YBt = Ring([A.alloc("YBt%d" % i, (8, 512), BF16) for i in range(1)])
        SGa = Ring([A.alloc("SGa%d" % i, 512) for i in range(3)])
        SGb = Ring([A.alloc("SGb%d" % i, 512) for i in range(3)])
        M1r = Ring([A.alloc("M1r%d" % i, 512) for i in range(2)])
        M2r = Ring([A.alloc("M2r%d" % i, 512) for i in range(2)])
        MTr = Ring([A.alloc("MT%d" % i, (8, 512), BF16) for i in range(1)])
        T1r = Ring([A.alloc("T1%d" % i, D) for i in range(2)])
        Xtr = Ring([A.alloc("Xt%d" % i, D) for i in range(2)])
        X1r = Ring([A.alloc("X1%d" % i, D) for i in range(2)])
        H2r = Ring([A.alloc("H2%d" % i, D) for i in range(2)])
        H2br = Ring([A.alloc("H2b%d" % i, D, BF16) for i in range(3)])
        H2Tr = Ring([A.alloc("H2T%d" % i, (8, 128)) for i in range(2)])
        sm = Ring([A.alloc("rsm%d" % i, 256) for i in range(3)])
        yav = E["yaT_d"]
        ybv = E["ybT_d"]
        sgv = E["sg_d"]
        TLm = 512
        for g in range(L // TLm):
            ts_ = slice(g * TLm, (g + 1) * TLm)
            ya, yb = YAt.next(), YBt.next()
            for k in range(8):
                self.dma(ya[:, k, :].k(k), V(yav.ap[b, k][:, ts_], [kk + (b, k) for kk in yav.keys]))
                self.dma(yb[:, k, :].k(k), V(ybv.ap[b, k][:, ts_], [kk + (b, k) for kk in ybv.keys]))
            MT = MTr.next()
            for dc in range(8):
                sga, sgb = SGa.next(), SGb.next()
                self.dma(sga, V(sgv.ap[b, dc][:, ts_], [kk + (b, dc) for kk in sgv.keys]))
                self.dma(sgb, V(sgv.ap[b, 8 + dc][:, ts_], [kk + (b, 8 + dc) for kk in sgv.keys]))
                psa = self.bank()
                for k in range(8):
                    self.mm(psa, wpa[:, k, dc * 128:(dc + 1) * 128].k(k), ya[:, k, :].k(k), start=(k == 0), stop=(k == 7))
                psb_ = self.bank()
                for k in range(8):
                    self.mm(psb_, wpb[:, k, dc * 128:(dc + 1) * 128].k(k), yb[:, k, :].k(k), start=(k == 0), stop=(k == 7))
                m1, m2 = M1r.next(), M2r.next()
                self.tt(m1, psa, sga, ALU.mult)
                self.tt(m2, psb_, sgb, ALU.mult)
                self.tt(MT[:, dc, :].k(dc), m1, m2, ALU.add, eng="gpsimd")
            MTa = V(MT.ap, [kk + (dc,) for kk in MT.keys for dc in range(8)])
            def sub_gen(sub):
                    tile = (b * L + g * TLm) // 128 + sub
                    tok0 = g * TLm + sub * 128
                    T1 = T1r.next()
                    for half in range(2):
                        ps = self.bank()
                        for dc in range(8):
                            self.mm(ps, MTa[:, dc, sub * 128:(sub + 1) * 128], wout[:, dc, half * 512:(half + 1) * 512].k(dc),
                                    start=(dc == 0), stop=(dc == 7))
                        self.tt(T1[:, half * 512:(half + 1) * 512].k(half), ps, g1bc[:, half * 512:(half + 1) * 512], ALU.mult)
                    T1a = V(T1.ap, [kk + (hh,) for kk in T1.keys for hh in range(2)])
                    yield
                    xt = Xtr.next()
                    self.dma(xt, E["x_d"][b][tok0:tok0 + 128, :])
                    self.stt(T1a, xt, ALPHA, T1a, ALU.mult, ALU.add)
                    yield
                    mean, rstd = self.ln_stats(T1a)
                    self.ts(T1a, T1a, mean, rstd, ALU.subtract, ALU.mult)
                    yield
                    X1 = X1r.next()
                    self.tt(X1, T1a, ln1g, ALU.mult, eng="gpsimd")
                    self.tt(X1, X1, ln1b, ALU.add, eng="gpsimd")
                    yield
                    self.dma(V(E["x1_d"].ap[tile * 128:(tile + 1) * 128, :], [kk + (tile,) for kk in E["x1_d"].keys]), X1)
                    yield from self.route(tile, X1, sh2bc, sc2bc, wrt, H2r, H2br, H2Tr, sm, E)
            lockstep([sub_gen(0), sub_gen(1)])
            lockstep([sub_gen(2), sub_gen(3)])

    def route(self, tile, X1, sh2bc, sc2bc, wrt, H2r, H2br, H2Tr, sm, E):
        CAP = self.CAP
        ident_f, ones_b, mSU_b = E["ident_f"], E["ones_b"], E["mSU_b"]
        DEST, WT, CNT, ECAP, brt = E["DEST"], E["WT"], E["CNT"], E["ECAP"], E["brt"]
        mean, rstd = self.ln_stats(X1)
        yield
        H2 = H2r.next()
        self.ts(H2, X1, mean, rstd, ALU.subtract, ALU.mult)
        self.tt(H2, H2, sc2bc, ALU.mult, eng="gpsimd")
        self.tt(H2, H2, sh2bc, ALU.add, eng="gpsimd")
        yield
        H2b = H2br.next()
        self.cp(H2b, H2, eng="scalar")
        H2T = H2Tr.next()
        for half in range(2):
            ps = self.bank()
            for kk in range(4):
                k = half * 4 + kk
                self.tr(ps[:, kk * 128:(kk + 1) * 128], H2[:, k * 128:(k + 1) * 128], ident_f)
            self.evac(H2T[:, half * 4:(half + 1) * 4, :].k(half), ps.re("p (a b) -> p a b", b=128))
        yield
        H2Ta = V(H2T.ap, [kk + (hh,) for kk in H2T.keys for hh in range(2)])
        psr = self.bank()
        for k in range(8):
            self.mm(psr[:, 0:36], H2Ta[:, k, :], wrt[:, k, :], start=(k == 0), stop=(k == 7))
        s = sm.next()
        LG = s[:, 0:36]
        gmax, ngmax, gsum, pg = s[:, 36:37], s[:, 37:38], s[:, 38:39], s[:, 39:40]
        GE, OHG = s[:, 40:44], s[:, 44:48]
        ML = s[:, 48:80]
        m8 = s[:, 80:88]
        d21, e21, den, w1, w2 = s[:, 88:89], s[:, 89:90], s[:, 90:91], s[:, 91:92], s[:, 92:93]
        OH1, OH2 = s[:, 96:128], s[:, 128:160]
        SL = s[:, 160:192]
        TMP = s[:, 192:224]
        d1f, d2f = s[:, 224:225], s[:, 225:226]
        Ab = V(s.ap[:, 232:248].bitcast(BF16), s.keys)
        self.tt(LG, psr[:, 0:36], brt, ALU.add)
        rmx = lambda o, i: self.P.op("vector", (lambda oo, ii: lambda e: e.reduce_max(out=oo, in_=ii, axis=AX.X))(o.ap, i.ap),
                                     reads=_keys(i), writes=_keys(o))
        rsm = lambda o, i: self.P.op("vector", (lambda oo, ii: lambda e: e.reduce_sum(out=oo, in_=ii, axis=AX.X))(o.ap, i.ap),
                                     reads=_keys(i), writes=_keys(o))
        yield
        rmx(gmax, LG[:, 0:4])
        self.ts(ngmax, gmax, -1.0, None, ALU.mult)
        self.act(GE, LG[:, 0:4], AF.Exp, bias=ngmax)
        rsm(gsum, GE)
        yield
        self.recip(pg, gsum)
        self.ts(OHG, LG[:, 0:4], gmax, None, ALU.is_equal)
        self.ts(OHG, OHG, 1.0, 1e9, ALU.subtract, ALU.mult)
        self.tt(ML.re("p (g e) -> p g e", e=8), LG[:, 4:36].re("p (g e) -> p g e", e=8),
                V(OHG.ap.unsqueeze(2).to_broadcast([128, 4, 8]), OHG.keys), ALU.add)
        yield
        ml, m8a = ML.ap, m8.ap
        self.P.op("vector", lambda e: e.max(out=m8a, in_=ml), reads=_keys(ML), writes=_keys(m8))
        yield
        self.tt(d21, m8[:, 1:2], m8[:, 0:1], ALU.subtract)
        self.act(e21, d21, AF.Exp)
        self.ts(den, e21, 1.0, None, ALU.add)
        self.recip(den, den)
        self.tt(w1, pg, den, ALU.mult)
        self.tt(w2, w1, e21, ALU.mult)
        self.cp(WT[:, tile, 0:1].k(tile), w1)
        self.cp(WT[:, tile, 1:2].k(tile), w2)
        yield
        self.ts(OH1, ML, m8[:, 0:1], None, ALU.is_equal)
        self.ts(OH2, ML, m8[:, 1:2], None, ALU.is_equal)
        self.tt(Ab, OH1, OH2, ALU.add)
        yield
        psk = self.bank()
        self.mm(psk[:, 0:32], mSU_b, Ab)
        self.mm(psk[:, 32:64], ones_b, Ab)
        self.tt(SL, psk[:, 0:32], CNT, ALU.add)
        self.stt(SL, SL, float(CAP - 1), ECAP, ALU.min, ALU.add)
        self.tt(CNT, CNT, psk[:, 32:64], ALU.add)
        yield
        self.tt(TMP, OH1, SL, ALU.mult)
        rsm(d1f, TMP)
        self.tt(TMP, OH2, SL, ALU.mult)
        rsm(d2f, TMP)
        self.cp(DEST[:, tile, 0:1].k(tile), d1f)
        self.cp(DEST[:, tile, 1:2].k(tile), d2f)
        yield
        xg = E["xg_d"]
        self.scatter(V(xg.ap, [kk + (tile, 0) for kk in xg.keys]), DEST[:, tile, 0:1].k(tile), H2b)
        self.scatter(V(xg.ap, [kk + (tile, 1) for kk in xg.keys]), DEST[:, tile, 1:2].k(tile), H2b)

    def experts(self, E):
        A = self.A
        CAP, NB, L = self.CAP, self.NB, self.L
        NT = NB * L // 128
        ident_b = E["ident_b"]
        xg, yg = E["xg_d"], E["yg_d"]
        xg_all = V(xg.ap, [kk + (t, j) for kk in xg.keys for t in range(NT) for j in range(2)])
        nst = CAP // 128
        Wg = Ring([A.alloc("Wg%d" % i, (8, D), BF16) for i in range(2)])
        Wu = Ring([A.alloc("Wu%d" % i, (8, D), BF16) for i in range(2)])
        Wd = Ring([A.alloc("Wd%d" % i, (8, D), BF16) for i in range(2)])
        Xr = Ring([A.alloc("Xr%d" % i, D, BF16) for i in range(3)])
        XT = Ring([A.alloc("XTe%d" % i, (8, CAP), BF16) for i in range(2)])
        AT = Ring([A.alloc("ATe%d" % i, (8, CAP), BF16) for i in range(2)])
        SGr = Ring([A.alloc("SGe%d" % i, 512) for i in range(3)])
        Yr = Ring([A.alloc("Ye%d" % i, D) for i in range(3)])
        segs = []
        o = 0
        while o < CAP:
            n = min(512, CAP - o)
            segs.append((o, n))
            o += n
        for e in range(NEXP):
            wg, wu, wd = Wg.next(), Wu.next(), Wd.next()
            for (dst, src) in ((wg, E["weg_d"]), (wu, E["weu_d"]), (wd, E["wed_d"])):
                sv = src[e].re("(k p) n -> p k n", p=128)
                for k in range(8):
                    self.dma(dst[:, k, :].k(k), sv[:, k, :], eng="gpsimd")
            xT = XT.next()
            for s_ in range(nst):
                xr = Xr.next()
                self.dma(xr, V(xg_all.ap[e * CAP + s_ * 128:e * CAP + (s_ + 1) * 128, :], xg_all.keys))
                for half in range(2):
                    ps = self.bank()
                    for kk in range(4):
                        k = half * 4 + kk
                        self.mm(ps[:, kk * 128:(kk + 1) * 128], xr[:, k * 128:(k + 1) * 128], ident_b)
                    self.evac(xT[:, half * 4:(half + 1) * 4, s_ * 128:(s_ + 1) * 128].k(half, s_),
                              ps.re("p (a b) -> p a b", b=128))
            xTa = V(xT.ap, [kk + (hh, ss) for kk in xT.keys for hh in range(2) for ss in range(nst)])
            aT = AT.next()
            for j in range(8):
                for (o, n) in segs:
                    psg = self.bank()
                    for k in range(8):
                        self.mm(psg[:, 0:n], wg[:, k, j * 128:(j + 1) * 128].k(k), xTa[:, k, o:o + n], start=(k == 0), stop=(k == 7))
                    psu = self.bank()
                    for k in range(8):
                        self.mm(psu[:, 0:n], wu[:, k, j * 128:(j + 1) * 128].k(k), xTa[:, k, o:o + n], start=(k == 0), stop=(k == 7))
                    sg_ = SGr.next()
                    self.act(sg_[:, 0:n], psg[:, 0:n], AF.Silu)
                    self.tt(aT[:, j, o:o + n].k(j, o), sg_[:, 0:n], psu[:, 0:n], ALU.mult)
            aTa = V(aT.ap, [kk + (j, o) for kk in aT.keys for j in range(8) for (o, n) in segs])
            for s_ in range(nst):
                y = Yr.next()
                for half in range(2):
                    ps = self.bank()
                    for j in range(8):
                        self.mm(ps, aTa[:, j, s_ * 128:(s_ + 1) * 128], wd[:, j, half * 512:(half + 1) * 512].k(j),
                                start=(j == 0), stop=(j == 7))
                    self.evac(y[:, half * 512:(half + 1) * 512].k(half), ps)
                ya = V(y.ap, [kk + (hh,) for kk in y.keys for hh in range(2)])
                r0 = e * CAP + s_ * 128
                self.dma(V(yg.ap[r0:r0 + 128, :], [kk + (e, s_) for kk in yg.keys]), ya)

    def combine(self, E):
        A = self.A
        CAP, NB, L = self.CAP, self.NB, self.L
        NT = NB * L // 128
        nst = CAP // 128
        DEST, WT = E["DEST"], E["WT"]
        yg, x1 = E["yg_d"], E["x1_d"]
        yg_all = V(yg.ap, [kk + (e, s_) for kk in yg.keys for e in range(NEXP) for s_ in range(nst)])
        ln2g = A.alloc("ln2g", D)
        ln2b = A.alloc("ln2b", D)
        self.dma(ln2g, E["lnbc_d"][:, 2, :])
        self.dma(ln2b, E["lnbc_d"][:, 3, :])
        g2bc = [A.alloc("g2bc%d" % b, D) for b in range(NB)]
        bcs = E["bcs_d"]
        for b in range(NB):
            self.dma(g2bc[b], V(bcs.ap[b, 3], [kk + (b, ct) for kk in bcs.keys for ct in range(8)]))
        Y1r = Ring([A.alloc("Y1g%d" % i, D) for i in range(2)])
        Y2r = Ring([A.alloc("Y2g%d" % i, D) for i in range(2)])
        X1r = Ring([A.alloc("X1c%d" % i, D) for i in range(2)])
        Or = Ring([A.alloc("Oc%d" % i, D) for i in range(2)])
        def tile_gen(tile):
            b = tile * 128 // L
            Y1, Y2, X1, O = Y1r.next(), Y2r.next(), X1r.next(), Or.next()
            self.gather(Y1, yg_all, DEST[:, tile, 0:1].k(tile))
            self.gather(Y2, yg_all, DEST[:, tile, 1:2].k(tile))
            self.dma(X1, V(x1.ap[tile * 128:(tile + 1) * 128, :], [kk + (tile,) for kk in x1.keys]))
            yield
            self.ts(Y1, Y1, WT[:, tile, 0:1].k(tile), None, ALU.mult, eng="gpsimd")
            yield
            self.stt(Y1, Y2, WT[:, tile, 1:2].k(tile), Y1, ALU.mult, ALU.add)
            yield
            self.tt(Y1, Y1, g2bc[b], ALU.mult, eng="gpsimd")
            yield
            self.stt(Y1, X1, ALPHA, Y1, ALU.mult, ALU.add)
            yield
            mean, rstd = self.ln_stats(Y1)
            yield
            self.ts(Y1, Y1, mean, rstd, ALU.subtract, ALU.mult)
            yield
            self.tt(O, Y1, ln2g, ALU.mult, eng="gpsimd")
            yield
            self.tt(O, O, ln2b, ALU.add)
            self.dma(V(E["out_d"].ap[tile * 128:(tile + 1) * 128, :], [("out", tile)]), O, is_output=True)

        for t2 in range(0, NT, 2):
            lockstep([tile_gen(t2), tile_gen(t2 + 1)])


def chunk_starts():
    st = [n * 128 for n in range(8)]
    st += [1024 + n * 128 for n in range(8)]
    st += [2048 + n * 128 for n in range(24)]
    st += [5152 + n * 128 for n in range(8)]
    st += [6176 + n * 128 for n in range(16)]
    return st


def host_layout(inp, NB, core, NBP):
    f = lambda a: np.ascontiguousarray(a, dtype=np.float32)
    w_in = inp["w_in"][0]
    b_in = inp["b_in"][0]
    st = chunk_starts()
    w_in_t = np.stack([w_in[:, s:s + 128].reshape(8, 128, 128).transpose(1, 0, 2) for s in st], 0)
    w_in_lg = w_in[:, 5120:5152].reshape(8, 128, 32).transpose(1, 0, 2)
    b_inT = np.zeros((128, 65), np.float32)
    for i, s in enumerate(st):
        b_inT[:, i] = b_in[s:s + 128]
    b_inT[0:32, 64] = b_in[5120:5152]
    bs = slice(core * NB, (core + 1) * NB)
    c = inp["c"][bs]
    cT = np.zeros((128, 8, NBP), np.float32)
    for b in range(NB):
        cT[:, :, b] = c[b].reshape(8, 128).T
    cT[:, :, NB] = inp["c_ctx"].reshape(8, 128).T
    b_mod = inp["b_mod"][0]
    fm = lambda v: v.reshape(-1, 128).T
    rep = lambda v: np.broadcast_to(v[None, :], (128, v.shape[0]))
    lw = np.stack([inp["lru_wa"][0, 0], inp["lru_wx"][0, 0], inp["lru_wa"][0, 1], inp["lru_wx"][0, 1]], 0)
    lb = np.stack([inp["lru_ba"][0, 0], inp["lru_bx"][0, 0], inp["lru_ba"][0, 1], inp["lru_bx"][0, 1]], 0)
    conv_a = np.concatenate([inp["conv_a_w"][0], inp["conv_a_b"][0][None]], 0)
    d = {
        "x": inp["x"][bs], "ctx": inp["ctx"][bs], "cT": cT,
        "w_mod": inp["w_mod"][0], "bmodT": fm(b_mod[0:2048]), "bmod_bc": rep(b_mod[2048:]),
        "w_in_t": w_in_t, "w_in_lg": w_in_lg, "b_inT": b_inT,
        "conv_a": conv_a.reshape(5, 8, 128).transpose(2, 1, 0),
        "lru_w": lw.transpose(2, 0, 1, 3),
        "lru_b": lb.reshape(4, 8, 128).transpose(2, 0, 1),
        "lru_lam": inp["lru_lambda"][0].reshape(2, 8, 128).transpose(2, 0, 1),
        "conv_qkv": inp["conv_qkv_w"][0].reshape(4, 24, 128).transpose(2, 1, 0),
        "gdn_rows": rep(np.concatenate([inp["gdn_dt_bias"][0].reshape(16), inp["gdn_a_log"][0].reshape(16)])),
        "gdn_nw": inp["gdn_norm_w"][0].reshape(128, 1),
        "w_pa": inp["w_pa"][0], "w_pb": inp["w_pb"][0], "w_out": inp["w_out"][0],
        "ln_bc": np.stack([rep(inp["ln1_g"][0]), rep(inp["ln1_b"][0]), rep(inp["ln2_g"][0]), rep(inp["ln2_b"][0])], 1),
        "w_rt": np.concatenate([inp["w_router_g"][0], inp["w_router_e"][0]], 1).reshape(8, 128, 36).transpose(1, 0, 2),
        "b_rt_bc": rep(np.concatenate([inp["b_router_g"][0], inp["b_router_e"][0]])),
        "w_eg": inp["w_e_gate"][0], "w_eu": inp["w_e_up"][0], "w_ed": inp["w_e_down"][0],
    }
    return {k: f(v) for k, v in d.items()}


_CACHE = {}


def run(inputs, n_cores, NB, L, CTX, CAP, debug=()):
    key = (NB, L, CTX, CAP, tuple(debug))
    if key not in _CACHE:
        _CACHE[key] = KB(NB, L, CTX, CAP, debug).build()
    nc = _CACHE[key]
    NBP = NB + 1 + ((NB + 1) % 2)
    inp = {k: np.asarray(v) for k, v in inputs.items()}
    shared = None
    in_maps = []
    for core in range(n_cores):
        m = host_layout(inp, NB, core, NBP) if shared is None else None
        if shared is None:
            shared = m
        else:
            m = dict(shared)
            bs = slice(core * NB, (core + 1) * NB)
            m["x"] = np.ascontiguousarray(inp["x"][bs], dtype=np.float32)
            m["ctx"] = np.ascontiguousarray(inp["ctx"][bs], dtype=np.float32)
            c = inp["c"][bs]
            cT = shared["cT"].copy()
            for b in range(NB):
                cT[:, :, b] = c[b].reshape(8, 128).T
            m["cT"] = cT
        in_maps.append(m)
    res = run_bass_kernel_spmd(nc, in_maps, core_ids=list(range(n_cores)))
    return res


def kernel(**inputs):
    n_cores, NB, L, CTX, CAP = 8, 2, 4096, 256, 640
    res = run(inputs, n_cores, NB, L, CTX, CAP)
    outs = [r["out"].reshape(NB, L, D) for r in res.results]
    return np.concatenate(outs, 0).astype(np.float32)
```

```python
import math
from contextlib import ExitStack
import numpy as np
import concourse.bass as bass
import concourse.mybir as mybir
from concourse.bass_utils import run_bass_kernel_spmd

F32 = mybir.dt.float32
F32R = mybir.dt.float32r
BF16 = mybir.dt.bfloat16
I32 = mybir.dt.int32
AF = mybir.ActivationFunctionType
ALU = mybir.AluOpType
AX = mybir.AxisListType

ENGS = ("sync", "scalar", "gpsimd", "vector", "tensor")
NDMA_SEM = 16
SAME_ENGINE_SYNC = True

D = 1024
GW = 64
ALPHA = 2.0 ** 0.25
LN_EPS = 1e-6
NEXP = 32
HI_LVL = 99


class Prog:
    def __init__(self, nc, stack):
        self.nc = nc
        self.q = {e: [] for e in ENGS}
        self.cnt = {e: 0 for e in ENGS}
        self.esem = {e: stack.enter_context(nc.semaphore("es_" + e)) for e in ENGS}
        self.dsem, self.dcnt, self.dnext = {}, {}, {}
        for e in ("sync", "gpsimd"):
            self.dsem[e] = [stack.enter_context(nc.semaphore("ds_%s%d" % (e, i))) for i in range(NDMA_SEM)]
            self.dcnt[e] = [0] * NDMA_SEM
            self.dnext[e] = 0
        self.semobj = {}
        for e in ENGS:
            self.semobj[("e", e)] = self.esem[e]
        for e in self.dsem:
            for i, s in enumerate(self.dsem[e]):
                self.semobj[("d", e, i)] = s
        self.seen = {e: {} for e in ENGS}
        self.last_w = {}
        self.readers = {}
        self.out_tokens = []

    def _deps(self, eng, reads, writes):
        toks = []
        for k in reads:
            t = self.last_w.get(k)
            if t is not None:
                toks.append(t)
        for k in writes:
            t = self.last_w.get(k)
            if t is not None:
                toks.append(t)
            toks.extend(self.readers.get(k, ()))
        need = {}
        for (sk, v) in toks:
            if sk == ("e", eng) and (eng == "tensor" or not SAME_ENGINE_SYNC):
                continue
            if self.seen[eng].get(sk, 0) >= v:
                continue
            if need.get(sk, 0) < v:
                need[sk] = v
        for sk, v in need.items():
            self.seen[eng][sk] = v
        return list(need.items())

    def _commit(self, tok, reads, writes):
        for k in reads:
            if k in writes:
                continue
            self.readers.setdefault(k, []).append(tok)
        for k in writes:
            self.last_w[k] = tok
            self.readers[k] = []

    def op(self, eng, fn, reads=(), writes=()):
        psr = [k for k in reads if k[0] == "psb"]
        if psr:
            writes = list(writes) + psr
        waits = self._deps(eng, reads, writes)
        self.cnt[eng] += 1
        tok = (("e", eng), self.cnt[eng])
        self.q[eng].append((waits, fn, self.esem[eng], 1))
        self._commit(tok, reads, writes)
        return tok

    def dma(self, eng, fn, reads=(), writes=(), is_output=False):
        i = self.dnext[eng]
        self.dnext[eng] = (i + 1) % NDMA_SEM
        sk = ("d", eng, i)
        waits = self._deps(eng, reads, writes)
        prev = self.dcnt[eng][i]
        if prev > 0 and self.seen[eng].get(sk, 0) < prev:
            self.seen[eng][sk] = prev
            waits.append((sk, prev))
        self.dcnt[eng][i] += 16
        tok = (sk, self.dcnt[eng][i])
        self.q[eng].append((waits, fn, self.dsem[eng][i], 16))
        self._commit(tok, reads, writes)
        if is_output:
            self.out_tokens.append(tok)
        return tok

    def barrier(self):
        toks = [(("e", e), self.cnt[e]) for e in ENGS if self.cnt[e] > 0]
        for e in self.dsem:
            for i in range(NDMA_SEM):
                if self.dcnt[e][i] > 0:
                    toks.append((("d", e, i), self.dcnt[e][i]))
        for eng in ENGS:
            waits = []
            for (sk, v) in toks:
                if sk == ("e", eng) and eng == "tensor":
                    continue
                if self.seen[eng].get(sk, 0) >= v:
                    continue
                self.seen[eng][sk] = v
                waits.append((sk, v))
            if waits:
                self.q[eng].append((waits, None, None, 0))
        self.last_w = {}
        self.readers = {}

    def finish(self):
        self.q["sync"].append((list(self.out_tokens), None, None, 0))

    def emit(self, block):
        def run(engname):
            def body(eng):
                for (waits, fn, sem, inc) in self.q[engname]:
                    for (sk, v) in waits:
                        eng.wait_ge(self.semobj[sk], v)
                    if fn is not None:
                        fn(eng).then_inc(sem, inc)
            return body
        block.sync(run("sync"))
        block.scalar(run("scalar"))
        block.gpsimd(run("gpsimd"))
        block.vector(run("vector"))
        block.tensor(run("tensor"))


class V:
    __slots__ = ("ap", "keys")

    def __init__(self, ap, keys):
        self.ap = ap
        self.keys = tuple(keys)

    def __getitem__(self, idx):
        return V(self.ap[idx], self.keys)

    def re(self, pat, **kw):
        return V(self.ap.rearrange(pat, **kw), self.keys)

    def bc(self, shape):
        return V(self.ap.to_broadcast(list(shape)), self.keys)

    def k(self, *sub):
        return V(self.ap, [kk + tuple(sub) for kk in self.keys])

    def cast(self, dt):
        return V(self.ap.bitcast(dt), self.keys)


def _keys(*vs):
    out = []
    for v in vs:
        if isinstance(v, V):
            out.extend(v.keys)
    return out


def _a(v):
    return v.ap if isinstance(v, V) else v


DT_SIZE = {F32: 4, BF16: 2, I32: 4}


class Arena:
    def __init__(self, nc, stack, nbytes):
        self.words = nbytes // 4
        self.t = stack.enter_context(nc.sbuf_tensor("arena", [128, self.words], F32))
        self.off = 0
        self.uid = 0

    def alloc(self, name, free_shape, dt=F32):
        if isinstance(free_shape, int):
            free_shape = (free_shape,)
        nel = 1
        for s in free_shape:
            nel *= s
        words = (nel * DT_SIZE[dt] + 31) // 32 * 8
        assert self.off + words <= self.words, "SBUF arena overflow at %s (%d + %d > %d)" % (name, self.off, words, self.words)
        ap = self.t[:, self.off:self.off + words]
        if dt != F32:
            ap = ap.bitcast(dt)
        ap = ap[:, 0:nel]
        if len(free_shape) == 2:
            ap = ap.rearrange("p (a b) -> p a b", b=free_shape[1])
        elif len(free_shape) == 3:
            ap = ap.rearrange("p (a b c) -> p a b c", b=free_shape[1], c=free_shape[2])
        elif len(free_shape) == 4:
            ap = ap.rearrange("p (a b c d) -> p a b c d", b=free_shape[1], c=free_shape[2], d=free_shape[3])
        self.off += words
        self.uid += 1
        return V(ap, [(name, self.uid)])

    def mark(self):
        return self.off

    def reset(self, m):
        self.off = m


class Ring:
    def __init__(self, items):
        self.items = items
        self.i = 0

    def next(self):
        v = self.items[self.i % len(self.items)]
        self.i += 1
        return v


def lockstep(gens):
    alive = list(gens)
    while alive:
        for g_ in list(alive):
            try:
                next(g_)
            except StopIteration:
                alive.remove(g_)


class KB:
    def __init__(self, NB, L, CTX, CAP, debug=()):
        self.NB, self.L, self.CTX, self.CAP = NB, L, CTX, CAP
        self.R = L // GW
        self.debug = set(debug)
        self.nc = bass.Bass("TRN2", target_bir_lowering=False)
        self.alt = 0

    def din(self, name, shape, dt=F32):
        return V(self.nc.dram_tensor(name, list(shape), dt, kind="ExternalInput").ap(), [])

    def dscr(self, name, shape, dt):
        kind = "ExternalOutput" if name in self.debug else "Internal"
        return V(self.nc.dram_tensor(name, list(shape), dt, kind=kind).ap(), [(name,)])

    def mm(self, out, lhsT, rhs, start=True, stop=True):
        o, l, r = out.ap, lhsT.ap, rhs.ap
        self.P.op("tensor", lambda e: e.matmul(o, lhsT=l, rhs=r, start=start, stop=stop),
                  reads=_keys(lhsT, rhs), writes=_keys(out))

    def mmr(self, out, lhsT, rhs, start=True, stop=True):
        o, l, r = out.ap, lhsT.ap, rhs.ap
        self.P.op("tensor", lambda e: e.matmul(o, lhsT=l, rhs=r, start=start, stop=stop),
                  reads=_keys(lhsT, rhs), writes=_keys(out))

    def tr(self, out, in_, ident):
        o, i, d = out.ap, in_.ap, ident.ap
        self.P.op("tensor", lambda e: e.transpose(o, i, d), reads=_keys(in_, ident), writes=_keys(out))

    def act(self, out, in_, func, bias=0.0, scale=1.0, eng="scalar"):
        o, i, b, s = out.ap, in_.ap, _a(bias), _a(scale)
        self.P.op("scalar", lambda e: e.activation(out=o, in_=i, func=func, bias=b, scale=s),
                  reads=_keys(in_, bias, scale), writes=_keys(out))

    def ts(self, out, in0, s1, s2, op0, op1=None, eng="vector"):
        o, i, a, b = out.ap, in0.ap, _a(s1), _a(s2)
        if op1 is None:
            fn = lambda e: e.tensor_scalar(out=o, in0=i, scalar1=a, scalar2=None, op0=op0)
        else:
            fn = lambda e: e.tensor_scalar(out=o, in0=i, scalar1=a, scalar2=b, op0=op0, op1=op1)
        self.P.op(eng, fn, reads=_keys(in0, s1, s2), writes=_keys(out))

    def tt(self, out, in0, in1, op, eng="vector"):
        o, a, b = out.ap, in0.ap, in1.ap
        self.P.op(eng, lambda e: e.tensor_tensor(out=o, in0=a, in1=b, op=op), reads=_keys(in0, in1), writes=_keys(out))

    def stt(self, out, in0, scalar, in1, op0, op1):
        o, a, s, b = out.ap, in0.ap, _a(scalar), in1.ap
        self.P.op("vector", lambda e: e.scalar_tensor_tensor(out=o, in0=a, scalar=s, in1=b, op0=op0, op1=op1),
                  reads=_keys(in0, scalar, in1), writes=_keys(out))

    def cp(self, out, in_, eng="vector"):
        o, i = out.ap, in_.ap
        if eng == "scalar":
            self.P.op("scalar", lambda e: e.activation(out=o, in_=i, func=AF.Identity), reads=_keys(in_), writes=_keys(out))
        else:
            self.P.op(eng, lambda e: e.tensor_copy(out=o, in_=i), reads=_keys(in_), writes=_keys(out))

    def evac(self, out, in_):
        self.alt ^= 1
        self.cp(out, in_, eng="scalar" if self.alt else "vector")

    def memset(self, out, val, eng="gpsimd"):
        o = out.ap
        self.P.op(eng, lambda e: e.memset(o, val), writes=_keys(out))

    def scan(self, out, d0, d1, init):
        o, a, b, i = out.ap, d0.ap, d1.ap, _a(init)
        self.P.op("vector", lambda e: e.tensor_tensor_scan(out=o, data0=a, data1=b, initial=i, op0=ALU.mult, op1=ALU.add),
                  reads=_keys(d0, d1, init), writes=_keys(out))

    def dma(self, out, in_, eng="sync", is_output=False):
        o, i = out.ap, in_.ap
        if eng == "gpsimd":
            fn = lambda e: e.dma_start(out=o, in_=i, max_dma_last_dim=4096)
        else:
            fn = lambda e: e.dma_start(out=o, in_=i)
        self.P.dma(eng, fn, reads=_keys(in_), writes=_keys(out), is_output=is_output)

    def scatter(self, out_dram, idx, in_sb):
        o, x, i = out_dram.ap, idx.ap, in_sb.ap
        self.P.dma("gpsimd", lambda e: e.indirect_dma_start(out=o, out_offset=bass.IndirectOffsetOnAxis(ap=x, axis=0),
                                                            in_=i, in_offset=None),
                   reads=_keys(idx, in_sb), writes=_keys(out_dram))

    def gather(self, out_sb, in_dram, idx):
        o, x, i = out_sb.ap, idx.ap, in_dram.ap
        self.P.dma("gpsimd", lambda e: e.indirect_dma_start(out=o, out_offset=None, in_=i,
                                                            in_offset=bass.IndirectOffsetOnAxis(ap=x, axis=0)),
                   reads=_keys(idx, in_dram), writes=_keys(out_sb))

    def bank(self):
        return self.banks.next()

    def ln_stats(self, x):
        st = self.st_ring.next()
        mv = st[:, 12:14]
        self.P.op("vector", (lambda o, i: lambda e: e.bn_stats(out=o, in_=i))(st.ap[:, 0:6], x.ap[:, 0:512]),
                  reads=_keys(x), writes=_keys(st))
        self.P.op("vector", (lambda o, i: lambda e: e.bn_stats(out=o, in_=i))(st.ap[:, 6:12], x.ap[:, 512:1024]),
                  reads=_keys(x, st), writes=_keys(st))
        self.P.op("vector", (lambda o, i: lambda e: e.bn_aggr(out=o, in_=i))(mv.ap, st.ap[:, 0:12]),
                  reads=_keys(st), writes=_keys(st))
        self.act(st[:, 14:15], st[:, 13:14], AF.Sqrt, bias=LN_EPS)
        self.recip(st[:, 15:16], st[:, 14:15])
        return st[:, 12:13], st[:, 15:16]

    def recip(self, out, in_):
        o, i = out.ap, in_.ap
        self.P.op("vector", lambda e: e.reciprocal(out=o, in_=i), reads=_keys(in_), writes=_keys(out))

    def build(self):
        NB, L, CTX, CAP, R = self.NB, self.L, self.CTX, self.CAP, self.R
        nc = self.nc
        NBP = NB + 1 + ((NB + 1) % 2)
        NT = NB * L // 128
        x_d = self.din("x", [NB, L, D])
        ctx_d = self.din("ctx", [NB, CTX, D])
        cT_d = self.din("cT", [128, 8, NBP])
        wmod_d = self.din("w_mod", [D, 6 * D])
        bmodT_d = self.din("bmodT", [128, 16])
        bmodbc_d = self.din("bmod_bc", [128, 4 * D])
        wint_d = self.din("w_in_t", [64, 128, 8, 128])
        winlg_d = self.din("w_in_lg", [128, 8, 32])
        binT_d = self.din("b_inT", [128, 65])
        conva_d = self.din("conv_a", [128, 8, 5])
        lruw_d = self.din("lru_w", [128, 4, 8, 128])
        lrub_d = self.din("lru_b", [128, 4, 8])
        lrulam_d = self.din("lru_lam", [128, 2, 8])
        convq_d = self.din("conv_qkv", [128, 24, 4])
        gdnrows_d = self.din("gdn_rows", [128, 32])
        gdnnw_d = self.din("gdn_nw", [128, 1])
        wpa_d = self.din("w_pa", [D, D])
        wpb_d = self.din("w_pb", [D, D])
        wout_d = self.din("w_out", [D, D])
        lnbc_d = self.din("ln_bc", [128, 4, D])
        wrt_d = self.din("w_rt", [128, 8, 36])
        brt_d = self.din("b_rt_bc", [128, 36])
        weg_d = self.din("w_eg", [NEXP, D, D])
        weu_d = self.din("w_eu", [NEXP, D, D])
        wed_d = self.din("w_ed", [NEXP, D, D])
        out_d = V(nc.dram_tensor("out", [NB * L, D], F32, kind="ExternalOutput").ap(), [("out",)])
        yaT_d = self.dscr("yaT", [NB, 8, 128, L], BF16)
        ybT_d = self.dscr("ybT", [NB, 8, 128, L], BF16)
        sg_d = self.dscr("sg", [NB, 16, 128, L], F32)
        x1_d = self.dscr("x1s", [NB * L, D], F32)
        xg_d = self.dscr("xg", [NEXP * CAP, D], BF16)
        yg_d = self.dscr("yg", [NEXP * CAP, D], F32)
        bcs_d = self.dscr("bcs", [NB, 4, 128, D], F32)
        pq_d = self.dscr("pq", [32, 128, L], F32)
        dbg_d = {}

        st = ExitStack()
        with st:
            self.P = P = Prog(nc, st)
            cap_bytes = 206 * 1024
            self.A = A = Arena(nc, st, cap_bytes)
            banks = []
            for i in range(8):
                t = st.enter_context(nc.psum_tensor("psb%d" % i, [128, 512], F32))
                banks.append(V(t[:, :], [("psb", i)]))
            self.banks = Ring(banks)
            ident_f = A.alloc("ident_f", 128)
            ones_f = A.alloc("ones_f", 128)
            mIL = A.alloc("mIL", 128)
            mSL = A.alloc("mSL", 128)
            mIU = A.alloc("mIU", 128)
            mSU = A.alloc("mSU", 128)
            ident_b = A.alloc("ident_b", 128, BF16)
            ones_b = A.alloc("ones_b", 128, BF16)
            mSU_b = A.alloc("mSU_b", 128, BF16)
            self.st_ring = Ring([A.alloc("lnst%d" % i, 16) for i in range(4)])

            def mask(dst, pat_step, cm, op):
                self.memset(dst, 1.0)
                o = dst.ap
                self.P.op("gpsimd", lambda e: e.affine_select(out=o, in_=o, pattern=[[pat_step, 128]], base=0,
                                                               channel_multiplier=cm, compare_op=op, fill=0.0),
                          reads=_keys(dst), writes=_keys(dst))
            self.memset(ones_f, 1.0)
            mask(ident_f, -1, 1, ALU.is_equal)
            mask(mIL, -1, 1, ALU.is_ge)
            mask(mSL, -1, 1, ALU.is_gt)
            mask(mIU, 1, -1, ALU.is_ge)
            mask(mSU, 1, -1, ALU.is_gt)
            self.cp(ident_b, ident_f, eng="gpsimd")
            self.cp(ones_b, ones_f, eng="gpsimd")
            self.cp(mSU_b, mSU, eng="gpsimd")

            modT = A.alloc("modT", (16, NBP))
            binT = A.alloc("binT", 65)
            conva = A.alloc("conva", (8, 5))
            lrub = A.alloc("lrub", (4, 8))
            cch = A.alloc("cch", (2, 8))
            convq = A.alloc("convq", (24, 4))
            gdnrows = A.alloc("gdnrows", 32)
            nega = A.alloc("nega", 16)
            gdnnw = A.alloc("gdnnw", 1)
            lruw = A.alloc("lruw", (4, 8, 128), BF16)
            lru_state = A.alloc("lru_state", (8, 2))
            gdn_state = A.alloc("gdn_state", (8, 2, 128))
            DEST = A.alloc("DEST", (NT, 2), I32)
            WT = A.alloc("WT", (NT, 2))
            CNT = A.alloc("CNT", 32)
            ECAP = A.alloc("ECAP", 32)
            brt = A.alloc("brt", 36)
            for (dst, src) in ((binT, binT_d), (conva, conva_d), (lrub, lrub_d), (cch, lrulam_d), (convq, convq_d),
                               (gdnrows, gdnrows_d), (gdnnw, gdnnw_d), (brt, brt_d)):
                self.dma(dst, src)
            self.dma(lruw, lruw_d, eng="gpsimd")
            self.act(cch, cch, AF.Exp, scale=-1.0)
            self.act(cch, cch, AF.Ln, bias=1.0)
            self.ts(cch, cch, -8.0, None, ALU.mult)
            self.act(nega, gdnrows[:, 16:32], AF.Exp)
            self.ts(nega, nega, -1.0, None, ALU.mult)
            self.memset(CNT, 0.0)
            ec = ECAP.ap
            self.P.op("gpsimd", lambda e: e.iota(ec, pattern=[[CAP, 32]], base=0, channel_multiplier=0,
                                                 allow_small_or_imprecise_dtypes=True), writes=_keys(ECAP))
            zt = A.alloc("zt", D, BF16)
            self.memset(zt, 0.0)
            for e_ in range(NEXP):
                self.dma(V(xg_d.ap[e_ * CAP:(e_ + 1) * CAP, :].rearrange("(n p) d -> p n d", p=128), xg_d.keys),
                         V(zt.ap.unsqueeze(1).to_broadcast([128, CAP // 128, D]), zt.keys))
            base_mark = A.mark()

            cT = A.alloc("cT", (8, NBP))
            self.dma(cT, cT_d)
            sT = A.alloc("sT", (8, NBP))
            self.act(sT, cT, AF.Silu)
            bmodT = A.alloc("bmodT", 16)
            self.dma(bmodT, bmodT_d)
            wmA = A.alloc("wmA", (8, 2048))
            wm_v = wmod_d.re("(k p) n -> p k n", p=128)
            for k in range(8):
                self.dma(wmA[:, k, :].k(k), wm_v[:, k, 0:2048])
            for cc in range(16):
                ps = self.bank()
                for k in range(8):
                    self.mm(ps[:, 0:NBP], wmA[:, k, cc * 128:(cc + 1) * 128].k(k), sT[:, k, :], start=(k == 0), stop=(k == 7))
                self.ts(modT[:, cc, :], ps[:, 0:NBP], bmodT[:, cc:cc + 1], 1.0 if cc >= 8 else 0.0, ALU.add, ALU.add)
            A.reset(A.mark() - 0)
            sbc = [A.alloc("sbc%d" % b, (8, 128)) for b in range(NB)]
            for b in range(NB):
                self.cp(sbc[b], sT[:, :, b:b + 1].bc([128, 8, 128]))
            wmB = Ring([A.alloc("wmB%d" % i, (8, 512)) for i in range(2)])
            bmB = Ring([A.alloc("bmB%d" % i, 512) for i in range(2)])
            stg = Ring([A.alloc("stg%d" % i, 512) for i in range(3)])
            for ct in range(8):
                w = wmB.next()
                bm = bmB.next()
                for k in range(8):
                    self.dma(w[:, k, :].k(k), wm_v[:, k, 2048 + ct * 512:2048 + (ct + 1) * 512])
                self.dma(bm, bmodbc_d[:, ct * 512:(ct + 1) * 512])
                for b in range(NB):
                    ps = self.bank()
                    for k in range(8):
                        self.mm(ps, sbc[b][:, k, :], w[:, k, :].k(k), start=(k == 0), stop=(k == 7))
                    sg_ = stg.next()
                    if ct // 2 == 2:
                        self.stt(sg_, ps, 1.0, bm, ALU.add, ALU.add)
                    else:
                        self.tt(sg_, ps, bm, ALU.add)
                    self.dma(bcs_d[b, ct // 2, :, (ct % 2) * 512:(ct % 2 + 1) * 512].k(b, ct), sg_)
            self.P.barrier()
            A.reset(base_mark)

            import os as _os
            KSTOP = int(_os.environ.get("KSTOP", "99"))
            for b in range(NB if KSTOP > 0 else 0):
                for which in ("ctx", "lat"):
                    if KSTOP == 1 and which == "lat":
                        continue
                    tabs, tab_mark = self.mixer(b, which, locals())
                    self.P.barrier()
                    A.reset(tab_mark)
                    if KSTOP > 2:
                        self.gdn(b, which == "lat", tabs, locals())
                    self.P.barrier()
                    A.reset(base_mark)
                if KSTOP > 3:
                    self.merge(b, locals())
                self.P.barrier()
                A.reset(base_mark)
            if KSTOP > 4:
                self.experts(locals())
            self.P.barrier()
            A.reset(base_mark)
            if KSTOP > 5:
                self.combine(locals())
            P.finish()
            with nc.Block() as block:
                P.emit(block)
        return nc

    def load_wchunk(self, wring, wint_d, c):
        w = wring.next()
        self.dma(w, wint_d[c], eng="gpsimd")
        return w

    def proj(self, w, M, hT, Lx, evac_fn):
        TL = min(512, Lx)
        for t in range(Lx // TL):
            ps = self.bank()
            for k in range(8):
                self.mm(ps[0:M, 0:TL], w[:, k, 0:M], hT[:, k, t * TL:(t + 1) * TL], start=(k == 0), stop=(k == 7))
            evac_fn(t, TL, ps[0:M, 0:TL])

    def mixer(self, b, which, E):
        A, P = self.A, self.P
        NB, L, CTX, R = self.NB, self.L, self.CTX, self.R
        lat = which == "lat"
        Lx = L if lat else CTX
        nch = Lx // 128
        NH = nch * 8
        col = b if lat else NB
        src = E["x_d"][b] if lat else E["ctx_d"][b]
        modT, binT, ident_f, ident_b, ones_f, ones_b = E["modT"], E["binT"], E["ident_f"], E["ident_b"], E["ones_f"], E["ones_b"]
        mIL, mIU = E["mIL"], E["mIU"]
        gdnrows, nega = E["gdnrows"], E["nega"]
        wint_d = E["wint_d"]
        tabs = {}
        for nm in ("BT", "NBT", "GC", "EKG", "GL", "BEG"):
            tabs[nm] = A.alloc(nm, (2, nch, 8))
        tab_mark = A.mark()
        hT = A.alloc("hT", (8, Lx), BF16)
        m0 = A.mark()
        xin = Ring([A.alloc("xin%d" % i, D) for i in range(2)])
        xnr = Ring([A.alloc("xn%d" % i, D) for i in range(2)])
        def ht_gen(t):
            xt = xin.next()
            self.dma(xt, src[t * 128:(t + 1) * 128, :])
            yield
            mean, rstd = self.ln_stats(xt)
            yield
            xn = xnr.next()
            self.ts(xn, xt, mean, rstd, ALU.subtract, ALU.mult)
            yield
            for half in range(2):
                ps = self.bank()
                for kk in range(4):
                    k = half * 4 + kk
                    self.tr(ps[:, kk * 128:(kk + 1) * 128], xn[:, k * 128:(k + 1) * 128], ident_f)
                yield
                for kk in range(4):
                    k = half * 4 + kk
                    o = hT[:, k, t * 128:(t + 1) * 128].k(t)
                    if kk % 2 == 0:
                        self.act(o, ps[:, kk * 128:(kk + 1) * 128], AF.Identity, bias=modT[:, k, col:col + 1], scale=modT[:, 8 + k, col:col + 1])
                    else:
                        self.ts(o, ps[:, kk * 128:(kk + 1) * 128], modT[:, 8 + k, col:col + 1], modT[:, k, col:col + 1], ALU.mult, ALU.add)
                yield

        for t2 in range(0, Lx // 128, 2):
            lockstep([ht_gen(t2), ht_gen(t2 + 1)])
        hT = V(hT.ap, [kk + (t,) for kk in hT.keys for t in range(Lx // 128)])
        self.P.barrier()
        A.reset(m0)
        wring = Ring([A.alloc("wch%d" % i, (8, 128), BF16) for i in range(2)])
        m1 = A.mark()

        conva, lrub, cch, lruw, lru_state = E["conva"], E["lrub"], E["cch"], E["lruw"], E["lru_state"]
        xa_pad = A.alloc("xa_pad", Lx + 3)
        u = A.alloc("u", Lx)
        u_bf = A.alloc("u_bf", Lx, BF16)
        Ab = A.alloc("Abuf", Lx)
        Ib = A.alloc("Ibuf", Lx)
        HF = A.alloc("HF", Lx)
        Tb = V(xa_pad.ap[:, 0:Lx], xa_pad.keys)
        HB = Tb
        YA = V(Ib.ap.bitcast(BF16)[:, 0:Lx], Ib.keys)
        TL = min(512, Lx)
        for n in range(8):
            w = self.load_wchunk(wring, wint_d, n)
            self.memset(xa_pad[:, 0:1], 0.0)
            self.memset(xa_pad[:, Lx + 1:Lx + 3], 0.0)
            self.proj(w, 128, hT, Lx, lambda t, tl, ps: self.act(xa_pad[:, 1 + t * tl:1 + (t + 1) * tl], ps, AF.Identity,
                                                                    bias=binT[:, n:n + 1]))
            self.ts(u, xa_pad[:, 0:Lx], conva[:, n, 0:1], conva[:, n, 4:5], ALU.mult, ALU.add)
            for j in range(1, 4):
                self.stt(u, xa_pad[:, j:j + Lx], conva[:, n, j:j + 1], u, ALU.mult, ALU.add)
            self.cp(u_bf, u, eng="scalar")
            for d in range(2):
                for t in range(Lx // TL):
                    sl = slice(t * TL, (t + 1) * TL)
                    ps = self.bank()
                    self.mm(ps[:, 0:TL], lruw[:, 2 * d, n, :], u_bf[:, sl])
                    self.act(Ab[:, sl], ps[:, 0:TL], AF.Sigmoid, bias=lrub[:, 2 * d, n:n + 1])
                    ps2 = self.bank()
                    self.mm(ps2[:, 0:TL], lruw[:, 2 * d + 1, n, :], u_bf[:, sl])
                    self.act(Ib[:, sl], ps2[:, 0:TL], AF.Sigmoid, bias=lrub[:, 2 * d + 1, n:n + 1])
                self.act(Ab, Ab, AF.Exp, scale=cch[:, d, n:n + 1])
                self.tt(Tb, Ab, Ab, ALU.mult, eng="gpsimd")
                self.ts(Tb, Tb, 0.99999994, -1.0, ALU.min, ALU.mult)
                self.act(Tb, Tb, AF.Sqrt, bias=1.0)
                self.tt(Ib, Ib, u, ALU.mult, eng="gpsimd")
                self.tt(Ib, Ib, Tb, ALU.mult)
                init = lru_state[:, n, d:d + 1] if lat else 0.0
                if d == 0:
                    self.scan(HF, Ab, Ib, init)
                else:
                    self.scan(HB[:, ::-1], Ab[:, ::-1], Ib[:, ::-1], init)
            if not lat:
                self.cp(lru_state[:, n, 0:1], HF[:, Lx - 1:Lx])
                self.cp(lru_state[:, n, 1:2], HB[:, 0:1])
            else:
                self.tt(HF, HF, HB, ALU.add, eng="gpsimd")
                w = self.load_wchunk(wring, wint_d, 8 + n)
                self.proj(w, 128, hT, Lx, lambda t, tl, ps: self.act(Ab[:, t * tl:(t + 1) * tl], ps, AF.Identity,
                                                                        bias=binT[:, 8 + n:9 + n]))
                self.tt(Tb, Ab, Ab, ALU.mult, eng="gpsimd")
                self.ts(Tb, Tb, 0.044715, 1.0, ALU.mult, ALU.add)
                self.tt(Tb, Tb, Ab, ALU.mult)
                self.act(Tb, Tb, AF.Sigmoid, scale=2.0 * math.sqrt(2.0 / math.pi))
                self.tt(Tb, Tb, Ab, ALU.mult, eng="gpsimd")
                self.tt(YA, Tb, HF, ALU.mult)
                self.dma(E["yaT_d"][b, n].k(b, n), YA)
        self.P.barrier()
        A.reset(m1)

        SG = Ring([A.alloc("SG%d" % i, Lx) for i in range(2)])
        if lat:
            for dc in range(16):
                w = self.load_wchunk(wring, wint_d, 48 + dc)
                sgb = SG.next()
                self.proj(w, 128, hT, Lx, lambda t, tl, ps: self.act(sgb[:, t * tl:(t + 1) * tl], ps, AF.Sigmoid,
                                                                        bias=binT[:, 48 + dc:49 + dc]))
                self.dma(E["sg_d"][b, dc].k(b, dc), sgb)
        pq = E["pq_d"]
        for c in range(16, 48 if lat else 40):
            w = self.load_wchunk(wring, wint_d, c)
            sgb = SG.next()
            func = AF.Silu if c >= 40 else AF.Identity
            self.proj(w, 128, hT, Lx, lambda t, tl, ps: self.act(self.cm_out(sgb, 0, Lx, lat, t, tl), self.cm_in(ps, lat),
                                                                    func, bias=binT[:, c:c + 1]))
            self.dma(V(pq.ap[c - 16][:, 0:Lx], [kk + (c,) for kk in pq.keys]), sgb)
        self.P.barrier()
        A.reset(m1)

        wlg = A.alloc("wlg", (8, 32), BF16)
        self.dma(wlg, E["winlg_d"], eng="gpsimd")
        lgT = A.alloc("lgT", Lx)
        for t in range(Lx // TL):
            ps = self.bank()
            for k in range(8):
                self.mm(ps[0:32, 0:TL], wlg[:, k, :], hT[:, k, t * TL:(t + 1) * TL], start=(k == 0), stop=(k == 7))
            self.act(self.cm_out(lgT, 0, Lx, lat, t, TL)[0:32], self.cm_in(ps[0:32, 0:TL], lat), AF.Identity, bias=binT[0:32, 64:65])
        lg_tm = A.alloc("lg_tm", (nch, 32))
        for n in range(nch):
            ps = self.bank()
            self.tr(ps[:, 0:32], lgT[0:32, n * 128:(n + 1) * 128], ident_f[0:32, 0:32])
            self.evac(lg_tm[:, n, :], ps[:, 0:32])
        XG = A.alloc("XG", (2, nch, 8))
        AXb = A.alloc("AXb", (2, nch, 8))
        Gt = A.alloc("Gt", (2, nch, 8))
        GTOT = A.alloc("GTOT", (2, nch, 8))
        BT, NBT, GC, EKG, GL, BEG = (tabs[nm] for nm in ("BT", "NBT", "GC", "EKG", "GL", "BEG"))
        lg4 = lg_tm.re("p n (j h) -> p j n h", h=8)
        dtb = gdnrows[:, 0:16].re("p (d h) -> p d h", h=8)
        self.tt(XG, lg4[:, 0:2], V(dtb.ap.unsqueeze(2).to_broadcast([128, 2, nch, 8]), dtb.keys), ALU.add)
        self.act(AXb, XG, AF.Abs)
        self.act(AXb, AXb, AF.Exp, scale=-1.0)
        self.act(AXb, AXb, AF.Ln, bias=1.0)
        self.stt(XG, XG, 0.0, AXb, ALU.max, ALU.add)
        ng = nega.re("p (d h) -> p d h", h=8)
        self.tt(Gt, XG, V(ng.ap.unsqueeze(2).to_broadcast([128, 2, nch, 8]), ng.keys), ALU.mult)
        self.act(BT, lg4[:, 2:4], AF.Sigmoid)
        self.ts(NBT, BT, -1.0, None, ALU.mult)
        for d in range(2):
            ps = self.bank()
            tri = mIU if d == 0 else mIL
            self.mm(ps[:, 0:NH], tri, Gt[:, d].re("p n h -> p (n h)"))
            self.evac(GC[:, d].re("p n h -> p (n h)"), ps[:, 0:NH])
            ps = self.bank()
            self.mm(ps[:, 0:NH], ones_f, Gt[:, d].re("p n h -> p (n h)"))
            self.evac(GTOT[:, d].re("p n h -> p (n h)"), ps[:, 0:NH])
        self.tt(EKG, GTOT, GC, ALU.subtract)
        self.act(EKG, EKG, AF.Exp)
        self.act(GL, GTOT, AF.Exp)
        self.act(BEG, GC, AF.Exp)
        self.tt(BEG, BEG, BT, ALU.mult)
        return tabs, tab_mark

    def cm_out(self, buf, off, Lx, lat, t, tl):
        if not lat:
            return buf[:, off + t * tl:off + (t + 1) * tl]
        rows = tl // GW
        v = buf[:, off:off + Lx].re("p (c r) -> p r c", c=GW)
        return v[:, t * rows:(t + 1) * rows, :]

    def cm_in(self, ps, lat):
        if not lat:
            return ps
        return ps.re("p (r c) -> p r c", c=GW)

    def gdn(self, b, lat, tabs, E):
        A, P = self.A, self.P
        NB, R = self.NB, self.R
        Lx = self.L if lat else self.CTX
        nch = Lx // 128
        TL = min(512, Lx)
        binT, ident_f, ident_b, ones_f, ones_b = E["binT"], E["ident_f"], E["ident_b"], E["ones_f"], E["ones_b"]
        mIL, mSL, mIU, mSU = E["mIL"], E["mSL"], E["mIU"], E["mSU"]
        gdnnw, convq, gdn_state = E["gdnnw"], E["convq"], E["gdn_state"]
        BT, NBT, GC, EKG, GL, BEG = (tabs[nm] for nm in ("BT", "NBT", "GC", "EKG", "GL", "BEG"))
        pq = E["pq_d"]
        if not lat:
            self.memset(gdn_state, 0.0)
        PAD = A.alloc("PAD", Lx + 16)
        CV = A.alloc("CV", Lx)
        qT = A.alloc("qT", Lx, BF16)
        kT = A.alloc("kT", Lx, BF16)
        vT = A.alloc("vT", Lx, BF16)
        OT = A.alloc("OT", Lx)
        OTa = V(OT.ap, [kk + (n,) for kk in OT.keys for n in range(nch)])
        k_tm = A.alloc("k_tm", (nch, 128), BF16)
        v_tm = A.alloc("v_tm", (nch, 128), BF16)
        YB = V(PAD.ap[:, 0:Lx // 2].bitcast(BF16), PAD.keys)
        SQr = Ring([A.alloc("SQ%d" % i, TL, BF16) for i in range(2)])
        RTr = Ring([A.alloc("RT%d" % i, TL) for i in range(2)])
        S_bf = [A.alloc("S_bf%d" % d, 128, BF16) for d in range(2)]

        GC_ = 4 if nch % 4 == 0 else (2 if nch % 2 == 0 else 1)
        NI = 2 * GC_
        TMP = [{nm: A.alloc("%s_t%d" % (nm, i), 128) for nm in ("M1", "M2", "ER", "KB", "VB")} for i in range(NI)]
        for T_ in TMP:
            T_["DG"] = T_["M1"]
        BFR = [None] * NI
        PTI = [TMP[i]["M2"] for i in range(NI)]
        MB1 = [A.alloc("MB1_%d" % d, 128) for d in range(2)]
        MB2 = [A.alloc("MB2_%d" % d, 128) for d in range(2)]
        self.ts(MB1[0], mSL, -1e4, 1e4, ALU.mult, ALU.add, eng="gpsimd")
        self.ts(MB1[1], mSU, -1e4, 1e4, ALU.mult, ALU.add, eng="gpsimd")
        self.ts(MB2[0], mIU, 1e4, -1e4, ALU.mult, ALU.add, eng="gpsimd")
        self.ts(MB2[1], mIL, 1e4, -1e4, ALU.mult, ALU.add, eng="gpsimd")
        RNG = [{nm: Ring([A.alloc("%s_r%d_%d" % (nm, i, j), 128) for j in range(2)]) for nm in ("N", "M", "X")} for i in range(NI)]
        OUTT = [[{nm: A.alloc("%s_o%d_%d" % (nm, par, i), 128, F32 if nm == "UC" else BF16)
                  for nm in ("ACT", "WCT", "UC", "QG", "KG", "VN")} for i in range(NI)] for par in range(2)]

        import os as _os
        KG = int(_os.environ.get("KG", "99"))
        for h in range(8 if KG > 0 else 0):
            for (qi, dst) in ((0, qT), (1, kT), (2, vT)):
                c = 16 + qi * 8 + h
                self.memset(PAD[:, 7:8], 0.0)
                self.memset(PAD[:, Lx + 8:Lx + 10], 0.0)
                self.dma(PAD[:, 8:8 + Lx], V(pq.ap[c - 16][:, 0:Lx], [kk + (c,) for kk in pq.keys]))
                cw = convq[:, qi * 8 + h, :]
                self.ts(CV, PAD[:, 7:7 + Lx], cw[:, 0:1], None, ALU.mult)
                for j in range(1, 4):
                    self.stt(CV, PAD[:, 7 + j:7 + j + Lx], cw[:, j:j + 1], CV, ALU.mult, ALU.add)
                self.act(CV, CV, AF.Silu)
                if qi < 2:
                    for t in range(Lx // TL):
                        sl = slice(t * TL, (t + 1) * TL)
                        sq, rt = SQr.next(), RTr.next()
                        self.tt(sq, CV[:, sl], CV[:, sl], ALU.mult, eng="gpsimd")
                        ps = self.bank()
                        self.mm(ps[:, 0:TL], ones_b, sq)
                        self.act(rt, ps[:, 0:TL], AF.Sqrt, bias=1e-6)
                        self.recip(rt, rt)
                        self.stt(dst[:, sl], CV[:, sl], (128.0 ** -0.5) if qi == 0 else 1.0, rt, ALU.mult, ALU.mult)
                else:
                    self.cp(dst, CV, eng="scalar")
            if KG < 2:
                continue
            for n in range(nch):
                ps = self.bank()
                self.mm(ps[:, 0:128], kT[:, n * 128:(n + 1) * 128], ident_b)
                self.mm(ps[:, 128:256], vT[:, n * 128:(n + 1) * 128], ident_b)
                self.cp(k_tm[:, n, :].k(n), ps[:, 0:128], eng="scalar")
                self.cp(v_tm[:, n, :].k(n), ps[:, 128:256], eng="vector")
            self.memset(OTa, 0.0)
            for d in range(2):
                self.cp(S_bf[d], gdn_state[:, h, d, :], eng="scalar")
            ngroups = nch // GC_

            def group_insts(p):
                L_ = []
                for j in range(GC_):
                    for d in range(2):
                        s_ = p * GC_ + j
                        n = s_ if d == 0 else nch - 1 - s_
                        i = j * 2 + d
                        L_.append({"d": d, "n": n, "cs": slice(n * 128, (n + 1) * 128), "T": TMP[i], "R": RNG[i],
                                   "B": BFR[i], "PTI": PTI[i], "O": OUTT[p % 2][i]})
                return L_

            def pre(insts):
                for I in insts:
                    d, n, T = I["d"], I["n"], I["T"]
                    ktn, vtn = k_tm[:, n, :].k(n), v_tm[:, n, :].k(n)
                    I["Gp"] = GC[:, d, n, h:h + 1]
                    self.act(T["KB"], ktn, AF.Identity, scale=BEG[:, d, n, h:h + 1])
                    self.act(I["O"]["KG"], ktn, AF.Identity, scale=EKG[:, d, n, h:h + 1])
                    self.act(T["VB"], vtn, AF.Identity, scale=BT[:, d, n, h:h + 1])
                    self.ts(T["DG"], ident_f, I["Gp"], None, ALU.mult, eng="gpsimd")
                for I in insts:
                    I["psg"] = self.bank()
                    self.mm(I["psg"][:, 0:128], ones_f, I["T"]["DG"])
                for I in insts:
                    T, psg = I["T"], I["psg"]
                    self.stt(T["M1"], psg[:, 0:128], I["Gp"], MB1[I["d"]], ALU.subtract, ALU.max)
                    self.stt(T["M2"], psg[:, 0:128], I["Gp"], MB2[I["d"]], ALU.subtract, ALU.min)
                    self.act(T["ER"], psg[:, 0:128], AF.Exp)
                for I in insts:
                    T = I["T"]
                    self.act(T["M1"], T["M1"], AF.Exp, scale=-1.0)
                    self.act(T["M2"], T["M2"], AF.Exp)
                for I in insts:
                    cs = I["cs"]
                    I["psk"] = self.bank()
                    self.mm(I["psk"][:, 0:128], kT[:, cs], kT[:, cs])
                    self.mm(I["psk"][:, 128:256], kT[:, cs], qT[:, cs])
                for I in insts:
                    T, d, n = I["T"], I["d"], I["n"]
                    I["Nm"], I["Mm"], I["Xm"] = I["R"]["N"].next(), I["R"]["M"].next(), I["R"]["X"].next()
                    self.stt(I["Nm"], I["psk"][:, 0:128], NBT[:, d, n, h:h + 1], T["M1"], ALU.mult, ALU.mult)
                    self.tt(I["O"]["ACT"], I["psk"][:, 128:256], T["M2"], ALU.mult)
                for I in insts:
                    I["pst"] = self.bank()
                    self.tr(I["pst"][:, 0:128], I["Nm"].cast(F32), ident_f)
                for I in insts:
                    self.cp(I["Mm"], I["pst"][:, 0:128], eng="scalar")
                    self.tt(I["Xm"], I["Mm"].cast(F32), ident_f, ALU.add)
                    I["Pm"], I["PTm"] = I["Mm"], I["Nm"]
                    I["Xop"] = I["Xm"]
                for lvl in range(6):
                    last = lvl == 5
                    hi = lvl >= HI_LVL
                    nxt_hi = lvl >= HI_LVL - 1
                    for I in insts:
                        I["psl"] = self.bank()
                        if not last:
                            self.mm(I["psl"][:, 0:128], I["PTm"], I["Pm"])
                        self.mm(I["psl"][:, 128:256], I["Pm"], I["PTm"])
                    for I in insts:
                        if hi:
                            I["PT2"] = I["B"]["N"].next()
                            self.cp(I["PT2"], I["psl"][:, 128:256], eng="scalar")
                            I["PT2n"] = I["PT2"]
                        else:
                            I["PT2"] = I["R"]["N"].next()
                            self.cp(I["PT2"], I["psl"][:, 128:256], eng="scalar")
                            I["PT2n"] = I["PT2"]
                            if nxt_hi:
                                I["PT2n"] = I["B"]["N"].next()
                                self.cp(I["PT2n"], I["psl"][:, 128:256], eng="scalar")
                        if not last:
                            I["P2"] = (I["B"]["M"] if nxt_hi else I["R"]["M"]).next()
                            self.cp(I["P2"], I["psl"][:, 0:128], eng="vector")
                    for I in insts:
                        self.tt(I["PTI"], I["PT2"], ident_f, ALU.add, eng="gpsimd")
                    for I in insts:
                        I["psx"] = self.bank()
                        self.mm(I["psx"][:, 0:128], I["PTI"], I["Xop"])
                    for I in insts:
                        X2 = I["R"]["X"].next()
                        self.cp(X2, I["psx"][:, 0:128], eng="scalar" if lvl % 2 == 0 else "vector")
                        I["Xm"] = X2
                        I["Xop"] = X2
                        if nxt_hi and not last:
                            I["Xop"] = I["B"]["X"].next()
                            self.cp(I["Xop"], X2, eng="scalar")
                        I["PTm"] = I["PT2n"]
                        if not last:
                            I["Pm"] = I["P2"]
                for I in insts:
                    I["psw"] = self.bank()
                    self.mm(I["psw"][:, 0:128], I["T"]["KB"], I["Xm"].cast(F32))
                    self.mm(I["psw"][:, 128:256], I["Xm"].cast(F32), I["T"]["VB"])
                for I in insts:
                    self.cp(I["O"]["WCT"], I["psw"][:, 0:128], eng="scalar")
                    self.cp(I["O"]["UC"], I["psw"][:, 128:256], eng="vector")
                    self.tt(I["O"]["QG"], qT[:, I["cs"]], I["T"]["ER"], ALU.mult, eng="gpsimd")

            def state(insts):
                for I in insts:
                    d, n, cs, O = I["d"], I["n"], I["cs"], I["O"]
                    Sst = gdn_state[:, h, d, :]
                    pss = self.bank()
                    self.mm(pss[:, 0:128], O["WCT"], S_bf[d])
                    self.tt(O["VN"], O["UC"], pss[:, 0:128], ALU.subtract)
                    pso = self.bank()
                    self.mm(pso[:, 0:128], S_bf[d], O["QG"], start=True, stop=False)
                    self.mm(pso[:, 0:128], O["VN"], O["ACT"], start=False, stop=True)
                    psu = self.bank()
                    self.mm(psu[:, 0:128], O["KG"], O["VN"])
                    OTn = V(OT.ap[:, cs], [kk + (n,) for kk in OT.keys])
                    self.tt(OTn, OTn, pso[:, 0:128], ALU.add)
                    self.stt(Sst, Sst, GL[:, d, n, h:h + 1], psu[:, 0:128], ALU.mult, ALU.add)
                    self.cp(S_bf[d], Sst, eng="scalar")

            groups = [group_insts(p) for p in range(ngroups)]
            pre(groups[0])
            for p in range(ngroups):
                if p + 1 < ngroups:
                    pre(groups[p + 1])
                state(groups[p])
            if lat and KG > 6:
                c = 40 + h
                self.dma(CV, V(pq.ap[c - 16][:, 0:Lx], [kk + (c,) for kk in pq.keys]))
                for t in range(Lx // TL):
                    sl = slice(t * TL, (t + 1) * TL)
                    sq, rt = SQr.next(), RTr.next()
                    self.tt(sq, OTa[:, sl], OTa[:, sl], ALU.mult, eng="gpsimd")
                    ps = self.bank()
                    self.mm(ps[:, 0:TL], ones_b, sq)
                    self.act(rt, ps[:, 0:TL], AF.Sqrt, bias=LN_EPS, scale=1.0 / 128.0)
                    self.recip(rt, rt)
                    self.tt(CV[:, sl], CV[:, sl], rt, ALU.mult, eng="gpsimd")
                self.stt(YB.re("p (r c) -> p c r", c=GW), OTa.re("p (c r) -> p c r", c=GW), gdnnw[:, 0:1],
                         CV.re("p (c r) -> p c r", c=GW), ALU.mult, ALU.mult)
                self.dma(E["ybT_d"][b, h].k(b, h), YB)

    def merge(self, b, E):
        A, P = self.A, self.P
        NB, L, CAP = self.NB, self.L, self.CAP
        ident_f, ones_b, mSU_b = E["ident_f"], E["ones_b"], E["mSU_b"]
        DEST, WT, CNT, ECAP, brt = E["DEST"], E["WT"], E["CNT"], E["ECAP"], E["brt"]
        wpa = A.alloc("wpa", (8, D), BF16)
        wpb = A.alloc("wpb", (8, D), BF16)
        wout = A.alloc("wout", (8, D), BF16)
        for (dst, src) in ((wpa, E["wpa_d"]), (wpb, E["wpb_d"]), (wout, E["wout_d"])):
            sv = src.re("(k p) n -> p k n", p=128)
            for k in range(8):
                self.dma(dst[:, k, :].k(k), sv[:, k, :], eng="gpsimd")
        wrt = A.alloc("wrt", (8, 36))
        self.dma(wrt, E["wrt_d"])
        g1bc = A.alloc("g1bc", D)
        sh2bc = A.alloc("sh2bc", D)
        sc2bc = A.alloc("sc2bc", D)
        ln1g = A.alloc("ln1g", D)
        ln1b = A.alloc("ln1b", D)
        bcs = E["bcs_d"]
        bk = [kk + (b, ct) for kk in bcs.keys for ct in range(8)]
        self.dma(g1bc, V(bcs.ap[b, 0], bk))
        self.dma(sh2bc, V(bcs.ap[b, 1], bk))
        self.dma(sc2bc, V(bcs.ap[b, 2], bk))
        self.dma(ln1g, E["lnbc_d"][:, 0, :])
        self.dma(ln1b, E["lnbc_d"][:, 1, :])
        YAt = Ring([A.alloc("YAt%d" % i, (8, 512), BF16) for i in range(1)])
        YBt = Ring([A.alloc("YBt%d" % i, (8, 512), BF16) for i in range(1)])
        SGa = Ring([A.alloc("SGa%d" % i, 512) for i in range(3)])
        SGb = Ring([A.alloc("SGb%d" % i, 512) for i in range(3)])
        M1r = Ring([A.alloc("M1r%d" % i, 512) for i in range(2)])
        M2r = Ring([A.alloc("M2r%d" % i, 512) for i in range(2)])
        MTr = Ring([A.alloc("MT%d" % i, (8, 512), BF16) for i in range(1)])
        T1r = Ring([A.alloc("T1%d" % i, D) for i in range(2)])
        Xtr = Ring([A.alloc("Xt%d" % i, D) for i in range(2)])
        X1r = Ring([A.alloc("X1%d" % i, D) for i in range(2)])
        H2r = Ring([A.alloc("H2%d" % i, D) for i in range(2)])
        H2br = Ring([A.alloc("H2b%d" % i, D, BF16) for i in range(3)])
        H2Tr = Ring([A.alloc("H2T%d" % i, (8, 128)) for i in range(2)])
        sm = Ring([A.alloc("rsm%d" % i, 256) for i in range(3)])
        yav = E["yaT_d"]
        ybv = E["ybT_d"]
        sgv = E["sg_d"]
        TLm = 512
        for g in range(L // TLm):
            ts_ = slice(g * TLm, (g + 1) * TLm)
            ya, yb = YAt.next(), YBt.next()
            for k in range(8):
                self.dma(ya[:, k, :].k(k), V(yav.ap[b, k][:, ts_], [kk + (b, k) for kk in yav.keys]))
                self.dma(yb[:, k, :].k(k), V(ybv.ap[b, k][:, ts_], [kk + (b, k) for kk in ybv.keys]))
            MT = MTr.next()
            for dc in range(8):
                sga, sgb = SGa.next(), SGb.next()
                self.dma(sga, V(sgv.ap[b, dc][:, ts_], [kk + (b, dc) for kk in sgv.keys]))
                self.dma(sgb, V(sgv.ap[b, 8 + dc][:, ts_], [kk + (b, 8 + dc) for kk in sgv.keys]))
                psa = self.bank()
                for k in range(8):
                    self.mm(psa, wpa[:, k, dc * 128:(dc + 1) * 128].k(k), ya[:, k, :].k(k), start=(k == 0), stop=(k == 7))
                psb_ = self.bank()
                for k in range(8):
                    self.mm(psb_, wpb[:, k, dc * 128:(dc + 1) * 128].k(k), yb[:, k, :].k(k), start=(k == 0), stop=(k == 7))
                m1, m2 = M1r.next(), M2r.next()
                self.tt(m1, psa, sga, ALU.mult)
                self.tt(m2, psb_, sgb, ALU.mult)
                self.tt(MT[:, dc, :].k(dc), m1, m2, ALU.add, eng="gpsimd")
            MTa = V(MT.ap, [kk + (dc,) for kk in MT.keys for dc in range(8)])
            def sub_gen(sub):
                    tile = (b * L + g * TLm) // 128 + sub
                    tok0 = g * TLm + sub * 128
                    T1 = T1r.next()
                    for half in range(2):
                        ps = self.bank()
                        for dc in range(8):
                            self.mm(ps, MTa[:, dc, sub * 128:(sub + 1) * 128], wout[:, dc, half * 512:(half + 1) * 512].k(dc),
                                    start=(dc == 0), stop=(dc == 7))
                        self.tt(T1[:, half * 512:(half + 1) * 512].k(half), ps, g1bc[:, half * 512:(half + 1) * 512], ALU.mult)
                    T1a = V(T1.ap, [kk + (hh,) for kk in T1.keys for hh in range(2)])
                    yield
                    xt = Xtr.next()
                    self.dma(xt, E["x_d"][b][tok0:tok0 + 128, :])
                    self.stt(T1a, xt, ALPHA, T1a, ALU.mult, ALU.add)
                    yield
                    mean, rstd = self.ln_stats(T1a)
                    self.ts(T1a, T1a, mean, rstd, ALU.subtract, ALU.mult)
                    yield
                    X1 = X1r.next()
                    self.tt(X1, T1a, ln1g, ALU.mult, eng="gpsimd")
                    self.tt(X1, X1, ln1b, ALU.add, eng="gpsimd")
                    yield
                    self.dma(V(E["x1_d"].ap[tile * 128:(tile + 1) * 128, :], [kk + (tile,) for kk in E["x1_d"].keys]), X1)
                    yield from self.route(tile, X1, sh2bc, sc2bc, wrt, H2r, H2br, H2Tr, sm, E)
            lockstep([sub_gen(0), sub_gen(1)])
            lockstep([sub_gen(2), sub_gen(3)])

    def route(self, tile, X1, sh2bc, sc2bc, wrt, H2r, H2br, H2Tr, sm, E):
        CAP = self.CAP
        ident_f, ones_b, mSU_b = E["ident_f"], E["ones_b"], E["mSU_b"]
        DEST, WT, CNT, ECAP, brt = E["DEST"], E["WT"], E["CNT"], E["ECAP"], E["brt"]
        mean, rstd = self.ln_stats(X1)
        yield
        H2 = H2r.next()
        self.ts(H2, X1, mean, rstd, ALU.subtract, ALU.mult)
        self.tt(H2, H2, sc2bc, ALU.mult, eng="gpsimd")
        self.tt(H2, H2, sh2bc, ALU.add, eng="gpsimd")
        yield
        H2b = H2br.next()
        self.cp(H2b, H2, eng="scalar")
        H2T = H2Tr.next()
        for half in range(2):
            ps = self.bank()
            for kk in range(4):
                k = half * 4 + kk
                self.tr(ps[:, kk * 128:(kk + 1) * 128], H2[:, k * 128:(k + 1) * 128], ident_f)
            self.evac(H2T[:, half * 4:(half + 1) * 4, :].k(half), ps.re("p (a b) -> p a b", b=128))
        yield
        H2Ta = V(H2T.ap, [kk + (hh,) for kk in H2T.keys for hh in range(2)])
        psr = self.bank()
        for k in range(8):
            self.mm(psr[:, 0:36], H2Ta[:, k, :], wrt[:, k, :], start=(k == 0), stop=(k == 7))
        s = sm.next()
        LG = s[:, 0:36]
        gmax, ngmax, gsum, pg = s[:, 36:37], s[:, 37:38], s[:, 38:39], s[:, 39:40]
        GE, OHG = s[:, 40:44], s[:, 44:48]
        ML = s[:, 48:80]
        m8 = s[:, 80:88]
        d21, e21, den, w1, w2 = s[:, 88:89], s[:, 89:90], s[:, 90:91], s[:, 91:92], s[:, 92:93]
        OH1, OH2 = s[:, 96:128], s[:, 128:160]
        SL = s[:, 160:192]
        TMP = s[:, 192:224]
        d1f, d2f = s[:, 224:225], s[:, 225:226]
        Ab = V(s.ap[:, 232:248].bitcast(BF16), s.keys)
        self.tt(LG, psr[:, 0:36], brt, ALU.add)
        rmx = lambda o, i: self.P.op("vector", (lambda oo, ii: lambda e: e.reduce_max(out=oo, in_=ii, axis=AX.X))(o.ap, i.ap),
                                     reads=_keys(i), writes=_keys(o))
        rsm = lambda o, i: self.P.op("vector", (lambda oo, ii: lambda e: e.reduce_sum(out=oo, in_=ii, axis=AX.X))(o.ap, i.ap),
                                     reads=_keys(i), writes=_keys(o))
        yield
        rmx(gmax, LG[:, 0:4])
        self.ts(ngmax, gmax, -1.0, None, ALU.mult)
        self.act(GE, LG[:, 0:4], AF.Exp, bias=ngmax)
        rsm(gsum, GE)
        yield
        self.recip(pg, gsum)
        self.ts(OHG, LG[:, 0:4], gmax, None, ALU.is_equal)
        self.ts(OHG, OHG, 1.0, 1e9, ALU.subtract, ALU.mult)
        self.tt(ML.re("p (g e) -> p g e", e=8), LG[:, 4:36].re("p (g e) -> p g e", e=8),
                V(OHG.ap.unsqueeze(2).to_broadcast([128, 4, 8]), OHG.keys), ALU.add)
        yield
        ml, m8a = ML.ap, m8.ap
        self.P.op("vector", lambda e: e.max(out=m8a, in_=ml), reads=_keys(ML), writes=_keys(m8))
        yield
        self.tt(d21, m8[:, 1:2], m8[:, 0:1], ALU.subtract)
        self.act(e21, d21, AF.Exp)
        self.ts(den, e21, 1.0, None, ALU.add)
        self.recip(den, den)
        self.tt(w1, pg, den, ALU.mult)
        self.tt(w2, w1, e21, ALU.mult)
        self.cp(WT[:, tile, 0:1].k(tile), w1)
        self.cp(WT[:, tile, 1:2].k(tile), w2)
        yield
        self.ts(OH1, ML, m8[:, 0:1], None, ALU.is_equal)
        self.ts(OH2, ML, m8[:, 1:2], None, ALU.is_equal)
        self.tt(Ab, OH1, OH2, ALU.add)
        yield
        psk = self.bank()
        self.mm(psk[:, 0:32], mSU_b, Ab)
        self.mm(psk[:, 32:64], ones_b, Ab)
        self.tt(SL, psk[:, 0:32], CNT, ALU.add)
        self.stt(SL, SL, float(CAP - 1), ECAP, ALU.min, ALU.add)
        self.tt(CNT, CNT, psk[:, 32:64], ALU.add)
        yield
        self.tt(TMP, OH1, SL, ALU.mult)
        rsm(d1f, TMP)
        self.tt(TMP, OH2, SL, ALU.mult)
        rsm(d2f, TMP)
        self.cp(DEST[:, tile, 0:1].k(tile), d1f)
        self.cp(DEST[:, tile, 1:2].k(tile), d2f)
        yield
        xg = E["xg_d"]
        self.scatter(V(xg.ap, [kk + (tile, 0) for kk in xg.keys]), DEST[:, tile, 0:1].k(tile), H2b)
        self.scatter(V(xg.ap, [kk + (tile, 1) for kk in xg.keys]), DEST[:, tile, 1:2].k(tile), H2b)

    def experts(self, E):
        A = self.A
        CAP, NB, L = self.CAP, self.NB, self.L
        NT = NB * L // 128
        ident_b = E["ident_b"]
        xg, yg = E["xg_d"], E["yg_d"]
        xg_all = V(xg.ap, [kk + (t, j) for kk in xg.keys for t in range(NT) for j in range(2)])
        nst = CAP // 128
        Wg = Ring([A.alloc("Wg%d" % i, (8, D), BF16) for i in range(2)])
        Wu = Ring([A.alloc("Wu%d" % i, (8, D), BF16) for i in range(2)])
        Wd = Ring([A.alloc("Wd%d" % i, (8, D), BF16) for i in range(2)])
        Xr = Ring([A.alloc("Xr%d" % i, D, BF16) for i in range(3)])
        XT = Ring([A.alloc("XTe%d" % i, (8, CAP), BF16) for i in range(2)])
        AT = Ring([A.alloc("ATe%d" % i, (8, CAP), BF16) for i in range(2)])
        SGr = Ring([A.alloc("SGe%d" % i, 512) for i in range(3)])
        Yr = Ring([A.alloc("Ye%d" % i, D) for i in range(3)])
        segs = []
        o = 0
        while o < CAP:
            n = min(512, CAP - o)
            segs.append((o, n))
            o += n
        for e in range(NEXP):
            wg, wu, wd = Wg.next(), Wu.next(), Wd.next()
            for (dst, src) in ((wg, E["weg_d"]), (wu, E["weu_d"]), (wd, E["wed_d"])):
                sv = src[e].re("(k p) n -> p k n", p=128)
                for k in range(8):
                    self.dma(dst[:, k, :].k(k), sv[:, k, :], eng="gpsimd")
            xT = XT.next()
            for s_ in range(nst):
                xr = Xr.next()
                self.dma(xr, V(xg_all.ap[e * CAP + s_ * 128:e * CAP + (s_ + 1) * 128, :], xg_all.keys))
                for half in range(2):
                    ps = self.bank()
                    for kk in range(4):
                        k = half * 4 + kk
                        self.mm(ps[:, kk * 128:(kk + 1) * 128], xr[:, k * 128:(k + 1) * 128], ident_b)
                    self.evac(xT[:, half * 4:(half + 1) * 4, s_ * 128:(s_ + 1) * 128].k(half, s_),
                              ps.re("p (a b) -> p a b", b=128))
            xTa = V(xT.ap, [kk + (hh, ss) for kk in xT.keys for hh in range(2) for ss in range(nst)])
            aT = AT.next()
            for j in range(8):
                for (o, n) in segs:
                    psg = self.bank()
                    for k in range(8):
                        self.mm(psg[:, 0:n], wg[:, k, j * 128:(j + 1) * 128].k(k), xTa[:, k, o:o + n], start=(k == 0), stop=(k == 7))
                    psu = self.bank()
                    for k in range(8):
                        self.mm(psu[:, 0:n], wu[:, k, j * 128:(j + 1) * 128].k(k), xTa[:, k, o:o + n], start=(k == 0), stop=(k == 7))
                    sg_ = SGr.next()
                    self.act(sg_[:, 0:n], psg[:, 0:n], AF.Silu)
                    self.tt(aT[:, j, o:o + n].k(j, o), sg_[:, 0:n], psu[:, 0:n], ALU.mult)
            aTa = V(aT.ap, [kk + (j, o) for kk in aT.keys for j in range(8) for (o, n) in segs])
            for s_ in range(nst):
                y = Yr.next()
                for half in range(2):
                    ps = self.bank()
                    for j in range(8):
                        self.mm(ps, aTa[:, j, s_ * 128:(s_ + 1) * 128], wd[:, j, half * 512:(half + 1) * 512].k(j),
                                start=(j == 0), stop=(j == 7))
                    self.evac(y[:, half * 512:(half + 1) * 512].k(half), ps)
                ya = V(y.ap, [kk + (hh,) for kk in y.keys for hh in range(2)])
                r0 = e * CAP + s_ * 128
                self.dma(V(yg.ap[r0:r0 + 128, :], [kk + (e, s_) for kk in yg.keys]), ya)

    def combine(self, E):
        A = self.A
        CAP, NB, L = self.CAP, self.NB, self.L
        NT = NB * L // 128
        nst = CAP // 128
        DEST, WT = E["DEST"], E["WT"]
        yg, x1 = E["yg_d"], E["x1_d"]
        yg_all = V(yg.ap, [kk + (e, s_) for kk in yg.keys for e in range(NEXP) for s_ in range(nst)])
        ln2g = A.alloc("ln2g", D)
        ln2b = A.alloc("ln2b", D)
        self.dma(ln2g, E["lnbc_d"][:, 2, :])
        self.dma(ln2b, E["lnbc_d"][:, 3, :])
        g2bc = [A.alloc("g2bc%d" % b, D) for b in range(NB)]
        bcs = E["bcs_d"]
        for b in range(NB):
            self.dma(g2bc[b], V(bcs.ap[b, 3], [kk + (b, ct) for kk in bcs.keys for ct in range(8)]))
        Y1r = Ring([A.alloc("Y1g%d" % i, D) for i in range(2)])
        Y2r = Ring([A.alloc("Y2g%d" % i, D) for i in range(2)])
        X1r = Ring([A.alloc("X1c%d" % i, D) for i in range(2)])
        Or = Ring([A.alloc("Oc%d" % i, D) for i in range(2)])
        def tile_gen(tile):
            b = tile * 128 // L
            Y1, Y2, X1, O = Y1r.next(), Y2r.next(), X1r.next(), Or.next()
            self.gather(Y1, yg_all, DEST[:, tile, 0:1].k(tile))
            self.gather(Y2, yg_all, DEST[:, tile, 1:2].k(tile))
            self.dma(X1, V(x1.ap[tile * 128:(tile + 1) * 128, :], [kk + (tile,) for kk in x1.keys]))
            yield
            self.ts(Y1, Y1, WT[:, tile, 0:1].k(tile), None, ALU.mult, eng="gpsimd")
            yield
            self.stt(Y1, Y2, WT[:, tile, 1:2].k(tile), Y1, ALU.mult, ALU.add)
            yield
            self.tt(Y1, Y1, g2bc[b], ALU.mult, eng="gpsimd")
            yield
            self.stt(Y1, X1, ALPHA, Y1, ALU.mult, ALU.add)
            yield
            mean, rstd = self.ln_stats(Y1)
            yield
            self.ts(Y1, Y1, mean, rstd, ALU.subtract, ALU.mult)
            yield
            self.tt(O, Y1, ln2g, ALU.mult, eng="gpsimd")
            yield
            self.tt(O, O, ln2b, ALU.add)
            self.dma(V(E["out_d"].ap[tile * 128:(tile + 1) * 128, :], [("out", tile)]), O, is_output=True)

        for t2 in range(0, NT, 2):
            lockstep([tile_gen(t2), tile_gen(t2 + 1)])


def chunk_starts():
    st = [n * 128 for n in range(8)]
    st += [1024 + n * 128 for n in range(8)]
    st += [2048 + n * 128 for n in range(24)]
    st += [5152 + n * 128 for n in range(8)]
    st += [6176 + n * 128 for n in range(16)]
    return st


def host_layout(inp, NB, core, NBP):
    f = lambda a: np.ascontiguousarray(a, dtype=np.float32)
    w_in = inp["w_in"][0]
    b_in = inp["b_in"][0]
    st = chunk_starts()
    w_in_t = np.stack([w_in[:, s:s + 128].reshape(8, 128, 128).transpose(1, 0, 2) for s in st], 0)
    w_in_lg = w_in[:, 5120:5152].reshape(8, 128, 32).transpose(1, 0, 2)
    b_inT = np.zeros((128, 65), np.float32)
    for i, s in enumerate(st):
        b_inT[:, i] = b_in[s:s + 128]
    b_inT[0:32, 64] = b_in[5120:5152]
    bs = slice(core * NB, (core + 1) * NB)
    c = inp["c"][bs]
    cT = np.zeros((128, 8, NBP), np.float32)
    for b in range(NB):
        cT[:, :, b] = c[b].reshape(8, 128).T
    cT[:, :, NB] = inp["c_ctx"].reshape(8, 128).T
    b_mod = inp["b_mod"][0]
    fm = lambda v: v.reshape(-1, 128).T
    rep = lambda v: np.broadcast_to(v[None, :], (128, v.shape[0]))
    lw = np.stack([inp["lru_wa"][0, 0], inp["lru_wx"][0, 0], inp["lru_wa"][0, 1], inp["lru_wx"][0, 1]], 0)
    lb = np.stack([inp["lru_ba"][0, 0], inp["lru_bx"][0, 0], inp["lru_ba"][0, 1], inp["lru_bx"][0, 1]], 0)
    conv_a = np.concatenate([inp["conv_a_w"][0], inp["conv_a_b"][0][None]], 0)
    d = {
        "x": inp["x"][bs], "ctx": inp["ctx"][bs], "cT": cT,
        "w_mod": inp["w_mod"][0], "bmodT": fm(b_mod[0:2048]), "bmod_bc": rep(b_mod[2048:]),
        "w_in_t": w_in_t, "w_in_lg": w_in_lg, "b_inT": b_inT,
        "conv_a": conv_a.reshape(5, 8, 128).transpose(2, 1, 0),
        "lru_w": lw.transpose(2, 0, 1, 3),
        "lru_b": lb.reshape(4, 8, 128).transpose(2, 0, 1),
        "lru_lam": inp["lru_lambda"][0].reshape(2, 8, 128).transpose(2, 0, 1),
        "conv_qkv": inp["conv_qkv_w"][0].reshape(4, 24, 128).transpose(2, 1, 0),
        "gdn_rows": rep(np.concatenate([inp["gdn_dt_bias"][0].reshape(16), inp["gdn_a_log"][0].reshape(16)])),
        "gdn_nw": inp["gdn_norm_w"][0].reshape(128, 1),
        "w_pa": inp["w_pa"][0], "w_pb": inp["w_pb"][0], "w_out": inp["w_out"][0],
        "ln_bc": np.stack([rep(inp["ln1_g"][0]), rep(inp["ln1_b"][0]), rep(inp["ln2_g"][0]), rep(inp["ln2_b"][0])], 1),
        "w_rt": np.concatenate([inp["w_router_g"][0], inp["w_router_e"][0]], 1).reshape(8, 128, 36).transpose(1, 0, 2),
        "b_rt_bc": rep(np.concatenate([inp["b_router_g"][0], inp["b_router_e"][0]])),
        "w_eg": inp["w_e_gate"][0], "w_eu": inp["w_e_up"][0], "w_ed": inp["w_e_down"][0],
    }
    return {k: f(v) for k, v in d.items()}


_CACHE = {}


def run(inputs, n_cores, NB, L, CTX, CAP, debug=()):
    key = (NB, L, CTX, CAP, tuple(debug))
    if key not in _CACHE:
        _CACHE[key] = KB(NB, L, CTX, CAP, debug).build()
    nc = _CACHE[key]
    NBP = NB + 1 + ((NB + 1) % 2)
    inp = {k: np.asarray(v) for k, v in inputs.items()}
    shared = None
    in_maps = []
    for core in range(n_cores):
        m = host_layout(inp, NB, core, NBP) if shared is None else None
        if shared is None:
            shared = m
        else:
            m = dict(shared)
            bs = slice(core * NB, (core + 1) * NB)
            m["x"] = np.ascontiguousarray(inp["x"][bs], dtype=np.float32)
            m["ctx"] = np.ascontiguousarray(inp["ctx"][bs], dtype=np.float32)
            c = inp["c"][bs]
            cT = shared["cT"].copy()
            for b in range(NB):
                cT[:, :, b] = c[b].reshape(8, 128).T
            m["cT"] = cT
        in_maps.append(m)
    res = run_bass_kernel_spmd(nc, in_maps, core_ids=list(range(n_cores)))
    return res


def kernel(**inputs):
    n_cores, NB, L, CTX, CAP = 8, 2, 4096, 256, 640
    res = run(inputs, n_cores, NB, L, CTX, CAP)
    outs = [r["out"].reshape(NB, L, D) for r in res.results]
    return np.concatenate(outs, 0).astype(np.float32)
```
